# Optimizing a Trainium2 kernel written in Bass

```python
import math
import jax
import jax.numpy as jnp
from jax import lax
import numpy as np

D_MODEL = 1024
BATCH = 32
SEQ = 2048
DEPTH = 1

HEAD_DIM = 64
N_HEADS = D_MODEL // HEAD_DIM
NSA_HEADS = N_HEADS // 2
SB_HEADS = N_HEADS - NSA_HEADS
NSA_KV_GROUPS = 2
NSA_REP = NSA_HEADS // NSA_KV_GROUPS
CMP_BLOCK = 32
CMP_STRIDE = 16
CMP_HIDDEN = 256
SEL_BLOCK = 64
SEL_TOP = 8
SEL_QBLK = 32
WINDOW = 512
QBLK = 128
N_BUCKETS = 32
MAX_DISTANCE = 128
D_FF = 2816
CONV_WIDTH = 3
EPS = 1e-6
NEG_INF = -1e30
FORCED_BONUS = 1e6

NSA_WIDTH = NSA_HEADS * HEAD_DIM
SB_WIDTH = SB_HEADS * HEAD_DIM
KV_WIDTH = NSA_KV_GROUPS * HEAD_DIM
GATE_WIDTH = NSA_HEADS * 3
IN_SPLIT_SIZES = (NSA_WIDTH, KV_WIDTH, KV_WIDTH, KV_WIDTH, KV_WIDTH, KV_WIDTH, KV_WIDTH,
                  GATE_WIDTH, SB_WIDTH, SB_WIDTH, SB_WIDTH)
IN_COLS = sum(IN_SPLIT_SIZES)

kernel_name = 'hybrid_nsa_stickbreaking_convffn'


def _rms_norm(x, w):
    xf = x.astype(jnp.float32)
    y = xf * lax.rsqrt(jnp.mean(xf * xf, axis=-1, keepdims=True) + EPS)
    return (y * w.astype(jnp.float32)).astype(x.dtype)


def _head_rms_norm(o, w):
    B, T, H, Dh = o.shape
    of = o.astype(jnp.float32)
    y = of * lax.rsqrt(jnp.mean(of * of, axis=-1, keepdims=True) + EPS)
    return (y.reshape(B, T, H * Dh) * w.astype(jnp.float32)).astype(o.dtype)


def _split_cols(h):
    outs = []
    start = 0
    for size in IN_SPLIT_SIZES:
        outs.append(h[..., start:start + size])
        start += size
    return outs


def _t5_bucket(dist):
    n = jnp.maximum(dist, 0)
    max_exact = N_BUCKETS // 2
    nf = jnp.maximum(n, 1).astype(jnp.float32)
    log_b = max_exact + (jnp.log(nf / max_exact) / math.log(MAX_DISTANCE / max_exact)
                         * (N_BUCKETS - max_exact)).astype(jnp.int32)
    log_b = jnp.minimum(log_b, N_BUCKETS - 1)
    return jnp.where(n < max_exact, n, log_b)


def _masked_softmax(logits, mask):
    p = jax.nn.softmax(jnp.where(mask, logits, NEG_INF), axis=-1)
    return jnp.where(mask, p, 0.0)


def _compress(kv, pos, w1, w2):
    n_cmp = (kv.shape[2] - CMP_BLOCK) // CMP_STRIDE + 1
    idx = CMP_STRIDE * jnp.arange(n_cmp)[:, None] + jnp.arange(CMP_BLOCK)[None, :]
    blk = kv[:, :, idx] + pos
    blk = blk.reshape(blk.shape[0], blk.shape[1], n_cmp, CMP_BLOCK * HEAD_DIM)
    return jax.nn.gelu(blk @ w1) @ w2


def _gather_blocks(blocks, ix):
    return blocks[ix]


def _nsa(q, kc, vc, ks, vs, kw, vw, gates, pos_k, pos_v, k_w1, k_w2, v_w1, v_w2, rel_bias):
    B, G, R, T, Dh = q.shape
    scale = HEAD_DIM ** -0.5
    t_pos = jnp.arange(T)

    k_cmp = _compress(kc, pos_k, k_w1, k_w2)
    v_cmp = _compress(vc, pos_v, v_w1, v_w2)
    n_cmp = k_cmp.shape[2]
    cmp_end = CMP_STRIDE * jnp.arange(n_cmp) + CMP_BLOCK - 1
    dist_c = t_pos[:, None] - cmp_end[None, :]
    bias_c = rel_bias[_t5_bucket(dist_c)].transpose(2, 0, 1).reshape(G, R, T, n_cmp).astype(jnp.float32)
    logit_c = jnp.einsum('bgrtd,bgnd->bgrtn', q, k_cmp).astype(jnp.float32) * scale + bias_c
    p_cmp = _masked_softmax(logit_c, dist_c >= 0)
    o_cmp = jnp.einsum('bgrtn,bgnd->bgrtd', p_cmp.astype(v_cmp.dtype), v_cmp)

    n_sel = T // SEL_BLOCK
    sel_top = min(SEL_TOP, n_sel)
    sel_j = jnp.arange(n_sel)
    overlap = ((CMP_STRIDE * jnp.arange(n_cmp)[:, None] < SEL_BLOCK * (sel_j[None, :] + 1))
               & (cmp_end[:, None] + 1 > SEL_BLOCK * sel_j[None, :])).astype(jnp.float32)
    imp = jnp.einsum('bgrtn,nj->bgtj', p_cmp, overlap)
    cur = (t_pos // SEL_BLOCK)[:, None]
    causal_blk = sel_j[None, :] <= cur
    forced = (sel_j[None, :] == 0) | (sel_j[None, :] == cur) | (sel_j[None, :] == cur - 1)
    score = jnp.where(causal_blk, imp + jnp.where(forced, FORCED_BONUS, 0.0), NEG_INF)
    top_val, sel_idx = lax.top_k(score, sel_top)
    sel_ok = top_val > 0.5 * NEG_INF
    k_blocks = ks.reshape(B, G, n_sel, SEL_BLOCK, Dh)
    v_blocks = vs.reshape(B, G, n_sel, SEL_BLOCK, Dh)
    rel_bias_g = rel_bias.reshape(N_BUCKETS, G, R)
    g_idx = jnp.arange(G)[None, :, None, None]
    gather = jax.vmap(jax.vmap(_gather_blocks))
    n_kv = sel_top * SEL_BLOCK

    def sel_step(i):
        t0 = i * SEL_QBLK
        qb = lax.dynamic_slice_in_dim(q, t0, SEL_QBLK, axis=3)
        ib = lax.dynamic_slice_in_dim(sel_idx, t0, SEL_QBLK, axis=2)
        okb = lax.dynamic_slice_in_dim(sel_ok, t0, SEL_QBLK, axis=2)
        kg = gather(k_blocks, ib).reshape(B, G, SEL_QBLK, n_kv, Dh)
        vg = gather(v_blocks, ib).reshape(B, G, SEL_QBLK, n_kv, Dh)
        pos = (ib[..., None] * SEL_BLOCK + jnp.arange(SEL_BLOCK)).reshape(B, G, SEL_QBLK, n_kv)
        dist = (t0 + jnp.arange(SEL_QBLK))[:, None] - pos
        ok = jnp.broadcast_to(okb[..., None], okb.shape + (SEL_BLOCK,)).reshape(B, G, SEL_QBLK, n_kv) & (dist >= 0)
        bias = rel_bias_g[_t5_bucket(dist), g_idx].transpose(0, 1, 4, 2, 3).astype(jnp.float32)
        logits = jnp.einsum('bgrqd,bgqkd->bgrqk', qb, kg).astype(jnp.float32) * scale + bias
        p = _masked_softmax(logits, ok[:, :, None])
        return jnp.einsum('bgrqk,bgqkd->bgrqd', p.astype(vg.dtype), vg)

    o_sel = lax.map(sel_step, jnp.arange(T // SEL_QBLK))
    o_sel = o_sel.transpose(1, 2, 3, 0, 4, 5).reshape(B, G, R, T, Dh)

    span = QBLK + WINDOW
    k_pad = jnp.pad(kw, ((0, 0), (0, 0), (WINDOW, 0), (0, 0)))
    v_pad = jnp.pad(vw, ((0, 0), (0, 0), (WINDOW, 0), (0, 0)))
    band = jnp.arange(QBLK)[:, None] + WINDOW - jnp.arange(span)[None, :]
    band_ok = (band >= 0) & (band < WINDOW)
    bias_w = rel_bias[_t5_bucket(band)].transpose(2, 0, 1).reshape(G, R, QBLK, span).astype(jnp.float32)

    def win_step(i):
        t0 = i * QBLK
        qb = lax.dynamic_slice_in_dim(q, t0, QBLK, axis=3)
        kb = lax.dynamic_slice_in_dim(k_pad, t0, span, axis=2)
        vb = lax.dynamic_slice_in_dim(v_pad, t0, span, axis=2)
        s_real = t0 - WINDOW + jnp.arange(span)
        ok = band_ok & (s_real >= 0)[None, :]
        logits = jnp.einsum('bgrqd,bgkd->bgrqk', qb, kb).astype(jnp.float32) * scale + bias_w
        p = _masked_softmax(logits, ok)
        return jnp.einsum('bgrqk,bgkd->bgrqd', p.astype(vb.dtype), vb)

    o_win = lax.map(win_step, jnp.arange(T // QBLK))
    o_win = o_win.transpose(1, 2, 3, 0, 4, 5).reshape(B, G, R, T, Dh)

    o = gates[0] * o_cmp + gates[1] * o_sel + gates[2] * o_win
    return o.transpose(0, 3, 1, 2, 4).reshape(B, T, G * R, Dh)


def _stick_breaking(q, k, v):
    T = q.shape[2]
    scale = HEAD_DIM ** -0.5
    outs = []
    for nb in range(T // QBLK):
        t0 = nb * QBLK
        L = t0 + QBLK
        qb = q[:, :, t0:L]
        kb = k[:, :, :L]
        vb = v[:, :, :L]
        z = jnp.einsum('bhqd,bhkd->bhqk', qb, kb).astype(jnp.float32) * scale
        mask = jnp.arange(L)[None, :] < (t0 + jnp.arange(QBLK))[:, None]
        log_rest = jnp.where(mask, jax.nn.log_sigmoid(-z), 0.0)
        after = lax.cumsum(log_rest, axis=3, reverse=True) - log_rest
        a = jnp.where(mask, jnp.exp(jax.nn.log_sigmoid(z) + after), 0.0)
        outs.append(jnp.einsum('bhqk,bhkd->bhqd', a.astype(vb.dtype), vb))
    return jnp.concatenate(outs, axis=2)


def setup_inputs(seed: int = 0) -> dict:
    key = jax.random.key(seed)
    ks = jax.random.split(key, 24)
    f32 = jnp.float32
    L = DEPTH

    def normal(k, shape, scale):
        return jax.random.normal(k, shape, f32) * scale

    def gain(k, shape):
        return 1.0 + 0.02 * jax.random.normal(k, shape, f32)

    return {
        'x': normal(ks[0], (BATCH, SEQ, D_MODEL), 1.0),
        'norm1_w': gain(ks[1], (L, D_MODEL)),
        'w_in': normal(ks[2], (L, D_MODEL, IN_COLS), D_MODEL ** -0.5),
        'cmp_pos_k': normal(ks[3], (L, CMP_BLOCK, HEAD_DIM), 0.1),
        'cmp_pos_v': normal(ks[4], (L, CMP_BLOCK, HEAD_DIM), 0.1),
        'cmp_k_w1': normal(ks[5], (L, CMP_BLOCK * HEAD_DIM, CMP_HIDDEN), (CMP_BLOCK * HEAD_DIM) ** -0.5),
        'cmp_k_w2': normal(ks[6], (L, CMP_HIDDEN, HEAD_DIM), CMP_HIDDEN ** -0.5),
        'cmp_v_w1': normal(ks[7], (L, CMP_BLOCK * HEAD_DIM, CMP_HIDDEN), (CMP_BLOCK * HEAD_DIM) ** -0.5),
        'cmp_v_w2': normal(ks[8], (L, CMP_HIDDEN, HEAD_DIM), CMP_HIDDEN ** -0.5),
        'gate_b': normal(ks[9], (L, GATE_WIDTH), 0.01),
        'nsa_out_norm_w': gain(ks[10], (L, NSA_WIDTH)),
        'sb_out_norm_w': gain(ks[11], (L, SB_WIDTH)),
        'w_out': normal(ks[12], (L, D_MODEL, D_MODEL), D_MODEL ** -0.5),
        'norm2_w': gain(ks[13], (L, D_MODEL)),
        'w_up': normal(ks[14], (L, D_MODEL, 2 * D_FF), D_MODEL ** -0.5),
        'conv_w': normal(ks[15], (L, CONV_WIDTH, 2 * D_FF), CONV_WIDTH ** -0.5),
        'conv_b': normal(ks[16], (L, 2 * D_FF), 0.01),
        'w_down': normal(ks[17], (L, D_FF, D_MODEL), D_FF ** -0.5),
        'rel_bias': normal(ks[18], (N_BUCKETS, NSA_HEADS), 0.1),
        'final_norm_w': gain(ks[19], (D_MODEL,)),
    }


def _to_q_groups(a, B, T):
    return a.reshape(B, T, NSA_KV_GROUPS, NSA_REP, HEAD_DIM).transpose(0, 2, 3, 1, 4)


def _to_kv_groups(a, B, T):
    return a.reshape(B, T, NSA_KV_GROUPS, HEAD_DIM).transpose(0, 2, 1, 3)


def _to_heads(a, B, T):
    return a.reshape(B, T, SB_HEADS, HEAD_DIM).transpose(0, 2, 1, 3)


def reference(x, norm1_w, w_in, cmp_pos_k, cmp_pos_v, cmp_k_w1, cmp_k_w2, cmp_v_w1, cmp_v_w2,
              gate_b, nsa_out_norm_w, sb_out_norm_w, w_out, norm2_w, w_up, conv_w, conv_b,
              w_down, rel_bias, final_norm_w):
    B, T, _ = x.shape
    h = x
    for layer in range(DEPTH):
        u = _rms_norm(h, norm1_w[layer])
        proj = u @ w_in[layer]
        q_n, kc, vc, ks, vs, kw, vw, g_lin, q_s, k_s, v_s = _split_cols(proj)
        gates = jax.nn.sigmoid((g_lin + gate_b[layer]).astype(jnp.float32)).astype(x.dtype)
        gates = gates.reshape(B, T, NSA_KV_GROUPS, NSA_REP, 3).transpose(4, 0, 2, 3, 1)[..., None]
        o_nsa = _nsa(_to_q_groups(q_n, B, T),
                     _to_kv_groups(kc, B, T), _to_kv_groups(vc, B, T),
                     _to_kv_groups(ks, B, T), _to_kv_groups(vs, B, T),
                     _to_kv_groups(kw, B, T), _to_kv_groups(vw, B, T),
                     gates, cmp_pos_k[layer], cmp_pos_v[layer],
                     cmp_k_w1[layer], cmp_k_w2[layer], cmp_v_w1[layer], cmp_v_w2[layer],
                     rel_bias)
        o_sb = _stick_breaking(_to_heads(q_s, B, T), _to_heads(k_s, B, T),
                               _to_heads(v_s, B, T)).transpose(0, 2, 1, 3)
        mixed = jnp.concatenate([_head_rms_norm(o_nsa, nsa_out_norm_w[layer]),
                                 _head_rms_norm(o_sb, sb_out_norm_w[layer])], axis=-1)
        h = h + mixed @ w_out[layer]
        u = _rms_norm(h, norm2_w[layer])
        up = u @ w_up[layer]
        up = lax.conv_general_dilated(up, conv_w[layer][:, None, :].astype(up.dtype),
                                      window_strides=(1,), padding=[(CONV_WIDTH - 1, 0)],
                                      dimension_numbers=('NWC', 'WIO', 'NWC'),
                                      feature_group_count=2 * D_FF) + conv_b[layer]
        gate, val = up[..., :D_FF], up[..., D_FF:]
        h = h + (jax.nn.silu(gate) * val) @ w_down[layer]
    return _rms_norm(h, final_norm_w)
```

```python
import numpy as np
from contextlib import ExitStack
import concourse.bass as bass
import concourse.mybir as mybir
from concourse.bass_utils import run_bass_kernel_spmd

F32 = mybir.dt.float32
BF16 = mybir.dt.bfloat16
AF = mybir.ActivationFunctionType
ALU = mybir.AluOpType
AX = mybir.AxisListType

T = 2048
D = 1024
DFF = 2816
NCORE = 8
NEG = -30000.0
EPS = 1e-6
ENGS = ['pe', 'act', 'dve', 'pool', 'sp']
CH = 4096
NWS = 6


class Sched:
    def __init__(self):
        self.prog = {e: [] for e in ENGS}
        self.cnt = {e: 0 for e in ENGS}
        self.seen = {e: {} for e in ENGS}
        self.lastw = {}
        self.readers = {}
        self.dcnt = {}
        self.targets = {e: set() for e in ENGS}

    def _collect(self, r, w):
        deps = set()
        for k in r:
            if k in self.lastw:
                deps.add(self.lastw[k])
        for k in w:
            if k in self.lastw:
                deps.add(self.lastw[k])
            deps.update(self.readers.get(k, ()))
        return deps

    def _waits(self, eng, deps):
        best = {}
        for (src, v) in deps:
            if src == eng and eng == 'pe':
                continue
            if self.seen[eng].get(src, -1) >= v:
                continue
            best[src] = max(best.get(src, -1), v)
        out = []
        for src, v in best.items():
            self.seen[eng][src] = v
            out.append((src, v))
            if src in self.targets:
                self.targets[src].add(v)
        return out

    def _record(self, opid, r, w):
        for k in r:
            self.readers.setdefault(k, set()).add(opid)
        for k in w:
            self.lastw[k] = opid
            self.readers[k] = set()

    def op(self, eng, fn, r=(), w=()):
        waits = self._waits(eng, self._collect(r, w))
        i = self.cnt[eng]
        self.cnt[eng] += 1
        self.prog[eng].append((waits, fn, (eng, i)))
        self._record((eng, i), r, w)

    def dma(self, eng, out, in_, slot, r=(), w=()):
        waits = self._waits(eng, self._collect(r, w))
        n = self.dcnt.get(slot, 0) + 1
        self.dcnt[slot] = n
        src = ('dma', slot)
        self.prog[eng].append((waits, (lambda e, o=out, i=in_: e.dma_start(out=o, in_=i)), (src, n)))
        self._record((src, n), r, w)

    def barrier(self):
        latest = []
        for e in ENGS:
            if self.cnt[e] > 0:
                latest.append((e, self.cnt[e] - 1))
        for slot, n in self.dcnt.items():
            latest.append((('dma', slot), n))
        for e in ENGS:
            waits = self._waits(e, latest)
            if waits:
                self.prog[e].append((waits, None, None))
        self.lastw.clear()
        self.readers.clear()

    def emit(self, nc, st):
        sems = {}

        def getsem(key):
            if key not in sems:
                sems[key] = st.enter_context(nc.semaphore("s%d" % len(sems)))
            return sems[key]
        rank = {}
        for e in ENGS:
            tl = sorted(self.targets[e])
            rank[e] = {v: k for k, v in enumerate(tl)}

        def wait_of(src, v):
            if isinstance(src, tuple):
                return getsem(src), 16 * v
            k = rank[src][v]
            return getsem((src, k // CH)), k % CH + 1
        plan = {}
        for e in ENGS:
            lst = []
            for waits, fn, opid in self.prog[e]:
                ws = [wait_of(s, v) for (s, v) in waits]
                inc = None
                if fn is not None:
                    src, v = opid
                    if isinstance(src, tuple):
                        inc = (getsem(src), 16)
                    elif v in rank[src]:
                        k = rank[src][v]
                        inc = (getsem((src, k // CH)), 1)
                lst.append((ws, fn, inc))
            plan[e] = lst

        def run(name, e):
            for ws, fn, inc in plan[name]:
                for (s, v) in ws:
                    e.wait_ge(s, v)
                if fn is not None:
                    ins = fn(e)
                    if inc is not None:
                        ins.then_inc(inc[0], inc[1])
        block = st.enter_context(nc.Block())

        @block.tensor
        def _(e):
            run('pe', e)

        @block.scalar
        def _(e):
            run('act', e)

        @block.vector
        def _(e):
            run('dve', e)

        @block.gpsimd
        def _(e):
            run('pool', e)

        @block.sync
        def _(e):
            run('sp', e)


class Arena:
    def __init__(self, t, nelem_bf16):
        self.t = t
        self.n = nelem_bf16
        self.off = 0
        self.peak = 0

    def mark(self):
        return self.off

    def release(self, m):
        self.off = m

    def alloc(self, shape, dtype, parts=128):
        sz = 4 if dtype == F32 else 2
        n = 1
        for s in shape:
            n *= s
        nb = (n * sz + 3) // 4 * 4
        start = self.off
        self.off += nb
        self.peak = max(self.peak, self.off)
        assert self.off <= self.n * 2, ("arena overflow", self.off)
        ap = self.t[:, start // 2:(start + n * sz) // 2]
        if dtype == F32:
            ap = ap.bitcast(F32)
        if len(shape) == 2:
            ap = ap.rearrange("p (a b) -> p a b", a=shape[0])
        elif len(shape) == 3:
            ap = ap.rearrange("p (a b c) -> p a b c", a=shape[0], b=shape[1])
        if parts != 128:
            ap = ap[0:parts]
        return ap


def _t5_bucket_np(dist):
    n = np.maximum(dist, 0)
    nf = np.maximum(n, 1).astype(np.float32)
    lb = 16 + (np.log(nf / 16.0) / np.log(8.0) * 16.0).astype(np.int32)
    lb = np.minimum(lb, 31)
    return np.where(n < 16, n, lb)


def _win_chunks():
    ch = []
    for c in range(4):
        ch.append(list(range(128 * c, 128 * c + 128)))
    for g in range(2):
        ch.append(list(range(512 + 64 * g, 576 + 64 * g)) + list(range(640 + 64 * g, 704 + 64 * g)))
    for g in range(2):
        ch.append(list(range(768 + 64 * g, 832 + 64 * g)) * 2)
    for g in range(2):
        ch.append(list(range(1024 + 64 * g, 1088 + 64 * g)) * 2)
    ch.append(list(range(896, 1024)))
    ch.append(list(range(1152, 1280)))
    ch.append(list(range(1280, 1304)) + [-1] * 104)
    for c in range(4):
        ch.append(list(range(1304 + 128 * c, 1304 + 128 * c + 128)))
    for c in range(4):
        ch.append(list(range(1816 + 128 * c, 1816 + 128 * c + 128)))
    for c in range(4):
        ch.append(list(range(2328 + 128 * c, 2328 + 128 * c + 128)))
    return ch


def _host_consts():
    c = {}
    c['c_ident'] = np.eye(128, dtype=np.float32)
    j = np.arange(128)[:, None]
    s = np.arange(128)[None, :]
    c['c_tri'] = (j >= s).astype(np.float32)
    c['c_strict'] = np.where(s <= j, NEG, 0.0).astype(np.float32)
    c['c_wincorr'] = np.where(s >= j, NEG, 0.0).astype(np.float32)
    sel = np.zeros((32, 16, 128), np.float32)
    for cc in range(16):
        for sp in range(128):
            sel[2 * cc + sp // 64, cc, sp] = 1.0
    c['c_sele'] = sel.reshape(32, 16 * 128)
    n = np.arange(127)[:, None]
    sj = np.arange(32)[None, :]
    ovl = ((16 * n < 64 * (sj + 1)) & (16 * n + 32 > 64 * sj)).astype(np.float32)
    c['c_ovl'] = ovl
    addc = np.zeros((128, 16, 32), np.float32)
    for i in range(16):
        t = 128 * i + np.arange(128)
        cur = (t // 64)[:, None]
        jj = np.arange(32)[None, :]
        forced = (jj == 0) | (jj == cur) | (jj == cur - 1)
        a = np.where(forced, 1e6, 0.0)
        a = np.where(jj <= cur, a, -1e30)
        addc[:, i, :] = a
    c['c_addc'] = addc.reshape(128, 512)
    idx = np.zeros((128, 503), np.float32)
    msk = np.zeros((128, 503), np.float32)
    p = np.arange(128)[:, None]
    jp = np.arange(256)[None, :]
    d1 = jp - p
    idx[:, 0:256] = _t5_bucket_np(d1)
    msk[:, 0:256] = np.where(d1 < 0, NEG, 0.0)
    m = np.arange(247)[None, :] - 120
    d2 = p - 16 * m - 31
    idx[:, 256:503] = _t5_bucket_np(d2)
    msk[:, 256:503] = np.where(d2 < 0, NEG, 0.0)
    c['c_idx'] = idx
    c['c_mask'] = msk
    return c


def _prep_weights(inp):
    f = lambda a: np.ascontiguousarray(np.asarray(a, dtype=np.float32))
    w = {}
    w_in = f(inp['w_in'])[0]
    chs = _win_chunks()
    wr = np.zeros((len(chs), 128, 8, 128), np.float32)
    w3 = w_in.reshape(8, 128, 2840)
    for k, cols in enumerate(chs):
        cols = np.array(cols)
        ok = cols >= 0
        wr[k][:, :, ok] = np.transpose(w3[:, :, cols[ok]], (1, 0, 2))
    w['w_in_r'] = wr
    w_up = f(inp['w_up'])[0]
    w['w_up_r'] = np.ascontiguousarray(np.transpose(w_up.reshape(8, 128, 44, 128), (2, 1, 0, 3)))
    w['w_down_r'] = f(inp['w_down'])[0].reshape(22, 128, 1024)
    w['w_out_r'] = f(inp['w_out'])[0].reshape(8, 128, 1024)
    k1 = f(inp['cmp_k_w1'])[0].reshape(32, 64, 256).transpose(1, 0, 2)
    v1 = f(inp['cmp_v_w1'])[0].reshape(32, 64, 256).transpose(1, 0, 2)
    w['w1r'] = np.ascontiguousarray(np.concatenate([k1, v1], axis=0))
    k2 = f(inp['cmp_k_w2'])[0].reshape(2, 128, 64).transpose(1, 0, 2)
    w['w2k_r'] = np.ascontiguousarray(np.concatenate([k2, k2], axis=2))
    w['w2v_r'] = np.ascontiguousarray(f(inp['cmp_v_w2'])[0].reshape(2, 128, 64).transpose(1, 0, 2))
    w['posT'] = np.ascontiguousarray(np.concatenate([f(inp['cmp_pos_k'])[0].T, f(inp['cmp_pos_v'])[0].T], axis=0))
    w['norm1_w'] = f(inp['norm1_w']).reshape(1, 1024)
    w['norm2_w'] = f(inp['norm2_w']).reshape(1, 1024)
    w['final_w'] = f(inp['final_norm_w']).reshape(1, 1024)
    w['onorm_w'] = np.concatenate([f(inp['nsa_out_norm_w']).reshape(1, 512), f(inp['sb_out_norm_w']).reshape(1, 512)], axis=1)
    w['gate_b'] = f(inp['gate_b']).reshape(1, 24)
    w['rel_bias'] = f(inp['rel_bias']).reshape(1, 256)
    cw = f(inp['conv_w'])[0]
    cb = f(inp['conv_b'])[0]
    cp = np.stack([cw[0], cw[1], cw[2], cb], axis=1).reshape(44, 128, 4).transpose(1, 0, 2)
    w['convp'] = np.ascontiguousarray(cp)
    w.update(_host_consts())
    return w


def build_program(nseq):
    nc = bass.Bass("TRN2", target_bir_lowering=False)
    S = Sched()
    dr = {}

    def din(name, shape):
        dr[name] = nc.dram_tensor(name, list(shape), F32, kind="ExternalInput").ap()
        return dr[name]
    x_d = din('x', (nseq, T, D))
    win_d = din('w_in_r', (25, 128, 8, 128))
    wup_d = din('w_up_r', (44, 128, 8, 128))
    wdn_d = din('w_down_r', (22, 128, 1024))
    wout_d = din('w_out_r', (8, 128, 1024))
    w1_d = din('w1r', (128, 32, 256))
    w2k_d = din('w2k_r', (128, 2, 128))
    w2v_d = din('w2v_r', (128, 2, 64))
    posT_d = din('posT', (128, 32))
    n1_d = din('norm1_w', (1, 1024))
    n2_d = din('norm2_w', (1, 1024))
    nf_d = din('final_w', (1, 1024))
    on_d = din('onorm_w', (1, 1024))
    gb_d = din('gate_b', (1, 24))
    rb_d = din('rel_bias', (1, 256))
    cp_d = din('convp', (128, 44, 4))
    cid_d = din('c_ident', (128, 128))
    ctri_d = din('c_tri', (128, 128))
    cstr_d = din('c_strict', (128, 128))
    cwc_d = din('c_wincorr', (128, 128))
    csel_d = din('c_sele', (32, 2048))
    covl_d = din('c_ovl', (127, 32))
    cadd_d = din('c_addc', (128, 512))
    cidx_d = din('c_idx', (128, 503))
    cmsk_d = din('c_mask', (128, 503))
    out_d = nc.dram_tensor("out", [nseq, T, D], F32, kind="ExternalOutput").ap()

    st = ExitStack()
    NEL = 105000
    arena_t = st.enter_context(nc.sbuf_tensor("arena", [128, NEL], BF16))
    A = Arena(arena_t, NEL)
    PS = [st.enter_context(nc.psum_tensor("ps%d" % i, [128, 512], F32)) for i in range(8)]

    def psf(b):
        return PS[b][:]

    def psb(b):
        return PS[b][:].bitcast(BF16)

    def mm(out, lhsT, rhs, start, stop, r, w, skip=False):
        S.op('pe', lambda e, o=out, l=lhsT, rr=rhs, s0=start, s1=stop, sk=skip: e.matmul(o, lhsT=l, rhs=rr, start=s0, stop=s1, skip_group_check=sk), r=r, w=w)

    def tp(out, in_, idn, r, w):
        S.op('pe', lambda e, o=out, i=in_, d=idn: e.transpose(o, i, d), r=r, w=w)

    def act(out, in_, func, r, w, bias=None, scale=None, accum=None, eng='act'):
        kw = {}
        if bias is not None:
            kw['bias'] = bias
        if scale is not None:
            kw['scale'] = scale
        if accum is not None:
            kw['accum_out'] = accum
        S.op('act', lambda e, o=out, i=in_, f=func, k=kw: e.activation(out=o, in_=i, func=f, **k), r=r, w=w)

    def tt(eng, out, in0, in1, op, r, w):
        S.op(eng, lambda e, o=out, a=in0, b=in1, p=op: e.tensor_tensor(out=o, in0=a, in1=b, op=p), r=r, w=w)

    def ts(eng, out, in0, s1, s2, op0, op1, r, w):
        if s2 is None:
            S.op(eng, lambda e, o=out, a=in0, s=s1, p=op0: e.tensor_single_scalar(out=o, in_=a, scalar=s, op=p), r=r, w=w)
        else:
            S.op(eng, lambda e, o=out, a=in0, x1=s1, x2=s2, p0=op0, p1=op1: e.tensor_scalar(out=o, in0=a, scalar1=x1, scalar2=x2, op0=p0, op1=p1), r=r, w=w)

    def stt(eng, out, in0, scalar, in1, op0, op1, r, w):
        S.op(eng, lambda e, o=out, a=in0, s=scalar, b=in1, p0=op0, p1=op1: e.scalar_tensor_tensor(out=o, in0=a, scalar=s, in1=b, op0=p0, op1=p1), r=r, w=w)

    def cp(eng, out, in_, r, w):
        if eng == 'act':
            S.op('act', lambda e, o=out, i=in_: e.copy(out=o, in_=i), r=r, w=w)
        else:
            S.op(eng, lambda e, o=out, i=in_: e.tensor_copy(out=o, in_=i), r=r, w=w)


    def rsq(vec, scale, key):
        act(vec, vec, AF.Sqrt, [key], [key], bias=EPS, scale=scale)
        S.op('dve', lambda e, v=vec: e.reciprocal(out=v, in_=v), r=[key], w=[key])

    def memset(eng, ap, val, w):
        S.op(eng, lambda e, a=ap, v=val: e.memset(a, v), r=(), w=w)

    ident = A.alloc([128], BF16)
    tri = A.alloc([128], BF16)
    strict = A.alloc([128], BF16)
    wincorr = A.alloc([128], BF16)
    onescol = A.alloc([2], BF16)
    sele = A.alloc([16, 128], BF16)
    addc = A.alloc([16, 32], F32)
    corr = A.alloc([8, 256], BF16)
    bcm = A.alloc([8, 247], BF16)
    vcA = A.alloc([2, 97], BF16)
    b31c = A.alloc([8], F32)
    gateb = A.alloc([24], F32)
    convp = A.alloc([44, 4], F32)
    normw1 = A.alloc([1024], F32)
    normw2 = A.alloc([1024], F32)
    normwf = A.alloc([1024], F32)
    onormw = A.alloc([1024], F32)
    w2k = A.alloc([2, 128], BF16)
    w2v = A.alloc([2, 64], BF16)
    cbias = A.alloc([4], F32)
    posT = A.alloc([32], BF16)
    ws = [A.alloc([1024], BF16) for _ in range(NWS)]
    xbuf = [A.alloc([1024], F32) for _ in range(2)]
    ubuf = [A.alloc([1024], BF16) for _ in range(2)]
    junk = A.alloc([1024], BF16)
    ss = A.alloc([16], F32)
    rstd = A.alloc([16], F32)
    halo = A.alloc([44, 2], F32)
    m_common = A.mark()

    def ld(eng, dst, src, slot, wkey):
        S.dma(eng, dst, src, slot=slot, w=[wkey])
    ld('pool', ident, cid_d, 'c0', 'ident')
    ld('pool', tri, ctri_d, 'c1', 'tri')
    ld('pool', strict, cstr_d, 'c2', 'strict')
    ld('pool', wincorr, cwc_d, 'c3', 'wincorr')
    ld('pool', sele[0:32].rearrange("p a b -> p (a b)"), csel_d, 'c4', 'sele')
    ld('sp', addc.rearrange("p a b -> p (a b)"), cadd_d, 'c5', 'addc')
    ld('pool', vcA[0:127, 0, 65:97], covl_d, 'c6', 'vcA')
    ld('pool', vcA[0:127, 1, 65:97], covl_d, 'c7', 'vcA')
    ld('sp', gateb, gb_d[0:1, :].partition_broadcast(128), 'c8', 'gateb')
    ld('sp', convp.rearrange("p a b -> p (a b)"), cp_d.rearrange("p a b -> p (a b)"), 'c9', 'convp')
    ld('sp', normw1, n1_d[0:1, :].partition_broadcast(128), 'c10', 'normw1')
    ld('sp', normw2, n2_d[0:1, :].partition_broadcast(128), 'c11', 'normw2')
    ld('sp', normwf, nf_d[0:1, :].partition_broadcast(128), 'c12', 'normwf')
    ld('sp', onormw, on_d[0:1, :].partition_broadcast(128), 'c13', 'onormw')
    ld('pool', w2k.rearrange("p a b -> p (a b)"), w2k_d.rearrange("p a b -> p (a b)"), 'c14', 'w2k')
    ld('pool', w2v.rearrange("p a b -> p (a b)"), w2v_d.rearrange("p a b -> p (a b)"), 'c15', 'w2v')
    ld('pool', posT, posT_d, 'c16', 'posT')
    memset('dve', onescol, 1.0, ['onescol'])
    memset('dve', vcA[:, :, 64:65], 1.0, ['vcA'])
    memset('dve', junk, 0.0, ['junk'])

    m0 = A.mark()
    RB = A.alloc([32, 8], F32)
    idxt = A.alloc([503], F32)
    mskt = A.alloc([503], F32)
    accb = A.alloc([8, 503], F32)
    eqm = A.alloc([503], F32)
    ld('sp', RB.rearrange("p a b -> p (a b)"), rb_d[0:1, :].partition_broadcast(128), 'c17', 'RB')
    ld('sp', idxt, cidx_d, 'c18', 'idxt')
    ld('sp', mskt, cmsk_d, 'c19', 'mskt')
    cp('dve', b31c, RB[:, 31, :], ['RB'], ['b31c'])
    tt('dve', RB, RB, b31c.unsqueeze(1).to_broadcast([128, 32, 8]), ALU.subtract, ['RB', 'b31c'], ['RB'])
    memset('dve', accb, 0.0, ['accb'])
    for k in range(31):
        ts('dve', eqm, idxt, float(k), None, ALU.is_equal, None, ['idxt'], ['eqm'])
        for h in range(8):
            stt('dve', accb[:, h, :], eqm, RB[:, k, h:h + 1], accb[:, h, :], ALU.mult, ALU.add, ['eqm', 'RB', 'accb'], ['accb'])
    for h in range(8):
        tt('dve', corr[:, h, :], accb[:, h, 0:256], mskt[:, 0:256], ALU.add, ['accb', 'mskt'], ['corr'])
        tt('dve', bcm[:, h, :], accb[:, h, 256:503], mskt[:, 256:503], ALU.add, ['accb', 'mskt'], ['bcm'])
    S.barrier()
    A.release(m0)

    plan = []
    for b in range(nseq):
        for k in range(25):
            plan.append(win_d[k].rearrange("p a b -> p (a b)"))
        for blk in range(2):
            for j in range(22):
                plan.append(wup_d[j].rearrange("p a b -> p (a b)"))
                plan.append(wup_d[22 + j].rearrange("p a b -> p (a b)"))
    wstate = {'issued': 0, 'next': 0}

    def w_issue():
        k = wstate['issued']
        if k < len(plan):
            S.dma('pool', ws[k % NWS], plan[k], slot='ws%d' % (k % NWS), w=[('ws', k % NWS)])
            wstate['issued'] = k + 1

    def w_get():
        k = wstate['next']
        wstate['next'] = k + 1
        assert k < wstate['issued']
        return ws[k % NWS].rearrange("p (a b) -> p a b", a=8), ('ws', k % NWS)

    for _ in range(NWS):
        w_issue()


    for b in range(nseq):
        m_seq = A.mark()
        mixed = A.alloc([16, 1024], BF16)
        m_ab = A.mark()
        uT = A.alloc([8, 2048], BF16)
        qkv_m = A.mark()

        memset('dve', ss, 0.0, ['ss'])
        for i in range(16):
            xb = xbuf[i % 2]
            ub = ubuf[i % 2]
            S.dma('sp', xb, x_d[b, 128 * i:128 * i + 128, :], slot='x%d' % (i % 2), w=[('xbuf', i % 2)])
            act(junk, xb, AF.Square, [('xbuf', i % 2)], ['junk', 'ss'], accum=ss[:, i:i + 1])
            cp('dve', rstd[:, i:i + 1], ss[:, i:i + 1], ['ss'], ['rstd'])
            rsq(rstd[:, i:i + 1], 1.0 / D, 'rstd')
            stt('dve', ub, xb, rstd[:, i:i + 1], normw1, ALU.mult, ALU.mult, [('xbuf', i % 2), 'rstd', 'normw1'], [('ub', i % 2)])
            bk = i % 2
            for dc in range(8):
                tp(psb(bk)[:, dc * 128:(dc + 1) * 128], ub[:, dc * 128:(dc + 1) * 128], ident, [('ub', i % 2), 'ident'], [('ps', bk)])
            cp('act', uT[:, :, 128 * i:128 * i + 128], psb(bk).rearrange("p (a b) -> p a b", a=8), [('ps', bk)], ['uT'])

        def proj_F(dst_fn, scale, keyw):
            wch, wk = w_get()
            for Q in range(4):
                bk = 2 + (proj_F.n % 4)
                proj_F.n += 1
                for dc in range(8):
                    mm(psf(bk), wch[:, dc, :], uT[:, dc, 512 * Q:512 * Q + 512], dc == 0, dc == 7, [wk, 'uT'], [('ps', bk)])
                dst = dst_fn(Q)
                if proj_F.n % 2 == 0:
                    act(dst, psf(bk), AF.Copy, [('ps', bk)], [keyw], scale=scale)
                else:
                    ts('dve', dst, psf(bk), scale, None, ALU.mult, None, [('ps', bk)], [keyw])
            w_issue()
        proj_F.n = 0

        def proj_T(evac_fn, ncols=128):
            wch, wk = w_get()
            for tg in range(4):
                bk = 2 + (proj_F.n % 4)
                proj_F.n += 1
                for tl in range(4):
                    i = 4 * tg + tl
                    for dc in range(8):
                        mm(psf(bk)[:, tl * 128:tl * 128 + ncols], uT[:, dc, 128 * i:128 * i + 128], wch[:, dc, 0:ncols], dc == 0, dc == 7, [wk, 'uT'], [('ps', bk)])
                evac_fn(tg, bk)
            w_issue()

        qnT = A.alloc([4, 2048], BF16)
        kcvcT = A.alloc([2, 2048], BF16)
        ksT = A.alloc([2, 2048], BF16)
        kwT = A.alloc([2, 2048], BF16)
        vsA = A.alloc([16, 130], BF16)
        vwA = A.alloc([16, 130], BF16)
        gT = A.alloc([16, 24], F32)
        kcmpT = A.alloc([2, 127], BF16)
        work_m = A.mark()
        memset('dve', vsA, 1.0, ['vsA'])
        memset('dve', vwA, 1.0, ['vwA'])
        for c in range(4):
            proj_F(lambda Q, c=c: qnT[:, c, 512 * Q:512 * Q + 512], 0.125, 'qnT')
        for g in range(2):
            proj_F(lambda Q, g=g: kcvcT[:, g, 512 * Q:512 * Q + 512], 1.0, 'kcvcT')
        for g in range(2):
            proj_F(lambda Q, g=g: ksT[:, g, 512 * Q:512 * Q + 512], 1.0, 'ksT')
        for g in range(2):
            proj_F(lambda Q, g=g: kwT[:, g, 512 * Q:512 * Q + 512], 1.0, 'kwT')

        def evac_v(dstA, key):
            def f(tg, bk):
                src = psf(bk).rearrange("p (a g d) -> p a g d", a=4, g=2)
                dst = dstA[:, 4 * tg:4 * tg + 4, :].rearrange("p a (g e) -> p a g e", g=2)[:, :, :, 0:64]
                cp('dve', dst, src, [('ps', bk)], [key])
            return f
        proj_T(evac_v(vsA, 'vsA'))
        proj_T(evac_v(vwA, 'vwA'))

        def evac_g(tg, bk):
            src = psf(bk).rearrange("p (a c) -> p a c", a=4)[:, :, 0:24]
            dst = gT[:, 4 * tg:4 * tg + 4, :]
            tt('dve', dst, src, gateb.unsqueeze(1).to_broadcast([128, 4, 24]), ALU.add, [('ps', bk), 'gateb'], ['gT'])
            act(dst, dst, AF.Sigmoid, ['gT'], ['gT'])
        proj_T(evac_g, ncols=24)

        w1sb = A.alloc([32, 256], BF16)
        S.dma('pool', w1sb.rearrange("p a b -> p (a b)"), w1_d.rearrange("p a b -> p (a b)"), slot='w1', w=['w1sb'])
        geluT = A.alloc([2, 127], BF16)
        gx = A.alloc([127], F32)
        gt_ = A.alloc([127], F32)
        for kv in range(2):
            rows = slice(64 * kv, 64 * kv + 64)
            for hcc in range(2):
                bk = 0
                for i in range(32):
                    mm(psf(bk)[:, 0:1], w1sb[rows, i, hcc * 128:hcc * 128 + 128], posT[rows, i:i + 1], i == 0, i == 31, ['w1sb', 'posT'], [('ps', bk)])
                cp('dve', cbias[:, kv * 2 + hcc:kv * 2 + hcc + 1], psf(bk)[:, 0:1], [('ps', bk)], ['cbias'])
        for g in range(2):
            for kv in range(2):
                rows = slice(64 * kv, 64 * kv + 64)
                for hcc in range(2):
                    bk = hcc
                    for i in range(32):
                        mm(psf(bk)[:, 0:127], w1sb[rows, i, hcc * 128:hcc * 128 + 128], kcvcT[rows, g, i:i + 16 * 126 + 1:16], i == 0, i == 31, ['w1sb', 'kcvcT'], [('ps', bk)])
                    ts('dve', gx, psf(bk)[:, 0:127], cbias[:, kv * 2 + hcc:kv * 2 + hcc + 1], None, ALU.add, None, [('ps', bk), 'cbias'], ['gx'])
                    tt('dve', gt_, gx, gx, ALU.mult, ['gx'], ['gt'])
                    ts('dve', gt_, gt_, 0.044715, 1.0, ALU.mult, ALU.add, ['gt'], ['gt'])
                    tt('dve', gt_, gt_, gx, ALU.mult, ['gt', 'gx'], ['gt'])
                    act(gt_, gt_, AF.Sigmoid, ['gt'], ['gt'], scale=1.5957691216057308)
                    tt('dve', geluT[:, hcc, :], gx, gt_, ALU.mult, ['gx', 'gt'], ['geluT'])
                bk = 2
                if kv == 0:
                    for hcc in range(2):
                        mm(psf(bk)[:, 0:127], w2k[:, hcc, :], geluT[:, hcc, :], hcc == 0, hcc == 1, ['w2k', 'geluT'], [('ps', bk)])
                    cp('dve', kcmpT[:, g, :], psf(bk)[:, 0:127], [('ps', bk)], ['kcmpT'])
                else:
                    for hcc in range(2):
                        mm(psf(bk)[0:127, 0:64], geluT[:, hcc, :], w2v[:, hcc, :], hcc == 0, hcc == 1, ['w2v', 'geluT'], [('ps', bk)])
                    cp('dve', vcA[0:127, g, 0:64], psf(bk)[0:127, 0:64], [('ps', bk)], ['vcA'])
        S.barrier()
        A.release(work_m)
        kcmp_keep = kcmpT
        att_m = A.mark()

        Pc = A.alloc([127], BF16)
        PcT = A.alloc([128], BF16)
        PT = [A.alloc([512], BF16) for _ in range(3)]
        onsa = A.alloc([16, 64], F32)
        tmpo = A.alloc([4, 64], F32)
        impa = A.alloc([4, 32], F32)
        score = A.alloc([4, 32], F32)
        top8 = A.alloc([8], F32)
        thr = A.alloc([1], F32)
        negm = A.alloc([4, 32], BF16)
        negmT = A.alloc([512], BF16)
        rd = A.alloc([4], F32)
        sc = A.alloc([4], F32)
        sqt = A.alloc([16, 64], F32)
        ssh = A.alloc([16], F32)
        nrm = sqt
        ptn = [0]
        obn = [0]

        def finalize(ob, h, branch, Q, with_imp=False):
            O = psf(ob).rearrange("p (a c) -> p a c", a=4)
            r_ = h % 4
            ts('dve', rd, O[:, :, 64], 1e-30, None, ALU.max, None, [('ps', ob)], ['rd'])
            S.op('dve', lambda e: e.reciprocal(out=rd, in_=rd), r=['rd'], w=['rd'])
            tt('dve', sc, rd, gT[:, 4 * Q:4 * Q + 4, 3 * h + branch], ALU.mult, ['rd', 'gT'], ['sc'])
            tt('dve', tmpo, O[:, :, 0:64], sc.unsqueeze(2).to_broadcast([128, 4, 64]), ALU.mult, [('ps', ob), 'sc'], ['tmpo'])
            ov = onsa.rearrange("p (a r) d -> p a r d", a=4)[:, :, r_, :]
            tt('dve', ov, ov, tmpo, ALU.add, ['tmpo', 'onsa'], ['onsa'])
            if with_imp:
                tt('dve', score, O[:, :, 65:97], rd.unsqueeze(2).to_broadcast([128, 4, 32]), ALU.mult, [('ps', ob), 'rd'], ['score'])
                tt('dve', impa, impa, score, ALU.add, ['score', 'impa'], ['impa'])

        for Q in range(4):
            for g in range(2):
                memset('dve', onsa, 0.0, ['onsa'])
                memset('dve', impa, 0.0, ['impa'])
                for r_ in range(4):
                    h = 4 * g + r_
                    hf = slice(64 * (h % 2), 64 * (h % 2) + 64)
                    ob = 6 + (obn[0] % 2)
                    obn[0] += 1
                    O = psf(ob).rearrange("p (a c) -> p a c", a=4)
                    for tl in range(4):
                        i = 4 * Q + tl
                        bk = tl % 2
                        mm(psf(bk)[:, 0:127], qnT[hf, h // 2, 128 * i:128 * i + 128], kcmp_keep[hf, g, :], True, False, ['qnT', 'kcmpT'], [('ps', bk)])
                        mm(psf(bk)[:, 0:127], ident, bcm[:, h, 120 - 8 * i:120 - 8 * i + 127], False, True, ['ident', 'bcm'], [('ps', bk)])
                        act(Pc, psf(bk)[:, 0:127], AF.Exp, [('ps', bk), 'b31c'], ['Pc'], bias=b31c[:, h:h + 1])
                        tp(psb(2)[0:127, 0:128], Pc, ident, ['Pc', 'ident'], [('ps', 2)])
                        cp('dve', PcT[0:127, :], psb(2)[0:127, 0:128], [('ps', 2)], ['PcT'])
                        mm(O[:, tl, 0:97], PcT[0:127, :], vcA[0:127, g, :], True, True, ['PcT', 'vcA'], [('ps', ob)])
                    finalize(ob, h, 0, Q, with_imp=True)
                tt('dve', score, impa, addc[:, 4 * Q:4 * Q + 4, :], ALU.add, ['impa', 'addc'], ['score'])
                for tl in range(4):
                    S.op('dve', lambda e, tl=tl: e.max(out=top8, in_=score[:, tl, :]), r=['score'], w=['top8'])
                    ts('dve', thr, top8[:, 7:8], -5e29, None, ALU.max, None, ['top8'], ['thr'])
                    ts('dve', negm[:, tl, :], score[:, tl, :], thr[:, 0:1], NEG, ALU.is_lt, ALU.mult, ['score', 'thr'], ['negm'])
                    tp(psb(3)[0:32, tl * 128:tl * 128 + 128], negm[:, tl, :], ident, ['negm', 'ident'], [('ps', 3)])
                cp('dve', negmT[0:32, :], psb(3)[0:32, 0:512], [('ps', 3)], ['negmT'])
                for branch in (1, 2):
                    kT = ksT if branch == 1 else kwT
                    vA = vsA if branch == 1 else vwA
                    kkey = 'ksT' if branch == 1 else 'kwT'
                    vkey = 'vsA' if branch == 1 else 'vwA'
                    for r_ in range(4):
                        h = 4 * g + r_
                        hf = slice(64 * (h % 2), 64 * (h % 2) + 64)
                        ob = 6 + (obn[0] % 2)
                        obn[0] += 1
                        O = psf(ob).rearrange("p (a c) -> p a c", a=4)
                        c_lo = 0 if branch == 1 else max(0, 4 * Q - 4)
                        memset('dve', psf(ob), 0.0, [('ps', ob)])
                        for c in range(c_lo, 4 * Q + 4):
                            qt_lo = max(c, 4 * Q)
                            qt_hi = 4 * Q + 3 if branch == 1 else min(c + 4, 4 * Q + 3)
                            lo = 128 * (qt_lo - 4 * Q)
                            hi = 128 * (qt_hi - 4 * Q + 1)
                            bk = 3 + (ptn[0] % 3)
                            pt = PT[ptn[0] % 3]
                            pk = ('PT', ptn[0] % 3)
                            ptn[0] += 1
                            extra = []
                            if branch == 1:
                                extra.append(('m', lo, hi))
                            for qt in range(qt_lo, qt_hi + 1):
                                o_ = qt - c
                                cl = 128 * (qt - 4 * Q)
                                if o_ <= 1:
                                    extra.append(('c', cl, o_))
                                if branch == 2 and o_ == 4:
                                    extra.append(('w', cl, 0))
                            mm(psf(bk)[:, lo:hi], kT[hf, g, 128 * c:128 * c + 128], qnT[hf, h // 2, 512 * Q + lo:512 * Q + hi], True, len(extra) == 0, [kkey, 'qnT'], [('ps', bk)])
                            for k_, ex in enumerate(extra):
                                last = k_ == len(extra) - 1
                                if ex[0] == 'm':
                                    mm(psf(bk)[:, lo:hi], sele[0:32, c, :], negmT[0:32, lo:hi], False, last, ['sele', 'negmT'], [('ps', bk)])
                                elif ex[0] == 'c':
                                    mm(psf(bk)[:, ex[1]:ex[1] + 128], ident, corr[:, h, 128 * ex[2]:128 * ex[2] + 128], False, last, ['ident', 'corr'], [('ps', bk)])
                                else:
                                    mm(psf(bk)[:, ex[1]:ex[1] + 128], ident, wincorr, False, last, ['ident', 'wincorr'], [('ps', bk)])
                            act(pt[:, lo:hi], psf(bk)[:, lo:hi], AF.Exp, [('ps', bk), 'b31c'], [pk], bias=b31c[:, h:h + 1])
                            for qt in range(qt_lo, qt_hi + 1):
                                tl = qt - 4 * Q
                                first_c = 0 if branch == 1 else max(0, qt - 4)
                                mm(O[:, tl, 0:65], pt[:, 128 * tl:128 * tl + 128], vA[:, c, 65 * g:65 * g + 65], False, c == qt, [pk, vkey], [('ps', ob)], skip=True)
                        finalize(ob, h, branch, Q)
                tt('dve', sqt, onsa, onsa, ALU.mult, ['onsa'], ['sqt'])
                S.op('dve', lambda e: e.tensor_reduce(out=ssh, in_=sqt, axis=AX.X, op=ALU.add), r=['sqt'], w=['ssh'])
                rsq(ssh, 1.0 / 64, 'ssh')
                tt('dve', nrm, onsa, ssh.unsqueeze(2).to_broadcast([128, 16, 64]), ALU.mult, ['onsa', 'ssh', 'sqt'], ['sqt'])
                tt('dve', mixed[:, 4 * Q:4 * Q + 4, 256 * g:256 * g + 256], nrm.rearrange("p (a r) d -> p a (r d)", a=4),
                   onormw[:, 256 * g:256 * g + 256].unsqueeze(1).to_broadcast([128, 4, 256]), ALU.mult, ['sqt', 'onormw'], ['mixed'])

        S.barrier()
        A.release(qkv_m)

        qsT = A.alloc([4, 2048], BF16)
        ksbT = A.alloc([4, 2048], BF16)
        vS = A.alloc([16, 512], BF16)
        for c in range(4):
            proj_F(lambda Q, c=c: qsT[:, c, 512 * Q:512 * Q + 512], 0.125, 'qsT')
        for c in range(4):
            proj_F(lambda Q, c=c: ksbT[:, c, 512 * Q:512 * Q + 512], 1.0, 'ksbT')
        for c in range(4):
            def evac_vs(tg, bk, c=c):
                src = psf(bk).rearrange("p (a d) -> p a d", a=4)
                cp('dve', vS[:, 4 * tg:4 * tg + 4, 128 * c:128 * c + 128], src, [('ps', bk)], ['vS'])
            proj_T(evac_vs)

        Eb = [A.alloc([512], F32) for _ in range(2)]
        SPb = [A.alloc([512], BF16) for _ in range(2)]
        Xb = [A.alloc([512], F32) for _ in range(2)]
        Pb = [A.alloc([512], BF16) for _ in range(2)]
        osb = A.alloc([32, 64], F32)
        carry = A.alloc([4], F32)
        gsc = A.alloc([4], F32)
        tmps = A.alloc([4, 64], F32)
        sq2 = A.alloc([32, 64], F32)
        ss2h = A.alloc([32], F32)
        nrm2 = sq2
        it = [0]
        for Q in range(4):
            memset('dve', osb, 0.0, ['osb'])
            for h in range(8):
                hf = slice(64 * (h % 2), 64 * (h % 2) + 64)
                memset('dve', carry, 0.0, ['carry'])
                memset('dve', gsc, 1.0, ['gsc'])
                accv = osb.rearrange("p (a h) d -> p a h d", a=4)[:, :, h, :]
                for c in range(4 * Q + 3, -1, -1):
                    qt_lo = max(c, 4 * Q)
                    lo = 128 * (qt_lo - 4 * Q)
                    hi = 512
                    n_ = it[0]
                    it[0] += 1
                    zb = n_ % 2
                    cb_ = 2 + n_ % 2
                    rb_ = 4 + n_ % 2
                    E = Eb[n_ % 2]
                    SP = SPb[n_ % 2]
                    X = Xb[n_ % 2]
                    P = Pb[n_ % 2]
                    kE, kS, kX, kP = ('E', n_ % 2), ('SP', n_ % 2), ('X', n_ % 2), ('P', n_ % 2)
                    diag = c >= 4 * Q
                    mm(psf(zb)[:, lo:hi], ksbT[hf, h // 2, 128 * c:128 * c + 128], qsT[hf, h // 2, 512 * Q + lo:512 * Q + hi], True, not diag, ['ksbT', 'qsT'], [('ps', zb)])
                    if diag:
                        mm(psf(zb)[:, lo:lo + 128], ident, strict, False, True, ['ident', 'strict'], [('ps', zb)])
                    act(E[:, lo:hi], psf(zb)[:, lo:hi], AF.Exp, [('ps', zb)], [kE])
                    act(SP[:, lo:hi], E[:, lo:hi], AF.Ln, [kE], [kS], bias=1.0)
                    mm(psf(cb_)[:, lo:hi], tri, SP[:, lo:hi], True, True, ['tri', kS], [('ps', cb_)])
                    act(X[:, lo:hi], psf(cb_)[:, lo:hi], AF.Exp, [('ps', cb_)], [kX], scale=-1.0)
                    tt('pool', P[:, lo:hi], E[:, lo:hi], X[:, lo:hi], ALU.mult, [kE, kX], [kP])
                    R = psf(rb_).rearrange("p (a c) -> p a c", a=4)
                    tl0 = qt_lo - 4 * Q
                    for tl in range(tl0, 4):
                        mm(R[:, tl, 0:64], P[:, 128 * tl:128 * tl + 128], vS[:, c, 64 * h:64 * h + 64], True, True, [kP, 'vS'], [('ps', rb_)])
                        mm(R[:, tl, 64:65], SP[:, 128 * tl:128 * tl + 128], onescol[:, 0:1], True, True, [kS, 'onescol'], [('ps', rb_)])
                    tt('dve', tmps[:, tl0:4, :], R[:, tl0:4, 0:64], gsc[:, tl0:4].unsqueeze(2).to_broadcast([128, 4 - tl0, 64]), ALU.mult, [('ps', rb_), 'gsc'], ['tmps'])
                    tt('dve', accv[:, tl0:4, :], accv[:, tl0:4, :], tmps[:, tl0:4, :], ALU.add, ['tmps', 'osb'], ['osb'])
                    if c > 0:
                        tt('dve', carry[:, tl0:4], carry[:, tl0:4], R[:, tl0:4, 64], ALU.add, [('ps', rb_), 'carry'], ['carry'])
                        act(gsc[:, tl0:4], carry[:, tl0:4], AF.Exp, ['carry'], ['gsc'], scale=-1.0)
            tt('dve', sq2, osb, osb, ALU.mult, ['osb'], ['sq2'])
            S.op('dve', lambda e: e.tensor_reduce(out=ss2h, in_=sq2, axis=AX.X, op=ALU.add), r=['sq2'], w=['ss2h'])
            rsq(ss2h, 1.0 / 64, 'ss2h')
            tt('dve', nrm2, osb, ss2h.unsqueeze(2).to_broadcast([128, 32, 64]), ALU.mult, ['osb', 'ss2h', 'sq2'], ['sq2'])
            tt('dve', mixed[:, 4 * Q:4 * Q + 4, 512:1024], nrm2.rearrange("p (a h) d -> p a (h d)", a=4),
               onormw[:, 512:1024].unsqueeze(1).to_broadcast([128, 4, 512]), ALU.mult, ['sq2', 'onormw'], ['mixed'])

        S.barrier()
        A.release(m_ab)

        u2T = A.alloc([8, 2048], BF16)
        m_c1 = A.mark()
        wout = A.alloc([8, 1024], BF16)
        mT = A.alloc([8, 128], BF16)
        hbuf = A.alloc([1024], F32)
        S.dma('pool', wout, wout_d.rearrange("c p n -> p c n"), slot='wout', w=['wout'])
        memset('dve', ss, 0.0, ['ss'])
        for i in range(16):
            xb = xbuf[i % 2]
            ub = ubuf[i % 2]
            S.dma('sp', xb, x_d[b, 128 * i:128 * i + 128, :], slot='x%d' % (i % 2), w=[('xbuf', i % 2)])
            bk = i % 2
            for dc in range(8):
                tp(psb(bk)[:, dc * 128:(dc + 1) * 128], mixed[:, i, dc * 128:(dc + 1) * 128], ident, ['mixed', 'ident'], [('ps', bk)])
            cp('act', mT, psb(bk).rearrange("p (a b) -> p a b", a=8), [('ps', bk)], ['mT'])
            for half in range(2):
                pb = 2 + half
                for c in range(8):
                    mm(psf(pb), mT[:, c, :], wout[:, c, 512 * half:512 * half + 512], c == 0, c == 7, ['mT', 'wout'], [('ps', pb)])
                tt('dve', hbuf[:, 512 * half:512 * half + 512], psf(pb), xb[:, 512 * half:512 * half + 512], ALU.add, [('ps', pb), ('xbuf', i % 2)], ['hbuf'])
            S.dma('sp', out_d[b, 128 * i:128 * i + 128, :], hbuf, slot='hst', r=['hbuf'], w=[('outh', i)])
            act(junk, hbuf, AF.Square, ['hbuf'], ['junk', 'ss'], accum=ss[:, i:i + 1])
            cp('dve', rstd[:, i:i + 1], ss[:, i:i + 1], ['ss'], ['rstd'])
            rsq(rstd[:, i:i + 1], 1.0 / D, 'rstd')
            stt('dve', ub, hbuf, rstd[:, i:i + 1], normw2, ALU.mult, ALU.mult, ['hbuf', 'rstd', 'normw2'], [('ub', i % 2)])
            bk2 = 4 + i % 2
            for dc in range(8):
                tp(psb(bk2)[:, dc * 128:(dc + 1) * 128], ub[:, dc * 128:(dc + 1) * 128], ident, [('ub', i % 2), 'ident'], [('ps', bk2)])
            cp('act', u2T[:, :, 128 * i:128 * i + 128], psb(bk2).rearrange("p (a b) -> p a b", a=8), [('ps', bk2)], ['u2T'])
        S.barrier()
        A.release(m_seq)
        actT_lo = A.alloc([16, 1024], BF16)
        assert A.off == m_ab
        A.off = m_c1
        actT_hi = A.alloc([6, 1024], BF16)
        wdn = A.alloc([22, 1024], BF16)
        accg = [A.alloc([512], F32) for _ in range(2)]
        accv_ = [A.alloc([512], F32) for _ in range(2)]
        sil = [A.alloc([512], F32) for _ in range(2)]
        obuf = A.alloc([1024], F32)
        fx = A.alloc([8], F32)

        def actT(j):
            return actT_lo[:, j, :] if j < 16 else actT_hi[:, j - 16, :]

        memset('dve', halo, 0.0, ['halo'])
        for blk in range(2):
            S.dma('pool', wdn, wdn_d.rearrange("j p n -> p j n"), slot='wdn', w=['wdn'])
            for j in range(22):
                wg, wgk = w_get()
                wv, wvk = w_get()
                for tb in range(2):
                    n_ = (j * 2 + tb) % 2
                    col0 = 1024 * blk + 512 * tb
                    for which, wch, wk, pb, accs, fc in ((0, wg, wgk, 0 + n_, accg, j), (1, wv, wvk, 2 + n_, accv_, 22 + j)):
                        for dc in range(8):
                            mm(psf(pb), wch[:, dc, :], u2T[:, dc, col0:col0 + 512], dc == 0, dc == 7, [wk, 'u2T'], [('ps', pb)])
                        ac = accs[n_]
                        ak = ('acc', which, n_)
                        G = psf(pb)
                        S.op('act', lambda e, o=ac, i=G, fc=fc: e.activation(out=o, in_=i, func=AF.Identity, bias=convp[:, fc, 3:4], scale=convp[:, fc, 2:3]),
                             r=[('ps', pb), 'convp'], w=[ak])
                        stt('dve', ac[:, 1:512], G[:, 0:511], convp[:, fc, 1:2], ac[:, 1:512], ALU.mult, ALU.add, [('ps', pb), 'convp', ak], [ak])
                        stt('dve', ac[:, 2:512], G[:, 0:510], convp[:, fc, 0:1], ac[:, 2:512], ALU.mult, ALU.add, [('ps', pb), 'convp', ak], [ak])
                        stt('dve', ac[:, 0:1], halo[:, fc, 1:2], convp[:, fc, 1:2], ac[:, 0:1], ALU.mult, ALU.add, ['halo', 'convp', ak], [ak])
                        stt('dve', ac[:, 0:2], halo[:, fc, 0:2], convp[:, fc, 0:1], ac[:, 0:2], ALU.mult, ALU.add, ['halo', 'convp', ak], [ak])
                        cp('dve', halo[:, fc, :], G[:, 510:512], [('ps', pb)], ['halo'])
                    act(sil[n_], accg[n_], AF.Silu, [('acc', 0, n_)], [('sil', n_)])
                    tt('pool', actT(j)[:, 512 * tb:512 * tb + 512], sil[n_], accv_[n_], ALU.mult, [('sil', n_), ('acc', 1, n_)], ['actT'])
                w_issue()
                w_issue()
            memset('dve', ss, 0.0, ['ss'])
            for tl in range(8):
                i = 8 * blk + tl
                xb = xbuf[i % 2]
                S.dma('sp', xb, out_d[b, 128 * i:128 * i + 128, :], slot='x%d' % (i % 2), r=[('outh', i)], w=[('xbuf', i % 2)])
                for half in range(2):
                    pb = 4 + (2 * tl + half) % 4
                    for j in range(22):
                        mm(psf(pb), actT(j)[:, 128 * tl:128 * tl + 128], wdn[:, j, 512 * half:512 * half + 512], j == 0, j == 21, ['actT', 'wdn'], [('ps', pb)])
                    tt('dve', xb[:, 512 * half:512 * half + 512], psf(pb), xb[:, 512 * half:512 * half + 512], ALU.add, [('ps', pb), ('xbuf', i % 2)], [('xbuf', i % 2)])
                act(junk, xb, AF.Square, [('xbuf', i % 2)], ['junk', 'ss'], accum=ss[:, tl:tl + 1])
                cp('dve', fx[:, tl:tl + 1], ss[:, tl:tl + 1], ['ss'], ['fx'])
                rsq(fx[:, tl:tl + 1], 1.0 / D, 'fx')
                stt('dve', obuf, xb, fx[:, tl:tl + 1], normwf, ALU.mult, ALU.mult, [('xbuf', i % 2), 'fx', 'normwf'], ['obuf'])
                S.dma('sp', out_d[b, 128 * i:128 * i + 128, :], obuf, slot='ost', r=['obuf'], w=[('outf', i)])
        S.barrier()
        A.release(m_seq)

    S.barrier()
    S.emit(nc, st)
    st.close()
    return nc


_CACHE = {}


def kernel(**inputs):
    x = np.ascontiguousarray(np.asarray(inputs['x'], dtype=np.float32))
    B = x.shape[0]
    nseq = B // NCORE
    w = _prep_weights(inputs)
    if nseq not in _CACHE:
        _CACHE[nseq] = build_program(nseq)
    nc = _CACHE[nseq]
    in_maps = []
    for c in range(NCORE):
        m = dict(w)
        m['x'] = np.ascontiguousarray(x[c * nseq:(c + 1) * nseq])
        in_maps.append(m)
    res = run_bass_kernel_spmd(nc, in_maps, core_ids=list(range(NCORE)))
    out = np.concatenate([np.asarray(r['out'], dtype=np.float32) for r in res.results], axis=0)
    return out
```

```python
import numpy as np
from contextlib import ExitStack
import concourse.bass as bass
import concourse.mybir as mybir
from concourse.bass_utils import run_bass_kernel_spmd

F32 = mybir.dt.float32
BF16 = mybir.dt.bfloat16
AF = mybir.ActivationFunctionType
ALU = mybir.AluOpType
AX = mybir.AxisListType

T = 2048
D = 1024
DFF = 2816
NCORE = 8
NEG = -30000.0
EPS = 1e-6
ENGS = ['pe', 'act', 'dve', 'pool', 'sp']
CH = 4096
NWS = 6


class Sched:
    def __init__(self):
        self.prog = {e: [] for e in ENGS}
        self.cnt = {e: 0 for e in ENGS}
        self.seen = {e: {} for e in ENGS}
        self.lastw = {}
        self.readers = {}
        self.dcnt = {}
        self.targets = {e: set() for e in ENGS}

    def _collect(self, r, w):
        deps = set()
        for k in r:
            if k in self.lastw:
                deps.add(self.lastw[k])
        for k in w:
            if k in self.lastw:
                deps.add(self.lastw[k])
            deps.update(self.readers.get(k, ()))
        return deps

    def _waits(self, eng, deps):
        best = {}
        for (src, v) in deps:
            if src == eng and eng == 'pe':
                continue
            if self.seen[eng].get(src, -1) >= v:
                continue
            best[src] = max(best.get(src, -1), v)
        out = []
        for src, v in best.items():
            self.seen[eng][src] = v
            out.append((src, v))
            if src in self.targets:
                self.targets[src].add(v)
        return out

    def _record(self, opid, r, w):
        for k in r:
            self.readers.setdefault(k, set()).add(opid)
        for k in w:
            self.lastw[k] = opid
            self.readers[k] = set()

    def op(self, eng, fn, r=(), w=()):
        waits = self._waits(eng, self._collect(r, w))
        i = self.cnt[eng]
        self.cnt[eng] += 1
        self.prog[eng].append((waits, fn, (eng, i)))
        self._record((eng, i), r, w)

    def dma(self, eng, out, in_, slot, r=(), w=()):
        waits = self._waits(eng, self._collect(r, w))
        n = self.dcnt.get(slot, 0) + 1
        self.dcnt[slot] = n
        src = ('dma', slot)
        self.prog[eng].append((waits, (lambda e, o=out, i=in_: e.dma_start(out=o, in_=i)), (src, n)))
        self._record((src, n), r, w)

    def barrier(self):
        latest = []
        for e in ENGS:
            if self.cnt[e] > 0:
                latest.append((e, self.cnt[e] - 1))
        for slot, n in self.dcnt.items():
            latest.append((('dma', slot), n))
        for e in ENGS:
            waits = self._waits(e, latest)
            if waits:
                self.prog[e].append((waits, None, None))
        self.lastw.clear()
        self.readers.clear()

    def emit(self, nc, st):
        sems = {}

        def getsem(key):
            if key not in sems:
                sems[key] = st.enter_context(nc.semaphore("s%d" % len(sems)))
            return sems[key]
        rank = {}
        for e in ENGS:
            tl = sorted(self.targets[e])
            rank[e] = {v: k for k, v in enumerate(tl)}

        def wait_of(src, v):
            if isinstance(src, tuple):
                return getsem(src), 16 * v
            k = rank[src][v]
            return getsem((src, k // CH)), k % CH + 1
        plan = {}
        for e in ENGS:
            lst = []
            for waits, fn, opid in self.prog[e]:
                ws = [wait_of(s, v) for (s, v) in waits]
                inc = None
                if fn is not None:
                    src, v = opid
                    if isinstance(src, tuple):
                        inc = (getsem(src), 16)
                    elif v in rank[src]:
                        k = rank[src][v]
                        inc = (getsem((src, k // CH)), 1)
                lst.append((ws, fn, inc))
            plan[e] = lst

        def run(name, e):
            for ws, fn, inc in plan[name]:
                for (s, v) in ws:
                    e.wait_ge(s, v)
                if fn is not None:
                    ins = fn(e)
                    if inc is not None:
                        ins.then_inc(inc[0], inc[1])
        block = st.enter_context(nc.Block())

        @block.tensor
        def _(e):
            run('pe', e)

        @block.scalar
        def _(e):
            run('act', e)

        @block.vector
        def _(e):
            run('dve', e)

        @block.gpsimd
        def _(e):
            run('pool', e)

        @block.sync
        def _(e):
            run('sp', e)


class Arena:
    def __init__(self, t, nelem_bf16):
        self.t = t
        self.n = nelem_bf16
        self.off = 0
        self.peak = 0

    def mark(self):
        return self.off

    def release(self, m):
        self.off = m

    def alloc(self, shape, dtype, parts=128):
        sz = 4 if dtype == F32 else 2
        n = 1
        for s in shape:
            n *= s
        nb = (n * sz + 3) // 4 * 4
        start = self.off
        self.off += nb
        self.peak = max(self.peak, self.off)
        assert self.off <= self.n * 2, ("arena overflow", self.off)
        ap = self.t[:, start // 2:(start + n * sz) // 2]
        if dtype == F32:
            ap = ap.bitcast(F32)
        if len(shape) == 2:
            ap = ap.rearrange("p (a b) -> p a b", a=shape[0])
        elif len(shape) == 3:
            ap = ap.rearrange("p (a b c) -> p a b c", a=shape[0], b=shape[1])
        if parts != 128:
            ap = ap[0:parts]
        return ap


def _t5_bucket_np(dist):
    n = np.maximum(dist, 0)
    nf = np.maximum(n, 1).astype(np.float32)
    lb = 16 + (np.log(nf / 16.0) / np.log(8.0) * 16.0).astype(np.int32)
    lb = np.minimum(lb, 31)
    return np.where(n < 16, n, lb)


def _win_chunks():
    ch = []
    for c in range(4):
        ch.append(list(range(128 * c, 128 * c + 128)))
    for g in range(2):
        ch.append(list(range(512 + 64 * g, 576 + 64 * g)) + list(range(640 + 64 * g, 704 + 64 * g)))
    for g in range(2):
        ch.append(list(range(768 + 64 * g, 832 + 64 * g)) * 2)
    for g in range(2):
        ch.append(list(range(1024 + 64 * g, 1088 + 64 * g)) * 2)
    ch.append(list(range(896, 1024)))
    ch.append(list(range(1152, 1280)))
    ch.append(list(range(1280, 1304)) + [-1] * 104)
    for c in range(4):
        ch.append(list(range(1304 + 128 * c, 1304 + 128 * c + 128)))
    for c in range(4):
        ch.append(list(range(1816 + 128 * c, 1816 + 128 * c + 128)))
    for c in range(4):
        ch.append(list(range(2328 + 128 * c, 2328 + 128 * c + 128)))
    return ch


def _host_consts():
    c = {}
    c['c_ident'] = np.eye(128, dtype=np.float32)
    j = np.arange(128)[:, None]
    s = np.arange(128)[None, :]
    c['c_tri'] = (j >= s).astype(np.float32)
    c['c_strict'] = np.where(s <= j, NEG, 0.0).astype(np.float32)
    c['c_wincorr'] = np.where(s >= j, NEG, 0.0).astype(np.float32)
    sel = np.zeros((32, 16, 128), np.float32)
    for cc in range(16):
        for sp in range(128):
            sel[2 * cc + sp // 64, cc, sp] = 1.0
    c['c_sele'] = sel.reshape(32, 16 * 128)
    n = np.arange(127)[:, None]
    sj = np.arange(32)[None, :]
    ovl = ((16 * n < 64 * (sj + 1)) & (16 * n + 32 > 64 * sj)).astype(np.float32)
    c['c_ovl'] = ovl
    addc = np.zeros((128, 16, 32), np.float32)
    for i in range(16):
        t = 128 * i + np.arange(128)
        cur = (t // 64)[:, None]
        jj = np.arange(32)[None, :]
        forced = (jj == 0) | (jj == cur) | (jj == cur - 1)
        a = np.where(forced, 1e6, 0.0)
        a = np.where(jj <= cur, a, -1e30)
        addc[:, i, :] = a
    c['c_addc'] = addc.reshape(128, 512)
    idx = np.zeros((128, 503), np.float32)
    msk = np.zeros((128, 503), np.float32)
    p = np.arange(128)[:, None]
    jp = np.arange(256)[None, :]
    d1 = jp - p
    idx[:, 0:256] = _t5_bucket_np(d1)
    msk[:, 0:256] = np.where(d1 < 0, NEG, 0.0)
    m = np.arange(247)[None, :] - 120
    d2 = p - 16 * m - 31
    idx[:, 256:503] = _t5_bucket_np(d2)
    msk[:, 256:503] = np.where(d2 < 0, NEG, 0.0)
    c['c_idx'] = idx
    c['c_mask'] = msk
    return c


def _prep_weights(inp):
    f = lambda a: np.ascontiguousarray(np.asarray(a, dtype=np.float32))
    w = {}
    w_in = f(inp['w_in'])[0]
    chs = _win_chunks()
    wr = np.zeros((len(chs), 128, 8, 128), np.float32)
    w3 = w_in.reshape(8, 128, 2840)
    for k, cols in enumerate(chs):
        cols = np.array(cols)
        ok = cols >= 0
        wr[k][:, :, ok] = np.transpose(w3[:, :, cols[ok]], (1, 0, 2))
    w['w_in_r'] = wr
    w_up = f(inp['w_up'])[0]
    w['w_up_r'] = np.ascontiguousarray(np.transpose(w_up.reshape(8, 128, 44, 128), (2, 1, 0, 3)))
    w['w_down_r'] = f(inp['w_down'])[0].reshape(22, 128, 1024)
    w['w_out_r'] = f(inp['w_out'])[0].reshape(8, 128, 1024)
    k1 = f(inp['cmp_k_w1'])[0].reshape(32, 64, 256).transpose(1, 0, 2)
    v1 = f(inp['cmp_v_w1'])[0].reshape(32, 64, 256).transpose(1, 0, 2)
    w['w1r'] = np.ascontiguousarray(np.concatenate([k1, v1], axis=0))
    k2 = f(inp['cmp_k_w2'])[0].reshape(2, 128, 64).transpose(1, 0, 2)
    w['w2k_r'] = np.ascontiguousarray(np.concatenate([k2, k2], axis=2))
    w['w2v_r'] = np.ascontiguousarray(f(inp['cmp_v_w2'])[0].reshape(2, 128, 64).transpose(1, 0, 2))
    w['posT'] = np.ascontiguousarray(np.concatenate([f(inp['cmp_pos_k'])[0].T, f(inp['cmp_pos_v'])[0].T], axis=0))
    w['norm1_w'] = f(inp['norm1_w']).reshape(1, 1024)
    w['norm2_w'] = f(inp['norm2_w']).reshape(1, 1024)
    w['final_w'] = f(inp['final_norm_w']).reshape(1, 1024)
    w['onorm_w'] = np.concatenate([f(inp['nsa_out_norm_w']).reshape(1, 512), f(inp['sb_out_norm_w']).reshape(1, 512)], axis=1)
    w['gate_b'] = f(inp['gate_b']).reshape(1, 24)
    w['rel_bias'] = f(inp['rel_bias']).reshape(1, 256)
    cw = f(inp['conv_w'])[0]
    cb = f(inp['conv_b'])[0]
    cp = np.stack([cw[0], cw[1], cw[2], cb], axis=1).reshape(44, 128, 4).transpose(1, 0, 2)
    w['convp'] = np.ascontiguousarray(cp)
    w.update(_host_consts())
    return w


def build_program(nseq):
    nc = bass.Bass("TRN2", target_bir_lowering=False)
    S = Sched()
    dr = {}

    def din(name, shape):
        dr[name] = nc.dram_tensor(name, list(shape), F32, kind="ExternalInput").ap()
        return dr[name]
    x_d = din('x', (nseq, T, D))
    win_d = din('w_in_r', (25, 128, 8, 128))
    wup_d = din('w_up_r', (44, 128, 8, 128))
    wdn_d = din('w_down_r', (22, 128, 1024))
    wout_d = din('w_out_r', (8, 128, 1024))
    w1_d = din('w1r', (128, 32, 256))
    w2k_d = din('w2k_r', (128, 2, 128))
    w2v_d = din('w2v_r', (128, 2, 64))
    posT_d = din('posT', (128, 32))
    n1_d = din('norm1_w', (1, 1024))
    n2_d = din('norm2_w', (1, 1024))
    nf_d = din('final_w', (1, 1024))
    on_d = din('onorm_w', (1, 1024))
    gb_d = din('gate_b', (1, 24))
    rb_d = din('rel_bias', (1, 256))
    cp_d = din('convp', (128, 44, 4))
    cid_d = din('c_ident', (128, 128))
    ctri_d = din('c_tri', (128, 128))
    cstr_d = din('c_strict', (128, 128))
    cwc_d = din('c_wincorr', (128, 128))
    csel_d = din('c_sele', (32, 2048))
    covl_d = din('c_ovl', (127, 32))
    cadd_d = din('c_addc', (128, 512))
    cidx_d = din('c_idx', (128, 503))
    cmsk_d = din('c_mask', (128, 503))
    out_d = nc.dram_tensor("out", [nseq, T, D], F32, kind="ExternalOutput").ap()

    st = ExitStack()
    NEL = 105000
    arena_t = st.enter_context(nc.sbuf_tensor("arena", [128, NEL], BF16))
    A = Arena(arena_t, NEL)
    PS = [st.enter_context(nc.psum_tensor("ps%d" % i, [128, 512], F32)) for i in range(8)]

    def psf(b):
        return PS[b][:]

    def psb(b):
        return PS[b][:].bitcast(BF16)

    def mm(out, lhsT, rhs, start, stop, r, w, skip=False):
        S.op('pe', lambda e, o=out, l=lhsT, rr=rhs, s0=start, s1=stop, sk=skip: e.matmul(o, lhsT=l, rhs=rr, start=s0, stop=s1, skip_group_check=sk), r=r, w=w)

    def tp(out, in_, idn, r, w):
        S.op('pe', lambda e, o=out, i=in_, d=idn: e.transpose(o, i, d), r=r, w=w)

    def act(out, in_, func, r, w, bias=None, scale=None, accum=None, eng='act'):
        kw = {}
        if bias is not None:
            kw['bias'] = bias
        if scale is not None:
            kw['scale'] = scale
        if accum is not None:
            kw['accum_out'] = accum
        S.op('act', lambda e, o=out, i=in_, f=func, k=kw: e.activation(out=o, in_=i, func=f, **k), r=r, w=w)

    def tt(eng, out, in0, in1, op, r, w):
        S.op(eng, lambda e, o=out, a=in0, b=in1, p=op: e.tensor_tensor(out=o, in0=a, in1=b, op=p), r=r, w=w)

    def ts(eng, out, in0, s1, s2, op0, op1, r, w):
        if s2 is None:
            S.op(eng, lambda e, o=out, a=in0, s=s1, p=op0: e.tensor_single_scalar(out=o, in_=a, scalar=s, op=p), r=r, w=w)
        else:
            S.op(eng, lambda e, o=out, a=in0, x1=s1, x2=s2, p0=op0, p1=op1: e.tensor_scalar(out=o, in0=a, scalar1=x1, scalar2=x2, op0=p0, op1=p1), r=r, w=w)

    def stt(eng, out, in0, scalar, in1, op0, op1, r, w):
        S.op(eng, lambda e, o=out, a=in0, s=scalar, b=in1, p0=op0, p1=op1: e.scalar_tensor_tensor(out=o, in0=a, scalar=s, in1=b, op0=p0, op1=p1), r=r, w=w)

    def cp(eng, out, in_, r, w):
        if eng == 'act':
            S.op('act', lambda e, o=out, i=in_: e.copy(out=o, in_=i), r=r, w=w)
        else:
            S.op(eng, lambda e, o=out, i=in_: e.tensor_copy(out=o, in_=i), r=r, w=w)


    def rsq(vec, scale, key):
        act(vec, vec, AF.Sqrt, [key], [key], bias=EPS, scale=scale)
        S.op('dve', lambda e, v=vec: e.reciprocal(out=v, in_=v), r=[key], w=[key])

    def run_pipeline(items, lags):
        n = len(items)
        L = max(lags)
        for s_ in range(n + L):
            for j_, lag in enumerate(lags):
                k = s_ - lag
                if 0 <= k < n:
                    items[k][j_]()

    def memset(eng, ap, val, w):
        S.op(eng, lambda e, a=ap, v=val: e.memset(a, v), r=(), w=w)

    ident = A.alloc([128], BF16)
    tri = A.alloc([128], BF16)
    strict = A.alloc([128], BF16)
    wincorr = A.alloc([128], BF16)
    onescol = A.alloc([2], BF16)
    sele = A.alloc([16, 128], BF16)
    addc = A.alloc([16, 32], F32)
    corr = A.alloc([8, 256], BF16)
    bcm = A.alloc([8, 247], BF16)
    vcA = A.alloc([2, 97], BF16)
    b31c = A.alloc([8], F32)
    gateb = A.alloc([24], F32)
    convp = A.alloc([44, 4], F32)
    normw1 = A.alloc([1024], F32)
    normw2 = A.alloc([1024], F32)
    normwf = A.alloc([1024], F32)
    onormw = A.alloc([1024], F32)
    w2k = A.alloc([2, 128], BF16)
    w2v = A.alloc([2, 64], BF16)
    cbias = A.alloc([4], F32)
    posT = A.alloc([32], BF16)
    ws = [A.alloc([1024], BF16) for _ in range(NWS)]
    xbuf = [A.alloc([1024], F32) for _ in range(2)]
    ubuf = [A.alloc([1024], BF16) for _ in range(2)]
    junk = A.alloc([1024], BF16)
    ss = A.alloc([16], F32)
    rstd = A.alloc([16], F32)
    halo = A.alloc([44, 2], F32)
    m_common = A.mark()

    def ld(eng, dst, src, slot, wkey):
        S.dma(eng, dst, src, slot=slot, w=[wkey])
    ld('pool', ident, cid_d, 'c0', 'ident')
    ld('pool', tri, ctri_d, 'c1', 'tri')
    ld('pool', strict, cstr_d, 'c2', 'strict')
    ld('pool', wincorr, cwc_d, 'c3', 'wincorr')
    ld('pool', sele[0:32].rearrange("p a b -> p (a b)"), csel_d, 'c4', 'sele')
    ld('sp', addc.rearrange("p a b -> p (a b)"), cadd_d, 'c5', 'addc')
    ld('pool', vcA[0:127, 0, 65:97], covl_d, 'c6', 'vcA')
    ld('pool', vcA[0:127, 1, 65:97], covl_d, 'c7', 'vcA')
    ld('sp', gateb, gb_d[0:1, :].partition_broadcast(128), 'c8', 'gateb')
    ld('sp', convp.rearrange("p a b -> p (a b)"), cp_d.rearrange("p a b -> p (a b)"), 'c9', 'convp')
    ld('sp', normw1, n1_d[0:1, :].partition_broadcast(128), 'c10', 'normw1')
    ld('sp', normw2, n2_d[0:1, :].partition_broadcast(128), 'c11', 'normw2')
    ld('sp', normwf, nf_d[0:1, :].partition_broadcast(128), 'c12', 'normwf')
    ld('sp', onormw, on_d[0:1, :].partition_broadcast(128), 'c13', 'onormw')
    ld('pool', w2k.rearrange("p a b -> p (a b)"), w2k_d.rearrange("p a b -> p (a b)"), 'c14', 'w2k')
    ld('pool', w2v.rearrange("p a b -> p (a b)"), w2v_d.rearrange("p a b -> p (a b)"), 'c15', 'w2v')
    ld('pool', posT, posT_d, 'c16', 'posT')
    memset('dve', onescol, 1.0, ['onescol'])
    memset('dve', vcA[:, :, 64:65], 1.0, ['vcA'])
    memset('dve', junk, 0.0, ['junk'])

    m0 = A.mark()
    RB = A.alloc([32, 8], F32)
    idxt = A.alloc([503], F32)
    mskt = A.alloc([503], F32)
    accb = A.alloc([8, 503], F32)
    eqm = A.alloc([503], F32)
    ld('sp', RB.rearrange("p a b -> p (a b)"), rb_d[0:1, :].partition_broadcast(128), 'c17', 'RB')
    ld('sp', idxt, cidx_d, 'c18', 'idxt')
    ld('sp', mskt, cmsk_d, 'c19', 'mskt')
    cp('dve', b31c, RB[:, 31, :], ['RB'], ['b31c'])
    tt('dve', RB, RB, b31c.unsqueeze(1).to_broadcast([128, 32, 8]), ALU.subtract, ['RB', 'b31c'], ['RB'])
    memset('dve', accb, 0.0, ['accb'])
    for k in range(31):
        ts('dve', eqm, idxt, float(k), None, ALU.is_equal, None, ['idxt'], ['eqm'])
        for h in range(8):
            stt('dve', accb[:, h, :], eqm, RB[:, k, h:h + 1], accb[:, h, :], ALU.mult, ALU.add, ['eqm', 'RB', 'accb'], ['accb'])
    for h in range(8):
        tt('dve', corr[:, h, :], accb[:, h, 0:256], mskt[:, 0:256], ALU.add, ['accb', 'mskt'], ['corr'])
        tt('dve', bcm[:, h, :], accb[:, h, 256:503], mskt[:, 256:503], ALU.add, ['accb', 'mskt'], ['bcm'])
    S.barrier()
    A.release(m0)

    plan = []
    for b in range(nseq):
        for k in range(25):
            plan.append(win_d[k].rearrange("p a b -> p (a b)"))
        for blk in range(2):
            for j in range(22):
                plan.append(wup_d[j].rearrange("p a b -> p (a b)"))
                plan.append(wup_d[22 + j].rearrange("p a b -> p (a b)"))
    wstate = {'issued': 0, 'next': 0}

    def w_issue():
        k = wstate['issued']
        if k < len(plan):
            S.dma('pool', ws[k % NWS], plan[k], slot='ws%d' % (k % NWS), w=[('ws', k % NWS)])
            wstate['issued'] = k + 1

    def w_get():
        k = wstate['next']
        wstate['next'] = k + 1
        assert k < wstate['issued']
        return ws[k % NWS].rearrange("p (a b) -> p a b", a=8), ('ws', k % NWS)

    for _ in range(NWS):
        w_issue()


    for b in range(nseq):
        m_seq = A.mark()
        mixed = A.alloc([16, 1024], BF16)
        m_ab = A.mark()
        uT = A.alloc([8, 2048], BF16)
        qkv_m = A.mark()

        memset('dve', ss, 0.0, ['ss'])
        for i in range(16):
            xb = xbuf[i % 2]
            ub = ubuf[i % 2]
            S.dma('sp', xb, x_d[b, 128 * i:128 * i + 128, :], slot='x%d' % (i % 2), w=[('xbuf', i % 2)])
            act(junk, xb, AF.Square, [('xbuf', i % 2)], ['junk', 'ss'], accum=ss[:, i:i + 1])
            cp('dve', rstd[:, i:i + 1], ss[:, i:i + 1], ['ss'], ['rstd'])
            rsq(rstd[:, i:i + 1], 1.0 / D, 'rstd')
            stt('dve', ub, xb, rstd[:, i:i + 1], normw1, ALU.mult, ALU.mult, [('xbuf', i % 2), 'rstd', 'normw1'], [('ub', i % 2)])
            bk = i % 2
            for dc in range(8):
                tp(psb(bk)[:, dc * 128:(dc + 1) * 128], ub[:, dc * 128:(dc + 1) * 128], ident, [('ub', i % 2), 'ident'], [('ps', bk)])
            cp('act', uT[:, :, 128 * i:128 * i + 128], psb(bk).rearrange("p (a b) -> p a b", a=8), [('ps', bk)], ['uT'])

        def proj_F(dst_fn, scale, keyw):
            wch, wk = w_get()
            for Q in range(4):
                bk = 2 + (proj_F.n % 4)
                proj_F.n += 1
                for dc in range(8):
                    mm(psf(bk), wch[:, dc, :], uT[:, dc, 512 * Q:512 * Q + 512], dc == 0, dc == 7, [wk, 'uT'], [('ps', bk)])
                dst = dst_fn(Q)
                if proj_F.n % 2 == 0:
                    act(dst, psf(bk), AF.Copy, [('ps', bk)], [keyw], scale=scale)
                else:
                    ts('dve', dst, psf(bk), scale, None, ALU.mult, None, [('ps', bk)], [keyw])
            w_issue()
        proj_F.n = 0

        def proj_T(evac_fn, ncols=128):
            wch, wk = w_get()
            for tg in range(4):
                bk = 2 + (proj_F.n % 4)
                proj_F.n += 1
                for tl in range(4):
                    i = 4 * tg + tl
                    for dc in range(8):
                        mm(psf(bk)[:, tl * 128:tl * 128 + ncols], uT[:, dc, 128 * i:128 * i + 128], wch[:, dc, 0:ncols], dc == 0, dc == 7, [wk, 'uT'], [('ps', bk)])
                evac_fn(tg, bk)
            w_issue()

        qnT = A.alloc([4, 2048], BF16)
        kcvcT = A.alloc([2, 2048], BF16)
        ksT = A.alloc([2, 2048], BF16)
        kwT = A.alloc([2, 2048], BF16)
        vsA = A.alloc([16, 130], BF16)
        vwA = A.alloc([16, 130], BF16)
        gT = A.alloc([16, 24], F32)
        kcmpT = A.alloc([2, 127], BF16)
        work_m = A.mark()
        memset('dve', vsA, 1.0, ['vsA'])
        memset('dve', vwA, 1.0, ['vwA'])
        for c in range(4):
            proj_F(lambda Q, c=c: qnT[:, c, 512 * Q:512 * Q + 512], 0.125, 'qnT')
        for g in range(2):
            proj_F(lambda Q, g=g: kcvcT[:, g, 512 * Q:512 * Q + 512], 1.0, 'kcvcT')
        for g in range(2):
            proj_F(lambda Q, g=g: ksT[:, g, 512 * Q:512 * Q + 512], 1.0, 'ksT')
        for g in range(2):
            proj_F(lambda Q, g=g: kwT[:, g, 512 * Q:512 * Q + 512], 1.0, 'kwT')

        def evac_v(dstA, key):
            def f(tg, bk):
                src = psf(bk).rearrange("p (a g d) -> p a g d", a=4, g=2)
                dst = dstA[:, 4 * tg:4 * tg + 4, :].rearrange("p a (g e) -> p a g e", g=2)[:, :, :, 0:64]
                cp('dve', dst, src, [('ps', bk)], [key])
            return f
        proj_T(evac_v(vsA, 'vsA'))
        proj_T(evac_v(vwA, 'vwA'))

        def evac_g(tg, bk):
            src = psf(bk).rearrange("p (a c) -> p a c", a=4)[:, :, 0:24]
            dst = gT[:, 4 * tg:4 * tg + 4, :]
            tt('dve', dst, src, gateb.unsqueeze(1).to_broadcast([128, 4, 24]), ALU.add, [('ps', bk), 'gateb'], ['gT'])
            act(dst, dst, AF.Sigmoid, ['gT'], ['gT'])
        proj_T(evac_g, ncols=24)

        w1sb = A.alloc([32, 256], BF16)
        S.dma('pool', w1sb.rearrange("p a b -> p (a b)"), w1_d.rearrange("p a b -> p (a b)"), slot='w1', w=['w1sb'])
        geluT = A.alloc([2, 127], BF16)
        gx = A.alloc([127], F32)
        gt_ = A.alloc([127], F32)
        for kv in range(2):
            rows = slice(64 * kv, 64 * kv + 64)
            for hcc in range(2):
                bk = 0
                for i in range(32):
                    mm(psf(bk)[:, 0:1], w1sb[rows, i, hcc * 128:hcc * 128 + 128], posT[rows, i:i + 1], i == 0, i == 31, ['w1sb', 'posT'], [('ps', bk)])
                cp('dve', cbias[:, kv * 2 + hcc:kv * 2 + hcc + 1], psf(bk)[:, 0:1], [('ps', bk)], ['cbias'])
        for g in range(2):
            for kv in range(2):
                rows = slice(64 * kv, 64 * kv + 64)
                for hcc in range(2):
                    bk = hcc
                    for i in range(32):
                        mm(psf(bk)[:, 0:127], w1sb[rows, i, hcc * 128:hcc * 128 + 128], kcvcT[rows, g, i:i + 16 * 126 + 1:16], i == 0, i == 31, ['w1sb', 'kcvcT'], [('ps', bk)])
                    ts('dve', gx, psf(bk)[:, 0:127], cbias[:, kv * 2 + hcc:kv * 2 + hcc + 1], None, ALU.add, None, [('ps', bk), 'cbias'], ['gx'])
                    tt('dve', gt_, gx, gx, ALU.mult, ['gx'], ['gt'])
                    ts('dve', gt_, gt_, 0.044715, 1.0, ALU.mult, ALU.add, ['gt'], ['gt'])
                    tt('dve', gt_, gt_, gx, ALU.mult, ['gt', 'gx'], ['gt'])
                    act(gt_, gt_, AF.Sigmoid, ['gt'], ['gt'], scale=1.5957691216057308)
                    tt('dve', geluT[:, hcc, :], gx, gt_, ALU.mult, ['gx', 'gt'], ['geluT'])
                bk = 2
                if kv == 0:
                    for hcc in range(2):
                        mm(psf(bk)[:, 0:127], w2k[:, hcc, :], geluT[:, hcc, :], hcc == 0, hcc == 1, ['w2k', 'geluT'], [('ps', bk)])
                    cp('dve', kcmpT[:, g, :], psf(bk)[:, 0:127], [('ps', bk)], ['kcmpT'])
                else:
                    for hcc in range(2):
                        mm(psf(bk)[0:127, 0:64], geluT[:, hcc, :], w2v[:, hcc, :], hcc == 0, hcc == 1, ['w2v', 'geluT'], [('ps', bk)])
                    cp('dve', vcA[0:127, g, 0:64], psf(bk)[0:127, 0:64], [('ps', bk)], ['vcA'])
        S.barrier()
        A.release(work_m)
        kcmp_keep = kcmpT
        att_m = A.mark()

        Pc = [A.alloc([127], BF16) for _ in range(2)]
        PcT = [A.alloc([128], BF16) for _ in range(2)]
        PT = [A.alloc([512], BF16) for _ in range(3)]
        onsa2 = [A.alloc([16, 64], F32) for _ in range(2)]
        tmpo = A.alloc([4, 64], F32)
        impa2 = [A.alloc([4, 32], F32) for _ in range(2)]
        score = A.alloc([4, 32], F32)
        top8 = A.alloc([8], F32)
        thr = A.alloc([1], F32)
        negm = A.alloc([4, 32], BF16)
        negmT2 = [A.alloc([512], BF16) for _ in range(2)]
        rd = A.alloc([4], F32)
        sc = A.alloc([4], F32)
        sqt = A.alloc([16, 64], F32)
        ssh = A.alloc([16], F32)
        obn = [0]

        def finalize(ob, h, branch, Q, with_imp=False):
            g_ = h // 4
            onsa = onsa2[g_]
            impa = impa2[g_]
            O = psf(ob).rearrange("p (a c) -> p a c", a=4)
            r_ = h % 4
            ts('dve', rd, O[:, :, 64], 1e-30, None, ALU.max, None, [('ps', ob)], ['rd'])
            S.op('dve', lambda e: e.reciprocal(out=rd, in_=rd), r=['rd'], w=['rd'])
            tt('dve', sc, rd, gT[:, 4 * Q:4 * Q + 4, 3 * h + branch], ALU.mult, ['rd', 'gT'], ['sc'])
            tt('dve', tmpo, O[:, :, 0:64], sc.unsqueeze(2).to_broadcast([128, 4, 64]), ALU.mult, [('ps', ob), 'sc'], ['tmpo'])
            ov = onsa.rearrange("p (a r) d -> p a r d", a=4)[:, :, r_, :]
            tt('dve', ov, ov, tmpo, ALU.add, ['tmpo', ('onsa', g_)], [('onsa', g_)])
            if with_imp:
                tt('dve', score, O[:, :, 65:97], rd.unsqueeze(2).to_broadcast([128, 4, 32]), ALU.mult, [('ps', ob), 'rd'], ['score'])
                tt('dve', impa, impa, score, ALU.add, ['score', ('impa', g_)], [('impa', g_)])

        def headnorm_nsa(Q, g_):
            onsa = onsa2[g_]
            tt('dve', sqt, onsa, onsa, ALU.mult, [('onsa', g_)], ['sqt'])
            S.op('dve', lambda e: e.tensor_reduce(out=ssh, in_=sqt, axis=AX.X, op=ALU.add), r=['sqt'], w=['ssh'])
            rsq(ssh, 1.0 / 64, 'ssh')
            tt('dve', sqt, onsa, ssh.unsqueeze(2).to_broadcast([128, 16, 64]), ALU.mult, [('onsa', g_), 'ssh', 'sqt'], ['sqt'])
            tt('dve', mixed[:, 4 * Q:4 * Q + 4, 256 * g_:256 * g_ + 256], sqt.rearrange("p (a r) d -> p a (r d)", a=4),
               onormw[:, 256 * g_:256 * g_ + 256].unsqueeze(1).to_broadcast([128, 4, 256]), ALU.mult, ['sqt', 'onormw'], ['mixed'])

        for Q in range(4):
            for g in range(2):
                memset('dve', onsa2[g], 0.0, [('onsa', g)])
                memset('dve', impa2[g], 0.0, [('impa', g)])
            items = []
            for g in range(2):
                for r_ in range(4):
                    h = 4 * g + r_
                    ob = 6 + (obn[0] % 2)
                    obn[0] += 1
                    for tl in range(4):
                        k_ = len(items)
                        def mk(g=g, h=h, ob=ob, tl=tl, k_=k_, Q=Q):
                            hf = slice(64 * (h % 2), 64 * (h % 2) + 64)
                            i = 4 * Q + tl
                            bk = k_ % 2
                            pc = Pc[k_ % 2]
                            pct = PcT[k_ % 2]
                            tcol = 128 * (k_ % 2)
                            O = psf(ob).rearrange("p (a c) -> p a c", a=4)

                            def s1():
                                mm(psf(bk)[:, 0:127], qnT[hf, h // 2, 128 * i:128 * i + 128], kcmp_keep[hf, g, :], True, False, ['qnT', 'kcmpT'], [('ps', bk)])
                                mm(psf(bk)[:, 0:127], ident, bcm[:, h, 120 - 8 * i:120 - 8 * i + 127], False, True, ['ident', 'bcm'], [('ps', bk)])

                            def s2():
                                act(pc, psf(bk)[:, 0:127], AF.Exp, [('ps', bk), 'b31c'], [('Pc', k_ % 2)], bias=b31c[:, h:h + 1])

                            def s3():
                                tp(psb(2)[0:127, tcol:tcol + 128], pc, ident, [('Pc', k_ % 2), 'ident'], [('ps2', k_ % 2)])

                            def s4():
                                cp('dve', pct[0:127, :], psb(2)[0:127, tcol:tcol + 128], [('ps2', k_ % 2)], [('PcT', k_ % 2)])

                            def s5():
                                mm(O[:, tl, 0:97], pct[0:127, :], vcA[0:127, g, :], True, True, [('PcT', k_ % 2), 'vcA'], [('ps', ob)])
                                if tl == 3:
                                    finalize(ob, h, 0, Q, with_imp=True)
                            return [s1, s2, s3, s4, s5]
                        items.append(mk())
            run_pipeline(items, [0, 1, 2, 3, 4])
            for g in range(2):
                tt('dve', score, impa2[g], addc[:, 4 * Q:4 * Q + 4, :], ALU.add, [('impa', g), 'addc'], ['score'])
                for tl in range(4):
                    S.op('dve', lambda e, tl=tl: e.max(out=top8, in_=score[:, tl, :]), r=['score'], w=['top8'])
                    ts('dve', thr, top8[:, 7:8], -5e29, None, ALU.max, None, ['top8'], ['thr'])
                    ts('dve', negm[:, tl, :], score[:, tl, :], thr[:, 0:1], NEG, ALU.is_lt, ALU.mult, ['score', 'thr'], ['negm'])
                    tp(psb(2)[0:32, 256 + tl * 128:256 + tl * 128 + 128], negm[:, tl, :], ident, ['negm', 'ident'], [('ps2', 2)])
                cp('dve', negmT2[g][0:32, :], psb(2)[0:32, 256:768], [('ps2', 2)], [('negmT', g)])
            items = []
            for g in range(2):
                for branch in (2, 1):
                    kT = ksT if branch == 1 else kwT
                    vA = vsA if branch == 1 else vwA
                    kkey = 'ksT' if branch == 1 else 'kwT'
                    vkey = 'vsA' if branch == 1 else 'vwA'
                    for r_ in range(4):
                        h = 4 * g + r_
                        ob = 6 + (obn[0] % 2)
                        obn[0] += 1
                        c_lo = 0 if branch == 1 else max(0, 4 * Q - 4)
                        for c in range(c_lo, 4 * Q + 4):
                            k_ = len(items)
                            last_item = (branch == 1 and r_ == 3 and c == 4 * Q + 3)
                            def mk(g=g, branch=branch, kT=kT, vA=vA, kkey=kkey, vkey=vkey, h=h, ob=ob, c=c, c_lo=c_lo, k_=k_, Q=Q, last_item=last_item):
                                hf = slice(64 * (h % 2), 64 * (h % 2) + 64)
                                O = psf(ob).rearrange("p (a c) -> p a c", a=4)
                                qt_lo = max(c, 4 * Q)
                                qt_hi = 4 * Q + 3 if branch == 1 else min(c + 4, 4 * Q + 3)
                                lo = 128 * (qt_lo - 4 * Q)
                                hi = 128 * (qt_hi - 4 * Q + 1)
                                bk = 3 + (k_ % 3)
                                pt = PT[k_ % 3]
                                pk = ('PT', k_ % 3)

                                def s1():
                                    extra = []
                                    if branch == 1:
                                        extra.append(('m', lo, hi))
                                    for qt in range(qt_lo, qt_hi + 1):
                                        o_ = qt - c
                                        cl = 128 * (qt - 4 * Q)
                                        if o_ <= 1:
                                            extra.append(('c', cl, o_))
                                        if branch == 2 and o_ == 4:
                                            extra.append(('w', cl, 0))
                                    mm(psf(bk)[:, lo:hi], kT[hf, g, 128 * c:128 * c + 128], qnT[hf, h // 2, 512 * Q + lo:512 * Q + hi], True, len(extra) == 0, [kkey, 'qnT'], [('ps', bk)])
                                    for j_, ex in enumerate(extra):
                                        last = j_ == len(extra) - 1
                                        if ex[0] == 'm':
                                            mm(psf(bk)[:, lo:hi], sele[0:32, c, :], negmT2[g][0:32, lo:hi], False, last, ['sele', ('negmT', g)], [('ps', bk)])
                                        elif ex[0] == 'c':
                                            mm(psf(bk)[:, ex[1]:ex[1] + 128], ident, corr[:, h, 128 * ex[2]:128 * ex[2] + 128], False, last, ['ident', 'corr'], [('ps', bk)])
                                        else:
                                            mm(psf(bk)[:, ex[1]:ex[1] + 128], ident, wincorr, False, last, ['ident', 'wincorr'], [('ps', bk)])

                                def s2():
                                    act(pt[:, lo:hi], psf(bk)[:, lo:hi], AF.Exp, [('ps', bk), 'b31c'], [pk], bias=b31c[:, h:h + 1])

                                def s3():
                                    if c == c_lo:
                                        memset('dve', psf(ob), 0.0, [('ps', ob)])
                                    for qt in range(qt_lo, qt_hi + 1):
                                        tl = qt - 4 * Q
                                        mm(O[:, tl, 0:65], pt[:, 128 * tl:128 * tl + 128], vA[:, c, 65 * g:65 * g + 65], False, c == qt, [pk, vkey], [('ps', ob)], skip=True)
                                    if c == 4 * Q + 3:
                                        finalize(ob, h, branch, Q)
                                    if last_item:
                                        headnorm_nsa(Q, g)
                                return [s1, s2, s3]
                            items.append(mk())
            run_pipeline(items, [0, 1, 2])


        S.barrier()
        A.release(qkv_m)

        qsT = A.alloc([4, 2048], BF16)
        ksbT = A.alloc([4, 2048], BF16)
        vS = A.alloc([16, 512], BF16)
        for c in range(4):
            proj_F(lambda Q, c=c: qsT[:, c, 512 * Q:512 * Q + 512], 0.125, 'qsT')
        for c in range(4):
            proj_F(lambda Q, c=c: ksbT[:, c, 512 * Q:512 * Q + 512], 1.0, 'ksbT')
        for c in range(4):
            def evac_vs(tg, bk, c=c):
                src = psf(bk).rearrange("p (a d) -> p a d", a=4)
                cp('dve', vS[:, 4 * tg:4 * tg + 4, 128 * c:128 * c + 128], src, [('ps', bk)], ['vS'])
            proj_T(evac_vs)

        Eb = [A.alloc([512], F32) for _ in range(3)]
        SPb = [A.alloc([512], BF16) for _ in range(4)]
        Xb = [A.alloc([512], F32) for _ in range(2)]
        Pb = [A.alloc([512], BF16) for _ in range(2)]
        osb = A.alloc([32, 64], F32)
        carry = A.alloc([4], F32)
        gsc = A.alloc([4], F32)
        tmps = A.alloc([4, 64], F32)
        sq2 = A.alloc([32, 64], F32)
        ss2h = A.alloc([32], F32)
        items = []
        for Q in range(4):
            for h in range(8):
                for c in range(4 * Q + 3, -1, -1):
                    k_ = len(items)
                    def mk(Q=Q, h=h, c=c, k_=k_):
                        hf = slice(64 * (h % 2), 64 * (h % 2) + 64)
                        accv = osb.rearrange("p (a h) d -> p a h d", a=4)[:, :, h, :]
                        qt_lo = max(c, 4 * Q)
                        lo = 128 * (qt_lo - 4 * Q)
                        hi = 512
                        zb = k_ % 2
                        cb_ = 2 + k_ % 2
                        rb_ = 4 + k_ % 2
                        E = Eb[k_ % 3]
                        SP = SPb[k_ % 4]
                        X = Xb[k_ % 2]
                        P = Pb[k_ % 2]
                        kE, kS, kX, kP = ('E', k_ % 3), ('SP', k_ % 4), ('X', k_ % 2), ('P', k_ % 2)
                        diag = c >= 4 * Q
                        R = psf(rb_).rearrange("p (a c) -> p a c", a=4)
                        tl0 = qt_lo - 4 * Q
                        first = (c == 4 * Q + 3)

                        def s_g():
                            if not first:
                                act(gsc, carry, AF.Exp, ['carry'], ['gsc'], scale=-1.0)

                        def s1():
                            mm(psf(zb)[:, lo:hi], ksbT[hf, h // 2, 128 * c:128 * c + 128], qsT[hf, h // 2, 512 * Q + lo:512 * Q + hi], True, not diag, ['ksbT', 'qsT'], [('ps', zb)])
                            if diag:
                                mm(psf(zb)[:, lo:lo + 128], ident, strict, False, True, ['ident', 'strict'], [('ps', zb)])

                        def s2():
                            act(E[:, lo:hi], psf(zb)[:, lo:hi], AF.Exp, [('ps', zb)], [kE])
                            act(SP[:, lo:hi], E[:, lo:hi], AF.Ln, [kE], [kS], bias=1.0)

                        def s3():
                            mm(psf(cb_)[:, lo:hi], tri, SP[:, lo:hi], True, True, ['tri', kS], [('ps', cb_)])

                        def s4():
                            act(X[:, lo:hi], psf(cb_)[:, lo:hi], AF.Exp, [('ps', cb_)], [kX], scale=-1.0)

                        def s5():
                            tt('pool', P[:, lo:hi], E[:, lo:hi], X[:, lo:hi], ALU.mult, [kE, kX], [kP])

                        def s6():
                            for tl in range(tl0, 4):
                                mm(R[:, tl, 0:64], P[:, 128 * tl:128 * tl + 128], vS[:, c, 64 * h:64 * h + 64], True, True, [kP, 'vS'], [('ps', rb_)])
                                mm(R[:, tl, 64:65], SP[:, 128 * tl:128 * tl + 128], onescol[:, 0:1], True, True, [kS, 'onescol'], [('ps', rb_)])

                        def s7():
                            if first and h == 0:
                                memset('dve', osb, 0.0, ['osb'])
                            if first:
                                memset('dve', carry, 0.0, ['carry'])
                                memset('dve', gsc, 1.0, ['gsc'])
                            tt('dve', tmps[:, tl0:4, :], R[:, tl0:4, 0:64], gsc[:, tl0:4].unsqueeze(2).to_broadcast([128, 4 - tl0, 64]), ALU.mult, [('ps', rb_), 'gsc'], ['tmps'])
                            tt('dve', accv[:, tl0:4, :], accv[:, tl0:4, :], tmps[:, tl0:4, :], ALU.add, ['tmps', 'osb'], ['osb'])
                            if c > 0:
                                tt('dve', carry[:, tl0:4], carry[:, tl0:4], R[:, tl0:4, 64], ALU.add, [('ps', rb_), 'carry'], ['carry'])
                            if c == 0 and h == 7:
                                tt('dve', sq2, osb, osb, ALU.mult, ['osb'], ['sq2'])
                                S.op('dve', lambda e: e.tensor_reduce(out=ss2h, in_=sq2, axis=AX.X, op=ALU.add), r=['sq2'], w=['ss2h'])
                                rsq(ss2h, 1.0 / 64, 'ss2h')
                                tt('dve', sq2, osb, ss2h.unsqueeze(2).to_broadcast([128, 32, 64]), ALU.mult, ['osb', 'ss2h', 'sq2'], ['sq2'])
                                tt('dve', mixed[:, 4 * Q:4 * Q + 4, 512:1024], sq2.rearrange("p (a h) d -> p a (h d)", a=4),
                                   onormw[:, 512:1024].unsqueeze(1).to_broadcast([128, 4, 512]), ALU.mult, ['sq2', 'onormw'], ['mixed'])
                        return [s_g, s1, s2, s3, s4, s5, s6, s7]
                    items.append(mk())
        run_pipeline(items, [4, 0, 1, 2, 3, 3, 4, 4])


        S.barrier()
        A.release(m_ab)

        u2T = A.alloc([8, 2048], BF16)
        m_c1 = A.mark()
        wout = A.alloc([8, 1024], BF16)
        mT = A.alloc([8, 128], BF16)
        hbuf = A.alloc([1024], F32)
        S.dma('pool', wout, wout_d.rearrange("c p n -> p c n"), slot='wout', w=['wout'])
        memset('dve', ss, 0.0, ['ss'])
        for i in range(16):
            xb = xbuf[i % 2]
            ub = ubuf[i % 2]
            S.dma('sp', xb, x_d[b, 128 * i:128 * i + 128, :], slot='x%d' % (i % 2), w=[('xbuf', i % 2)])
            bk = i % 2
            for dc in range(8):
                tp(psb(bk)[:, dc * 128:(dc + 1) * 128], mixed[:, i, dc * 128:(dc + 1) * 128], ident, ['mixed', 'ident'], [('ps', bk)])
            cp('act', mT, psb(bk).rearrange("p (a b) -> p a b", a=8), [('ps', bk)], ['mT'])
            for half in range(2):
                pb = 2 + half
                for c in range(8):
                    mm(psf(pb), mT[:, c, :], wout[:, c, 512 * half:512 * half + 512], c == 0, c == 7, ['mT', 'wout'], [('ps', pb)])
                tt('dve', hbuf[:, 512 * half:512 * half + 512], psf(pb), xb[:, 512 * half:512 * half + 512], ALU.add, [('ps', pb), ('xbuf', i % 2)], ['hbuf'])
            S.dma('sp', out_d[b, 128 * i:128 * i + 128, :], hbuf, slot='hst', r=['hbuf'], w=[('outh', i)])
            act(junk, hbuf, AF.Square, ['hbuf'], ['junk', 'ss'], accum=ss[:, i:i + 1])
            cp('dve', rstd[:, i:i + 1], ss[:, i:i + 1], ['ss'], ['rstd'])
            rsq(rstd[:, i:i + 1], 1.0 / D, 'rstd')
            stt('dve', ub, hbuf, rstd[:, i:i + 1], normw2, ALU.mult, ALU.mult, ['hbuf', 'rstd', 'normw2'], [('ub', i % 2)])
            bk2 = 4 + i % 2
            for dc in range(8):
                tp(psb(bk2)[:, dc * 128:(dc + 1) * 128], ub[:, dc * 128:(dc + 1) * 128], ident, [('ub', i % 2), 'ident'], [('ps', bk2)])
            cp('act', u2T[:, :, 128 * i:128 * i + 128], psb(bk2).rearrange("p (a b) -> p a b", a=8), [('ps', bk2)], ['u2T'])
        S.barrier()
        A.release(m_seq)
        actT_lo = A.alloc([16, 1024], BF16)
        assert A.off == m_ab
        A.off = m_c1
        actT_hi = A.alloc([6, 1024], BF16)
        wdn = A.alloc([22, 1024], BF16)
        accg = [A.alloc([512], F32) for _ in range(2)]
        accv_ = [A.alloc([512], F32) for _ in range(2)]
        sil = [A.alloc([512], F32) for _ in range(2)]
        obuf = A.alloc([1024], F32)
        fx = A.alloc([8], F32)

        def actT(j):
            return actT_lo[:, j, :] if j < 16 else actT_hi[:, j - 16, :]

        memset('dve', halo, 0.0, ['halo'])
        for blk in range(2):
            S.dma('pool', wdn, wdn_d.rearrange("j p n -> p j n"), slot='wdn', w=['wdn'])
            for j in range(22):
                wg, wgk = w_get()
                wv, wvk = w_get()
                for tb in range(2):
                    n_ = (j * 2 + tb) % 2
                    col0 = 1024 * blk + 512 * tb
                    for which, wch, wk, pb, accs, fc in ((0, wg, wgk, 0 + n_, accg, j), (1, wv, wvk, 2 + n_, accv_, 22 + j)):
                        for dc in range(8):
                            mm(psf(pb), wch[:, dc, :], u2T[:, dc, col0:col0 + 512], dc == 0, dc == 7, [wk, 'u2T'], [('ps', pb)])
                        ac = accs[n_]
                        ak = ('acc', which, n_)
                        G = psf(pb)
                        S.op('act', lambda e, o=ac, i=G, fc=fc: e.activation(out=o, in_=i, func=AF.Identity, bias=convp[:, fc, 3:4], scale=convp[:, fc, 2:3]),
                             r=[('ps', pb), 'convp'], w=[ak])
                        stt('dve', ac[:, 1:512], G[:, 0:511], convp[:, fc, 1:2], ac[:, 1:512], ALU.mult, ALU.add, [('ps', pb), 'convp', ak], [ak])
                        stt('dve', ac[:, 2:512], G[:, 0:510], convp[:, fc, 0:1], ac[:, 2:512], ALU.mult, ALU.add, [('ps', pb), 'convp', ak], [ak])
                        stt('dve', ac[:, 0:1], halo[:, fc, 1:2], convp[:, fc, 1:2], ac[:, 0:1], ALU.mult, ALU.add, ['halo', 'convp', ak], [ak])
                        stt('dve', ac[:, 0:2], halo[:, fc, 0:2], convp[:, fc, 0:1], ac[:, 0:2], ALU.mult, ALU.add, ['halo', 'convp', ak], [ak])
                        cp('dve', halo[:, fc, :], G[:, 510:512], [('ps', pb)], ['halo'])
                    act(sil[n_], accg[n_], AF.Silu, [('acc', 0, n_)], [('sil', n_)])
                    tt('pool', actT(j)[:, 512 * tb:512 * tb + 512], sil[n_], accv_[n_], ALU.mult, [('sil', n_), ('acc', 1, n_)], ['actT'])
                w_issue()
                w_issue()
            memset('dve', ss, 0.0, ['ss'])
            for tl in range(8):
                i = 8 * blk + tl
                xb = xbuf[i % 2]
                S.dma('sp', xb, out_d[b, 128 * i:128 * i + 128, :], slot='x%d' % (i % 2), r=[('outh', i)], w=[('xbuf', i % 2)])
                for half in range(2):
                    pb = 4 + (2 * tl + half) % 4
                    for j in range(22):
                        mm(psf(pb), actT(j)[:, 128 * tl:128 * tl + 128], wdn[:, j, 512 * half:512 * half + 512], j == 0, j == 21, ['actT', 'wdn'], [('ps', pb)])
                    tt('dve', xb[:, 512 * half:512 * half + 512], psf(pb), xb[:, 512 * half:512 * half + 512], ALU.add, [('ps', pb), ('xbuf', i % 2)], [('xbuf', i % 2)])
                act(junk, xb, AF.Square, [('xbuf', i % 2)], ['junk', 'ss'], accum=ss[:, tl:tl + 1])
                cp('dve', fx[:, tl:tl + 1], ss[:, tl:tl + 1], ['ss'], ['fx'])
                rsq(fx[:, tl:tl + 1], 1.0 / D, 'fx')
                stt('dve', obuf, xb, fx[:, tl:tl + 1], normwf, ALU.mult, ALU.mult, [('xbuf', i % 2), 'fx', 'normwf'], ['obuf'])
                S.dma('sp', out_d[b, 128 * i:128 * i + 128, :], obuf, slot='ost', r=['obuf'], w=[('outf', i)])
        S.barrier()
        A.release(m_seq)

    S.barrier()
    build_program.info = {'peak': A.peak, 'ops': dict(S.cnt)}
    S.emit(nc, st)
    st.close()
    return nc


_CACHE = {}


def kernel(**inputs):
    x = np.ascontiguousarray(np.asarray(inputs['x'], dtype=np.float32))
    B = x.shape[0]
    nseq = B // NCORE
    w = _prep_weights(inputs)
    if nseq not in _CACHE:
        _CACHE[nseq] = build_program(nseq)
    nc = _CACHE[nseq]
    in_maps = []
    for c in range(NCORE):
        m = dict(w)
        m['x'] = np.ascontiguousarray(x[c * nseq:(c + 1) * nseq])
        in_maps.append(m)
    res = run_bass_kernel_spmd(nc, in_maps, core_ids=list(range(NCORE)))
    out = np.concatenate([np.asarray(r['out'], dtype=np.float32) for r in res.results], axis=0)
    return out
```

```python
import numpy as np
from contextlib import ExitStack
import concourse.bass as bass
import concourse.mybir as mybir
from concourse.bass_utils import run_bass_kernel_spmd

F32 = mybir.dt.float32
BF16 = mybir.dt.bfloat16
AF = mybir.ActivationFunctionType
ALU = mybir.AluOpType
AX = mybir.AxisListType

T = 2048
D = 1024
DFF = 2816
NCORE = 8
NEG = -30000.0
EPS = 1e-6
ENGS = ['pe', 'act', 'dve', 'pool', 'sp']
CH = 4096
NWS = 6


class Sched:
    def __init__(self):
        self.prog = {e: [] for e in ENGS}
        self.cnt = {e: 0 for e in ENGS}
        self.seen = {e: {} for e in ENGS}
        self.lastw = {}
        self.readers = {}
        self.dcnt = {}
        self.targets = {e: set() for e in ENGS}

    def _collect(self, r, w):
        deps = set()
        for k in r:
            if k in self.lastw:
                deps.add(self.lastw[k])
        for k in w:
            if k in self.lastw:
                deps.add(self.lastw[k])
            deps.update(self.readers.get(k, ()))
        return deps

    def _waits(self, eng, deps):
        best = {}
        for (src, v) in deps:
            if src == eng and eng == 'pe':
                continue
            if self.seen[eng].get(src, -1) >= v:
                continue
            best[src] = max(best.get(src, -1), v)
        out = []
        for src, v in best.items():
            self.seen[eng][src] = v
            out.append((src, v))
            if src in self.targets:
                self.targets[src].add(v)
        return out

    def _record(self, opid, r, w):
        for k in r:
            self.readers.setdefault(k, set()).add(opid)
        for k in w:
            self.lastw[k] = opid
            self.readers[k] = set()

    def op(self, eng, fn, r=(), w=()):
        waits = self._waits(eng, self._collect(r, w))
        i = self.cnt[eng]
        self.cnt[eng] += 1
        self.prog[eng].append((waits, fn, (eng, i)))
        self._record((eng, i), r, w)

    def dma(self, eng, out, in_, slot, r=(), w=()):
        waits = self._waits(eng, self._collect(r, w))
        n = self.dcnt.get(slot, 0) + 1
        self.dcnt[slot] = n
        src = ('dma', slot)
        self.prog[eng].append((waits, (lambda e, o=out, i=in_: e.dma_start(out=o, in_=i)), (src, n)))
        self._record((src, n), r, w)

    def barrier(self):
        latest = []
        for e in ENGS:
            if self.cnt[e] > 0:
                latest.append((e, self.cnt[e] - 1))
        for slot, n in self.dcnt.items():
            latest.append((('dma', slot), n))
        for e in ENGS:
            waits = self._waits(e, latest)
            if waits:
                self.prog[e].append((waits, None, None))
        self.lastw.clear()
        self.readers.clear()

    def emit(self, nc, st):
        sems = {}

        def getsem(key):
            if key not in sems:
                sems[key] = st.enter_context(nc.semaphore("s%d" % len(sems)))
            return sems[key]
        rank = {}
        for e in ENGS:
            tl = sorted(self.targets[e])
            rank[e] = {v: k for k, v in enumerate(tl)}

        def wait_of(src, v):
            if isinstance(src, tuple):
                return getsem(src), 16 * v
            k = rank[src][v]
            return getsem((src, k // CH)), k % CH + 1
        plan = {}
        for e in ENGS:
            lst = []
            for waits, fn, opid in self.prog[e]:
                ws = [wait_of(s, v) for (s, v) in waits]
                inc = None
                if fn is not None:
                    src, v = opid
                    if isinstance(src, tuple):
                        inc = (getsem(src), 16)
                    elif v in rank[src]:
                        k = rank[src][v]
                        inc = (getsem((src, k // CH)), 1)
                lst.append((ws, fn, inc))
            plan[e] = lst

        def run(name, e):
            for ws, fn, inc in plan[name]:
                for (s, v) in ws:
                    e.wait_ge(s, v)
                if fn is not None:
                    ins = fn(e)
                    if inc is not None:
                        ins.then_inc(inc[0], inc[1])
        block = st.enter_context(nc.Block())

        @block.tensor
        def _(e):
            run('pe', e)

        @block.scalar
        def _(e):
            run('act', e)

        @block.vector
        def _(e):
            run('dve', e)

        @block.gpsimd
        def _(e):
            run('pool', e)

        @block.sync
        def _(e):
            run('sp', e)


class Arena:
    def __init__(self, t, nelem_bf16):
        self.t = t
        self.n = nelem_bf16
        self.off = 0
        self.peak = 0

    def mark(self):
        return self.off

    def release(self, m):
        self.off = m

    def alloc(self, shape, dtype, parts=128):
        sz = 4 if dtype == F32 else 2
        n = 1
        for s in shape:
            n *= s
        nb = (n * sz + 3) // 4 * 4
        start = self.off
        self.off += nb
        self.peak = max(self.peak, self.off)
        assert self.off <= self.n * 2, ("arena overflow", self.off)
        ap = self.t[:, start // 2:(start + n * sz) // 2]
        if dtype == F32:
            ap = ap.bitcast(F32)
        if len(shape) == 2:
            ap = ap.rearrange("p (a b) -> p a b", a=shape[0])
        elif len(shape) == 3:
            ap = ap.rearrange("p (a b c) -> p a b c", a=shape[0], b=shape[1])
        if parts != 128:
            ap = ap[0:parts]
        return ap


def _t5_bucket_np(dist):
    n = np.maximum(dist, 0)
    nf = np.maximum(n, 1).astype(np.float32)
    lb = 16 + (np.log(nf / 16.0) / np.log(8.0) * 16.0).astype(np.int32)
    lb = np.minimum(lb, 31)
    return np.where(n < 16, n, lb)


def _win_chunks():
    ch = []
    for c in range(4):
        ch.append(list(range(128 * c, 128 * c + 128)))
    for g in range(2):
        ch.append(list(range(512 + 64 * g, 576 + 64 * g)) + list(range(640 + 64 * g, 704 + 64 * g)))
    for g in range(2):
        ch.append(list(range(768 + 64 * g, 832 + 64 * g)) * 2)
    for g in range(2):
        ch.append(list(range(1024 + 64 * g, 1088 + 64 * g)) * 2)
    ch.append(list(range(896, 1024)))
    ch.append(list(range(1152, 1280)))
    ch.append(list(range(1280, 1304)) + [-1] * 104)
    for c in range(4):
        ch.append(list(range(1304 + 128 * c, 1304 + 128 * c + 128)))
    for c in range(4):
        ch.append(list(range(1816 + 128 * c, 1816 + 128 * c + 128)))
    for c in range(4):
        ch.append(list(range(2328 + 128 * c, 2328 + 128 * c + 128)))
    return ch


def _host_consts():
    c = {}
    c['c_ident'] = np.eye(128, dtype=np.float32)
    j = np.arange(128)[:, None]
    s = np.arange(128)[None, :]
    c['c_tri'] = (j >= s).astype(np.float32)
    c['c_strict'] = np.where(s <= j, NEG, 0.0).astype(np.float32)
    c['c_wincorr'] = np.where(s >= j, NEG, 0.0).astype(np.float32)
    sel = np.zeros((32, 16, 128), np.float32)
    for cc in range(16):
        for sp in range(128):
            sel[2 * cc + sp // 64, cc, sp] = 1.0
    c['c_sele'] = sel.reshape(32, 16 * 128)
    n = np.arange(127)[:, None]
    sj = np.arange(32)[None, :]
    ovl = ((16 * n < 64 * (sj + 1)) & (16 * n + 32 > 64 * sj)).astype(np.float32)
    c['c_ovl'] = ovl
    addc = np.zeros((128, 16, 32), np.float32)
    for i in range(16):
        t = 128 * i + np.arange(128)
        cur = (t // 64)[:, None]
        jj = np.arange(32)[None, :]
        forced = (jj == 0) | (jj == cur) | (jj == cur - 1)
        a = np.where(forced, 1e6, 0.0)
        a = np.where(jj <= cur, a, -1e30)
        addc[:, i, :] = a
    c['c_addc'] = addc.reshape(128, 512)
    idx = np.zeros((128, 503), np.float32)
    msk = np.zeros((128, 503), np.float32)
    p = np.arange(128)[:, None]
    jp = np.arange(256)[None, :]
    d1 = jp - p
    idx[:, 0:256] = _t5_bucket_np(d1)
    msk[:, 0:256] = np.where(d1 < 0, NEG, 0.0)
    m = np.arange(247)[None, :] - 120
    d2 = p - 16 * m - 31
    idx[:, 256:503] = _t5_bucket_np(d2)
    msk[:, 256:503] = np.where(d2 < 0, NEG, 0.0)
    c['c_idx'] = idx
    c['c_mask'] = msk
    return c


def _prep_weights(inp):
    f = lambda a: np.ascontiguousarray(np.asarray(a, dtype=np.float32))
    w = {}
    w_in = f(inp['w_in'])[0]
    chs = _win_chunks()
    wr = np.zeros((len(chs), 128, 8, 128), np.float32)
    w3 = w_in.reshape(8, 128, 2840)
    for k, cols in enumerate(chs):
        cols = np.array(cols)
        ok = cols >= 0
        wr[k][:, :, ok] = np.transpose(w3[:, :, cols[ok]], (1, 0, 2))
    w['w_in_r'] = wr
    w_up = f(inp['w_up'])[0]
    w['w_up_r'] = np.ascontiguousarray(np.transpose(w_up.reshape(8, 128, 44, 128), (2, 1, 0, 3)))
    w['w_down_r'] = f(inp['w_down'])[0].reshape(22, 128, 1024)
    w['w_out_r'] = f(inp['w_out'])[0].reshape(8, 128, 1024)
    k1 = f(inp['cmp_k_w1'])[0].reshape(32, 64, 256).transpose(1, 0, 2)
    v1 = f(inp['cmp_v_w1'])[0].reshape(32, 64, 256).transpose(1, 0, 2)
    w['w1r'] = np.ascontiguousarray(np.concatenate([k1, v1], axis=0))
    k2 = f(inp['cmp_k_w2'])[0].reshape(2, 128, 64).transpose(1, 0, 2)
    w['w2k_r'] = np.ascontiguousarray(np.concatenate([k2, k2], axis=2))
    w['w2v_r'] = np.ascontiguousarray(f(inp['cmp_v_w2'])[0].reshape(2, 128, 64).transpose(1, 0, 2))
    w['posT'] = np.ascontiguousarray(np.concatenate([f(inp['cmp_pos_k'])[0].T, f(inp['cmp_pos_v'])[0].T], axis=0))
    w['norm1_w'] = f(inp['norm1_w']).reshape(1, 1024)
    w['norm2_w'] = f(inp['norm2_w']).reshape(1, 1024)
    w['final_w'] = f(inp['final_norm_w']).reshape(1, 1024)
    w['onorm_w'] = np.concatenate([f(inp['nsa_out_norm_w']).reshape(1, 512), f(inp['sb_out_norm_w']).reshape(1, 512)], axis=1)
    w['gate_b'] = f(inp['gate_b']).reshape(1, 24)
    w['rel_bias'] = f(inp['rel_bias']).reshape(1, 256)
    cw = f(inp['conv_w'])[0]
    cb = f(inp['conv_b'])[0]
    cp = np.stack([cw[0], cw[1], cw[2], cb], axis=1).reshape(44, 128, 4).transpose(1, 0, 2)
    w['convp'] = np.ascontiguousarray(cp)
    w.update(_host_consts())
    return w


def build_program(nseq):
    nc = bass.Bass("TRN2", target_bir_lowering=False)
    S = Sched()
    dr = {}

    def din(name, shape):
        dr[name] = nc.dram_tensor(name, list(shape), F32, kind="ExternalInput").ap()
        return dr[name]
    x_d = din('x', (nseq, T, D))
    win_d = din('w_in_r', (25, 128, 8, 128))
    wup_d = din('w_up_r', (44, 128, 8, 128))
    wdn_d = din('w_down_r', (22, 128, 1024))
    wout_d = din('w_out_r', (8, 128, 1024))
    w1_d = din('w1r', (128, 32, 256))
    w2k_d = din('w2k_r', (128, 2, 128))
    w2v_d = din('w2v_r', (128, 2, 64))
    posT_d = din('posT', (128, 32))
    n1_d = din('norm1_w', (1, 1024))
    n2_d = din('norm2_w', (1, 1024))
    nf_d = din('final_w', (1, 1024))
    on_d = din('onorm_w', (1, 1024))
    gb_d = din('gate_b', (1, 24))
    rb_d = din('rel_bias', (1, 256))
    cp_d = din('convp', (128, 44, 4))
    cid_d = din('c_ident', (128, 128))
    ctri_d = din('c_tri', (128, 128))
    cstr_d = din('c_strict', (128, 128))
    cwc_d = din('c_wincorr', (128, 128))
    csel_d = din('c_sele', (32, 2048))
    covl_d = din('c_ovl', (127, 32))
    cadd_d = din('c_addc', (128, 512))
    cidx_d = din('c_idx', (128, 503))
    cmsk_d = din('c_mask', (128, 503))
    out_d = nc.dram_tensor("out", [nseq, T, D], F32, kind="ExternalOutput").ap()

    st = ExitStack()
    NEL = 105000
    arena_t = st.enter_context(nc.sbuf_tensor("arena", [128, NEL], BF16))
    A = Arena(arena_t, NEL)
    PS = [st.enter_context(nc.psum_tensor("ps%d" % i, [128, 512], F32)) for i in range(8)]

    def psf(b):
        return PS[b][:]

    def psb(b):
        return PS[b][:].bitcast(BF16)

    def mm(out, lhsT, rhs, start, stop, r, w, skip=False):
        S.op('pe', lambda e, o=out, l=lhsT, rr=rhs, s0=start, s1=stop, sk=skip: e.matmul(o, lhsT=l, rhs=rr, start=s0, stop=s1, skip_group_check=sk), r=r, w=w)

    def tp(out, in_, idn, r, w):
        S.op('pe', lambda e, o=out, i=in_, d=idn: e.transpose(o, i, d), r=r, w=w)

    def act(out, in_, func, r, w, bias=None, scale=None, accum=None, eng='act'):
        kw = {}
        if bias is not None:
            kw['bias'] = bias
        if scale is not None:
            kw['scale'] = scale
        if accum is not None:
            kw['accum_out'] = accum
        S.op('act', lambda e, o=out, i=in_, f=func, k=kw: e.activation(out=o, in_=i, func=f, **k), r=r, w=w)

    def tt(eng, out, in0, in1, op, r, w):
        S.op(eng, lambda e, o=out, a=in0, b=in1, p=op: e.tensor_tensor(out=o, in0=a, in1=b, op=p), r=r, w=w)

    def ts(eng, out, in0, s1, s2, op0, op1, r, w):
        if s2 is None:
            S.op(eng, lambda e, o=out, a=in0, s=s1, p=op0: e.tensor_single_scalar(out=o, in_=a, scalar=s, op=p), r=r, w=w)
        else:
            S.op(eng, lambda e, o=out, a=in0, x1=s1, x2=s2, p0=op0, p1=op1: e.tensor_scalar(out=o, in0=a, scalar1=x1, scalar2=x2, op0=p0, op1=p1), r=r, w=w)

    def stt(eng, out, in0, scalar, in1, op0, op1, r, w):
        S.op(eng, lambda e, o=out, a=in0, s=scalar, b=in1, p0=op0, p1=op1: e.scalar_tensor_tensor(out=o, in0=a, scalar=s, in1=b, op0=p0, op1=p1), r=r, w=w)

    def cp(eng, out, in_, r, w):
        if eng == 'act':
            S.op('act', lambda e, o=out, i=in_: e.copy(out=o, in_=i), r=r, w=w)
        else:
            S.op(eng, lambda e, o=out, i=in_: e.tensor_copy(out=o, in_=i), r=r, w=w)


    def rsq(vec, scale, key):
        act(vec, vec, AF.Sqrt, [key], [key], bias=EPS, scale=scale)
        S.op('dve', lambda e, v=vec: e.reciprocal(out=v, in_=v), r=[key], w=[key])

    def run_pipeline(items, lags):
        n = len(items)
        L = max(lags)
        for s_ in range(n + L):
            for j_, lag in enumerate(lags):
                k = s_ - lag
                if 0 <= k < n:
                    items[k][j_]()

    def memset(eng, ap, val, w):
        S.op(eng, lambda e, a=ap, v=val: e.memset(a, v), r=(), w=w)

    ident = A.alloc([128], BF16)
    tri = A.alloc([128], BF16)
    strict = A.alloc([128], BF16)
    wincorr = A.alloc([128], BF16)
    onescol = A.alloc([2], BF16)
    sele = A.alloc([16, 128], BF16)
    addc = A.alloc([16, 32], F32)
    corr = A.alloc([8, 256], BF16)
    bcm = A.alloc([8, 247], BF16)
    vcA = A.alloc([2, 97], BF16)
    b31c = A.alloc([8], F32)
    gateb = A.alloc([24], F32)
    convp = A.alloc([44, 4], F32)
    normw1 = A.alloc([1024], F32)
    normw2 = A.alloc([1024], F32)
    normwf = A.alloc([1024], F32)
    onormw = A.alloc([1024], F32)
    w2k = A.alloc([2, 128], BF16)
    w2v = A.alloc([2, 64], BF16)
    cbias = A.alloc([4], F32)
    posT = A.alloc([32], BF16)
    ws = [A.alloc([1024], BF16) for _ in range(NWS)]
    xbuf = [A.alloc([1024], F32) for _ in range(2)]
    ubuf = [A.alloc([1024], BF16) for _ in range(2)]
    junk = A.alloc([1024], BF16)
    ss = A.alloc([16], F32)
    rstd = A.alloc([16], F32)
    halo = A.alloc([44, 2], F32)
    m_common = A.mark()

    def ld(eng, dst, src, slot, wkey):
        S.dma(eng, dst, src, slot=slot, w=[wkey])
    ld('pool', ident, cid_d, 'c0', 'ident')
    ld('pool', tri, ctri_d, 'c1', 'tri')
    ld('pool', strict, cstr_d, 'c2', 'strict')
    ld('pool', wincorr, cwc_d, 'c3', 'wincorr')
    ld('pool', sele[0:32].rearrange("p a b -> p (a b)"), csel_d, 'c4', 'sele')
    ld('sp', addc.rearrange("p a b -> p (a b)"), cadd_d, 'c5', 'addc')
    ld('pool', vcA[0:127, 0, 65:97], covl_d, 'c6', 'vcA')
    ld('pool', vcA[0:127, 1, 65:97], covl_d, 'c7', 'vcA')
    ld('sp', gateb, gb_d[0:1, :].partition_broadcast(128), 'c8', 'gateb')
    ld('sp', convp.rearrange("p a b -> p (a b)"), cp_d.rearrange("p a b -> p (a b)"), 'c9', 'convp')
    ld('sp', normw1, n1_d[0:1, :].partition_broadcast(128), 'c10', 'normw1')
    ld('sp', normw2, n2_d[0:1, :].partition_broadcast(128), 'c11', 'normw2')
    ld('sp', normwf, nf_d[0:1, :].partition_broadcast(128), 'c12', 'normwf')
    ld('sp', onormw, on_d[0:1, :].partition_broadcast(128), 'c13', 'onormw')
    ld('pool', w2k.rearrange("p a b -> p (a b)"), w2k_d.rearrange("p a b -> p (a b)"), 'c14', 'w2k')
    ld('pool', w2v.rearrange("p a b -> p (a b)"), w2v_d.rearrange("p a b -> p (a b)"), 'c15', 'w2v')
    ld('pool', posT, posT_d, 'c16', 'posT')
    memset('dve', onescol, 1.0, ['onescol'])
    memset('dve', vcA[:, :, 64:65], 1.0, ['vcA'])
    memset('dve', junk, 0.0, ['junk'])

    m0 = A.mark()
    RB = A.alloc([32, 8], F32)
    idxt = A.alloc([503], F32)
    mskt = A.alloc([503], F32)
    accb = A.alloc([8, 503], F32)
    eqm = A.alloc([503], F32)
    ld('sp', RB.rearrange("p a b -> p (a b)"), rb_d[0:1, :].partition_broadcast(128), 'c17', 'RB')
    ld('sp', idxt, cidx_d, 'c18', 'idxt')
    ld('sp', mskt, cmsk_d, 'c19', 'mskt')
    cp('dve', b31c, RB[:, 31, :], ['RB'], ['b31c'])
    tt('dve', RB, RB, b31c.unsqueeze(1).to_broadcast([128, 32, 8]), ALU.subtract, ['RB', 'b31c'], ['RB'])
    memset('dve', accb, 0.0, ['accb'])
    for k in range(31):
        ts('dve', eqm, idxt, float(k), None, ALU.is_equal, None, ['idxt'], ['eqm'])
        for h in range(8):
            stt('dve', accb[:, h, :], eqm, RB[:, k, h:h + 1], accb[:, h, :], ALU.mult, ALU.add, ['eqm', 'RB', 'accb'], ['accb'])
    for h in range(8):
        tt('dve', corr[:, h, :], accb[:, h, 0:256], mskt[:, 0:256], ALU.add, ['accb', 'mskt'], ['corr'])
        tt('dve', bcm[:, h, :], accb[:, h, 256:503], mskt[:, 256:503], ALU.add, ['accb', 'mskt'], ['bcm'])
    S.barrier()
    A.release(m0)

    plan = []
    for b in range(nseq):
        for k in range(25):
            plan.append(win_d[k].rearrange("p a b -> p (a b)"))
        for blk in range(2):
            for j in range(22):
                plan.append(wup_d[j].rearrange("p a b -> p (a b)"))
                plan.append(wup_d[22 + j].rearrange("p a b -> p (a b)"))
    wstate = {'issued': 0, 'next': 0}

    def w_issue():
        k = wstate['issued']
        if k < len(plan):
            S.dma('pool', ws[k % NWS], plan[k], slot='ws%d' % (k % NWS), w=[('ws', k % NWS)])
            wstate['issued'] = k + 1

    def w_get():
        k = wstate['next']
        wstate['next'] = k + 1
        assert k < wstate['issued']
        return ws[k % NWS].rearrange("p (a b) -> p a b", a=8), ('ws', k % NWS)

    for _ in range(NWS):
        w_issue()


    for b in range(nseq):
        m_seq = A.mark()
        mixed = A.alloc([16, 1024], BF16)
        m_ab = A.mark()
        uT = A.alloc([8, 2048], BF16)
        qkv_m = A.mark()

        memset('dve', ss, 0.0, ['ss'])
        for i in range(16):
            xb = xbuf[i % 2]
            ub = ubuf[i % 2]
            S.dma('sp', xb, x_d[b, 128 * i:128 * i + 128, :], slot='x%d' % (i % 2), w=[('xbuf', i % 2)])
            act(junk, xb, AF.Square, [('xbuf', i % 2)], ['junk', 'ss'], accum=ss[:, i:i + 1])
            cp('dve', rstd[:, i:i + 1], ss[:, i:i + 1], ['ss'], ['rstd'])
            rsq(rstd[:, i:i + 1], 1.0 / D, 'rstd')
            stt('dve', ub, xb, rstd[:, i:i + 1], normw1, ALU.mult, ALU.mult, [('xbuf', i % 2), 'rstd', 'normw1'], [('ub', i % 2)])
            bk = i % 2
            for dc in range(8):
                tp(psb(bk)[:, dc * 128:(dc + 1) * 128], ub[:, dc * 128:(dc + 1) * 128], ident, [('ub', i % 2), 'ident'], [('ps', bk)])
            cp('act', uT[:, :, 128 * i:128 * i + 128], psb(bk).rearrange("p (a b) -> p a b", a=8), [('ps', bk)], ['uT'])

        def proj_F(dst_fn, scale, keyw):
            wch, wk = w_get()
            for Q in range(4):
                bk = 2 + (proj_F.n % 4)
                proj_F.n += 1
                for dc in range(8):
                    mm(psf(bk), wch[:, dc, :], uT[:, dc, 512 * Q:512 * Q + 512], dc == 0, dc == 7, [wk, 'uT'], [('ps', bk)])
                dst = dst_fn(Q)
                if proj_F.n % 2 == 0:
                    act(dst, psf(bk), AF.Copy, [('ps', bk)], [keyw], scale=scale)
                else:
                    ts('dve', dst, psf(bk), scale, None, ALU.mult, None, [('ps', bk)], [keyw])
            w_issue()
        proj_F.n = 0

        def proj_T(evac_fn, ncols=128):
            wch, wk = w_get()
            for tg in range(4):
                bk = 2 + (proj_F.n % 4)
                proj_F.n += 1
                for tl in range(4):
                    i = 4 * tg + tl
                    for dc in range(8):
                        mm(psf(bk)[:, tl * 128:tl * 128 + ncols], uT[:, dc, 128 * i:128 * i + 128], wch[:, dc, 0:ncols], dc == 0, dc == 7, [wk, 'uT'], [('ps', bk)])
                evac_fn(tg, bk)
            w_issue()

        qnT = A.alloc([4, 2048], BF16)
        kcvcT = A.alloc([2, 2048], BF16)
        ksT = A.alloc([2, 2048], BF16)
        kwT = A.alloc([2, 2048], BF16)
        vsA = A.alloc([16, 130], BF16)
        vwA = A.alloc([16, 130], BF16)
        gT = A.alloc([16, 24], F32)
        kcmpT = A.alloc([2, 127], BF16)
        work_m = A.mark()
        memset('dve', vsA, 1.0, ['vsA'])
        memset('dve', vwA, 1.0, ['vwA'])
        for c in range(4):
            proj_F(lambda Q, c=c: qnT[:, c, 512 * Q:512 * Q + 512], 0.125, 'qnT')
        for g in range(2):
            proj_F(lambda Q, g=g: kcvcT[:, g, 512 * Q:512 * Q + 512], 1.0, 'kcvcT')
        for g in range(2):
            proj_F(lambda Q, g=g: ksT[:, g, 512 * Q:512 * Q + 512], 1.0, 'ksT')
        for g in range(2):
            proj_F(lambda Q, g=g: kwT[:, g, 512 * Q:512 * Q + 512], 1.0, 'kwT')

        def evac_v(dstA, key):
            def f(tg, bk):
                src = psf(bk).rearrange("p (a g d) -> p a g d", a=4, g=2)
                dst = dstA[:, 4 * tg:4 * tg + 4, :].rearrange("p a (g e) -> p a g e", g=2)[:, :, :, 0:64]
                cp('dve', dst, src, [('ps', bk)], [key])
            return f
        proj_T(evac_v(vsA, 'vsA'))
        proj_T(evac_v(vwA, 'vwA'))

        def evac_g(tg, bk):
            src = psf(bk).rearrange("p (a c) -> p a c", a=4)[:, :, 0:24]
            dst = gT[:, 4 * tg:4 * tg + 4, :]
            tt('dve', dst, src, gateb.unsqueeze(1).to_broadcast([128, 4, 24]), ALU.add, [('ps', bk), 'gateb'], ['gT'])
            act(dst, dst, AF.Sigmoid, ['gT'], ['gT'])
        proj_T(evac_g, ncols=24)

        w1sb = A.alloc([32, 256], BF16)
        S.dma('pool', w1sb.rearrange("p a b -> p (a b)"), w1_d.rearrange("p a b -> p (a b)"), slot='w1', w=['w1sb'])
        geluT = A.alloc([2, 127], BF16)
        gx = A.alloc([127], F32)
        gt_ = A.alloc([127], F32)
        for kv in range(2):
            rows = slice(64 * kv, 64 * kv + 64)
            for hcc in range(2):
                bk = 0
                for i in range(32):
                    mm(psf(bk)[:, 0:1], w1sb[rows, i, hcc * 128:hcc * 128 + 128], posT[rows, i:i + 1], i == 0, i == 31, ['w1sb', 'posT'], [('ps', bk)])
                cp('dve', cbias[:, kv * 2 + hcc:kv * 2 + hcc + 1], psf(bk)[:, 0:1], [('ps', bk)], ['cbias'])
        for g in range(2):
            for kv in range(2):
                rows = slice(64 * kv, 64 * kv + 64)
                for hcc in range(2):
                    bk = hcc
                    for i in range(32):
                        mm(psf(bk)[:, 0:127], w1sb[rows, i, hcc * 128:hcc * 128 + 128], kcvcT[rows, g, i:i + 16 * 126 + 1:16], i == 0, i == 31, ['w1sb', 'kcvcT'], [('ps', bk)])
                    ts('dve', gx, psf(bk)[:, 0:127], cbias[:, kv * 2 + hcc:kv * 2 + hcc + 1], None, ALU.add, None, [('ps', bk), 'cbias'], ['gx'])
                    tt('dve', gt_, gx, gx, ALU.mult, ['gx'], ['gt'])
                    ts('dve', gt_, gt_, 0.044715, 1.0, ALU.mult, ALU.add, ['gt'], ['gt'])
                    tt('dve', gt_, gt_, gx, ALU.mult, ['gt', 'gx'], ['gt'])
                    act(gt_, gt_, AF.Sigmoid, ['gt'], ['gt'], scale=1.5957691216057308)
                    tt('dve', geluT[:, hcc, :], gx, gt_, ALU.mult, ['gx', 'gt'], ['geluT'])
                bk = 2
                if kv == 0:
                    for hcc in range(2):
                        mm(psf(bk)[:, 0:127], w2k[:, hcc, :], geluT[:, hcc, :], hcc == 0, hcc == 1, ['w2k', 'geluT'], [('ps', bk)])
                    cp('dve', kcmpT[:, g, :], psf(bk)[:, 0:127], [('ps', bk)], ['kcmpT'])
                else:
                    for hcc in range(2):
                        mm(psf(bk)[0:127, 0:64], geluT[:, hcc, :], w2v[:, hcc, :], hcc == 0, hcc == 1, ['w2v', 'geluT'], [('ps', bk)])
                    cp('dve', vcA[0:127, g, 0:64], psf(bk)[0:127, 0:64], [('ps', bk)], ['vcA'])
        S.barrier()
        A.release(work_m)
        kcmp_keep = kcmpT
        att_m = A.mark()

        Pc = [A.alloc([127], BF16) for _ in range(2)]
        PcT = [A.alloc([128], BF16) for _ in range(2)]
        PT = [A.alloc([512], BF16) for _ in range(3)]
        onsa2 = [A.alloc([16, 64], F32) for _ in range(2)]
        tmpo = A.alloc([4, 64], F32)
        impa2 = [A.alloc([4, 32], F32) for _ in range(2)]
        score = A.alloc([4, 32], F32)
        top8 = A.alloc([8], F32)
        thr = A.alloc([1], F32)
        negm = A.alloc([4, 32], BF16)
        negmT2 = [A.alloc([512], BF16) for _ in range(2)]
        rd = A.alloc([4], F32)
        sc = A.alloc([4], F32)
        sqt = A.alloc([16, 64], F32)
        ssh = A.alloc([16], F32)
        obn = [0]

        def finalize(ob, h, branch, Q, with_imp=False):
            g_ = h // 4
            onsa = onsa2[g_]
            impa = impa2[g_]
            O = psf(ob).rearrange("p (a c) -> p a c", a=4)
            r_ = h % 4
            ts('dve', rd, O[:, :, 64], 1e-30, None, ALU.max, None, [('ps', ob)], ['rd'])
            S.op('dve', lambda e: e.reciprocal(out=rd, in_=rd), r=['rd'], w=['rd'])
            tt('dve', sc, rd, gT[:, 4 * Q:4 * Q + 4, 3 * h + branch], ALU.mult, ['rd', 'gT'], ['sc'])
            tt('dve', tmpo, O[:, :, 0:64], sc.unsqueeze(2).to_broadcast([128, 4, 64]), ALU.mult, [('ps', ob), 'sc'], ['tmpo'])
            ov = onsa.rearrange("p (a r) d -> p a r d", a=4)[:, :, r_, :]
            tt('dve', ov, ov, tmpo, ALU.add, ['tmpo', ('onsa', g_)], [('onsa', g_)])
            if with_imp:
                tt('dve', score, O[:, :, 65:97], rd.unsqueeze(2).to_broadcast([128, 4, 32]), ALU.mult, [('ps', ob), 'rd'], ['score'])
                tt('dve', impa, impa, score, ALU.add, ['score', ('impa', g_)], [('impa', g_)])

        def headnorm_nsa(Q, g_):
            onsa = onsa2[g_]
            tt('dve', sqt, onsa, onsa, ALU.mult, [('onsa', g_)], ['sqt'])
            S.op('dve', lambda e: e.tensor_reduce(out=ssh, in_=sqt, axis=AX.X, op=ALU.add), r=['sqt'], w=['ssh'])
            rsq(ssh, 1.0 / 64, 'ssh')
            tt('dve', sqt, onsa, ssh.unsqueeze(2).to_broadcast([128, 16, 64]), ALU.mult, [('onsa', g_), 'ssh', 'sqt'], ['sqt'])
            tt('dve', mixed[:, 4 * Q:4 * Q + 4, 256 * g_:256 * g_ + 256], sqt.rearrange("p (a r) d -> p a (r d)", a=4),
               onormw[:, 256 * g_:256 * g_ + 256].unsqueeze(1).to_broadcast([128, 4, 256]), ALU.mult, ['sqt', 'onormw'], ['mixed'])

        for Q in range(4):
            for g in range(2):
                memset('dve', onsa2[g], 0.0, [('onsa', g)])
                memset('dve', impa2[g], 0.0, [('impa', g)])
            items = []
            for g in range(2):
                for r_ in range(4):
                    h = 4 * g + r_
                    ob = 6 + (obn[0] % 2)
                    obn[0] += 1
                    for tl in range(4):
                        k_ = len(items)
                        def mk(g=g, h=h, ob=ob, tl=tl, k_=k_, Q=Q):
                            hf = slice(64 * (h % 2), 64 * (h % 2) + 64)
                            i = 4 * Q + tl
                            bk = k_ % 2
                            pc = Pc[k_ % 2]
                            pct = PcT[k_ % 2]
                            tcol = 128 * (k_ % 2)
                            O = psf(ob).rearrange("p (a c) -> p a c", a=4)

                            def s1():
                                mm(psf(bk)[:, 0:127], qnT[hf, h // 2, 128 * i:128 * i + 128], kcmp_keep[hf, g, :], True, False, ['qnT', 'kcmpT'], [('ps', bk)])
                                mm(psf(bk)[:, 0:127], ident, bcm[:, h, 120 - 8 * i:120 - 8 * i + 127], False, True, ['ident', 'bcm'], [('ps', bk)])

                            def s2():
                                act(pc, psf(bk)[:, 0:127], AF.Exp, [('ps', bk), 'b31c'], [('Pc', k_ % 2)], bias=b31c[:, h:h + 1])

                            def s3():
                                tp(psb(2)[0:127, tcol:tcol + 128], pc, ident, [('Pc', k_ % 2), 'ident'], [('ps2', k_ % 2)])

                            def s4():
                                cp('dve', pct[0:127, :], psb(2)[0:127, tcol:tcol + 128], [('ps2', k_ % 2)], [('PcT', k_ % 2)])

                            def s5():
                                mm(O[:, tl, 0:97], pct[0:127, :], vcA[0:127, g, :], True, True, [('PcT', k_ % 2), 'vcA'], [('ps', ob)])
                                if tl == 3:
                                    finalize(ob, h, 0, Q, with_imp=True)
                            return [s1, s2, s3, s4, s5]
                        items.append(mk())
            run_pipeline(items, [0, 1, 2, 3, 4])
            for g in range(2):
                tt('dve', score, impa2[g], addc[:, 4 * Q:4 * Q + 4, :], ALU.add, [('impa', g), 'addc'], ['score'])
                for tl in range(4):
                    S.op('dve', lambda e, tl=tl: e.max(out=top8, in_=score[:, tl, :]), r=['score'], w=['top8'])
                    ts('dve', thr, top8[:, 7:8], -5e29, None, ALU.max, None, ['top8'], ['thr'])
                    ts('dve', negm[:, tl, :], score[:, tl, :], thr[:, 0:1], NEG, ALU.is_lt, ALU.mult, ['score', 'thr'], ['negm'])
                    tp(psb(2)[0:32, 256 + tl * 128:256 + tl * 128 + 128], negm[:, tl, :], ident, ['negm', 'ident'], [('ps2', 2)])
                cp('dve', negmT2[g][0:32, :], psb(2)[0:32, 256:768], [('ps2', 2)], [('negmT', g)])
            items = []
            for g in range(2):
                for branch in (2, 1):
                    kT = ksT if branch == 1 else kwT
                    vA = vsA if branch == 1 else vwA
                    kkey = 'ksT' if branch == 1 else 'kwT'
                    vkey = 'vsA' if branch == 1 else 'vwA'
                    for r_ in range(4):
                        h = 4 * g + r_
                        ob = 6 + (obn[0] % 2)
                        obn[0] += 1
                        c_lo = 0 if branch == 1 else max(0, 4 * Q - 4)
                        for c in range(c_lo, 4 * Q + 4):
                            k_ = len(items)
                            last_item = (branch == 1 and r_ == 3 and c == 4 * Q + 3)
                            def mk(g=g, branch=branch, kT=kT, vA=vA, kkey=kkey, vkey=vkey, h=h, ob=ob, c=c, c_lo=c_lo, k_=k_, Q=Q, last_item=last_item):
                                hf = slice(64 * (h % 2), 64 * (h % 2) + 64)
                                O = psf(ob).rearrange("p (a c) -> p a c", a=4)
                                qt_lo = max(c, 4 * Q)
                                qt_hi = 4 * Q + 3 if branch == 1 else min(c + 4, 4 * Q + 3)
                                lo = 128 * (qt_lo - 4 * Q)
                                hi = 128 * (qt_hi - 4 * Q + 1)
                                bk = 3 + (k_ % 3)
                                pt = PT[k_ % 3]
                                pk = ('PT', k_ % 3)

                                def s1():
                                    extra = []
                                    if branch == 1:
                                        extra.append(('m', lo, hi))
                                    for qt in range(qt_lo, qt_hi + 1):
                                        o_ = qt - c
                                        cl = 128 * (qt - 4 * Q)
                                        if o_ <= 1:
                                            extra.append(('c', cl, o_))
                                        if branch == 2 and o_ == 4:
                                            extra.append(('w', cl, 0))
                                    mm(psf(bk)[:, lo:hi], kT[hf, g, 128 * c:128 * c + 128], qnT[hf, h // 2, 512 * Q + lo:512 * Q + hi], True, len(extra) == 0, [kkey, 'qnT'], [('ps', bk)])
                                    for j_, ex in enumerate(extra):
                                        last = j_ == len(extra) - 1
                                        if ex[0] == 'm':
                                            mm(psf(bk)[:, lo:hi], sele[0:32, c, :], negmT2[g][0:32, lo:hi], False, last, ['sele', ('negmT', g)], [('ps', bk)])
                                        elif ex[0] == 'c':
                                            mm(psf(bk)[:, ex[1]:ex[1] + 128], ident, corr[:, h, 128 * ex[2]:128 * ex[2] + 128], False, last, ['ident', 'corr'], [('ps', bk)])
                                        else:
                                            mm(psf(bk)[:, ex[1]:ex[1] + 128], ident, wincorr, False, last, ['ident', 'wincorr'], [('ps', bk)])

                                def s2():
                                    act(pt[:, lo:hi], psf(bk)[:, lo:hi], AF.Exp, [('ps', bk), 'b31c'], [pk], bias=b31c[:, h:h + 1])

                                def s3():
                                    if c == c_lo:
                                        memset('dve', psf(ob), 0.0, [('ps', ob)])
                                    for qt in range(qt_lo, qt_hi + 1):
                                        tl = qt - 4 * Q
                                        mm(O[:, tl, 0:65], pt[:, 128 * tl:128 * tl + 128], vA[:, c, 65 * g:65 * g + 65], False, c == qt, [pk, vkey], [('ps', ob)], skip=True)
                                    if c == 4 * Q + 3:
                                        finalize(ob, h, branch, Q)
                                    if last_item:
                                        headnorm_nsa(Q, g)
                                return [s1, s2, s3]
                            items.append(mk())
            run_pipeline(items, [0, 1, 2])


        S.barrier()
        A.release(qkv_m)

        qsT = A.alloc([4, 2048], BF16)
        ksbT = A.alloc([4, 2048], BF16)
        vS = A.alloc([16, 512], BF16)
        for c in range(4):
            proj_F(lambda Q, c=c: qsT[:, c, 512 * Q:512 * Q + 512], 0.125, 'qsT')
        for c in range(4):
            proj_F(lambda Q, c=c: ksbT[:, c, 512 * Q:512 * Q + 512], 1.0, 'ksbT')
        for c in range(4):
            def evac_vs(tg, bk, c=c):
                src = psf(bk).rearrange("p (a d) -> p a d", a=4)
                cp('dve', vS[:, 4 * tg:4 * tg + 4, 128 * c:128 * c + 128], src, [('ps', bk)], ['vS'])
            proj_T(evac_vs)

        Eb = [A.alloc([512], BF16) for _ in range(3)]
        SPb = [A.alloc([512], BF16) for _ in range(4)]
        Xb = [A.alloc([512], BF16) for _ in range(2)]
        Pb = [A.alloc([512], BF16) for _ in range(2)]
        osb = A.alloc([32, 64], F32)
        carry = A.alloc([4], F32)
        gsc = A.alloc([4], F32)
        tmps = A.alloc([4, 64], F32)
        sq2 = A.alloc([32, 64], F32)
        ss2h = A.alloc([32], F32)
        items = []
        for Q in range(4):
            for h in range(8):
                for c in range(4 * Q + 3, -1, -1):
                    k_ = len(items)
                    def mk(Q=Q, h=h, c=c, k_=k_):
                        hf = slice(64 * (h % 2), 64 * (h % 2) + 64)
                        accv = osb.rearrange("p (a h) d -> p a h d", a=4)[:, :, h, :]
                        qt_lo = max(c, 4 * Q)
                        lo = 128 * (qt_lo - 4 * Q)
                        hi = 512
                        zb = k_ % 2
                        cb_ = 2 + k_ % 2
                        rb_ = 4 + k_ % 2
                        E = Eb[k_ % 3]
                        SP = SPb[k_ % 4]
                        X = Xb[k_ % 2]
                        P = Pb[k_ % 2]
                        kE, kS, kX, kP = ('E', k_ % 3), ('SP', k_ % 4), ('X', k_ % 2), ('P', k_ % 2)
                        diag = c >= 4 * Q
                        R = psf(rb_).rearrange("p (a c) -> p a c", a=4)
                        tl0 = qt_lo - 4 * Q
                        first = (c == 4 * Q + 3)

                        def s_g():
                            if not first:
                                act(gsc, carry, AF.Exp, ['carry'], ['gsc'], scale=-1.0)

                        def s1():
                            mm(psf(zb)[:, lo:hi], ksbT[hf, h // 2, 128 * c:128 * c + 128], qsT[hf, h // 2, 512 * Q + lo:512 * Q + hi], True, not diag, ['ksbT', 'qsT'], [('ps', zb)])
                            if diag:
                                mm(psf(zb)[:, lo:lo + 128], ident, strict, False, True, ['ident', 'strict'], [('ps', zb)])

                        def s2():
                            act(E[:, lo:hi], psf(zb)[:, lo:hi], AF.Exp, [('ps', zb)], [kE])
                            act(SP[:, lo:hi], E[:, lo:hi], AF.Ln, [kE], [kS], bias=1.0)

                        def s3():
                            mm(psf(cb_)[:, lo:hi], tri, SP[:, lo:hi], True, True, ['tri', kS], [('ps', cb_)])

                        def s4():
                            act(X[:, lo:hi], psf(cb_)[:, lo:hi], AF.Exp, [('ps', cb_)], [kX], scale=-1.0)

                        def s5():
                            tt('dve', P[:, lo:hi], E[:, lo:hi], X[:, lo:hi], ALU.mult, [kE, kX], [kP])

                        def s6():
                            for tl in range(tl0, 4):
                                mm(R[:, tl, 0:64], P[:, 128 * tl:128 * tl + 128], vS[:, c, 64 * h:64 * h + 64], True, True, [kP, 'vS'], [('ps', rb_)])
                                mm(R[:, tl, 64:65], SP[:, 128 * tl:128 * tl + 128], onescol[:, 0:1], True, True, [kS, 'onescol'], [('ps', rb_)])

                        def s7():
                            if first and h == 0:
                                memset('dve', osb, 0.0, ['osb'])
                            if first:
                                memset('dve', carry, 0.0, ['carry'])
                                memset('dve', gsc, 1.0, ['gsc'])
                            tt('dve', tmps[:, tl0:4, :], R[:, tl0:4, 0:64], gsc[:, tl0:4].unsqueeze(2).to_broadcast([128, 4 - tl0, 64]), ALU.mult, [('ps', rb_), 'gsc'], ['tmps'])
                            tt('dve', accv[:, tl0:4, :], accv[:, tl0:4, :], tmps[:, tl0:4, :], ALU.add, ['tmps', 'osb'], ['osb'])
                            if c > 0:
                                tt('dve', carry[:, tl0:4], carry[:, tl0:4], R[:, tl0:4, 64], ALU.add, [('ps', rb_), 'carry'], ['carry'])
                            if c == 0 and h == 7:
                                tt('dve', sq2, osb, osb, ALU.mult, ['osb'], ['sq2'])
                                S.op('dve', lambda e: e.tensor_reduce(out=ss2h, in_=sq2, axis=AX.X, op=ALU.add), r=['sq2'], w=['ss2h'])
                                rsq(ss2h, 1.0 / 64, 'ss2h')
                                tt('dve', sq2, osb, ss2h.unsqueeze(2).to_broadcast([128, 32, 64]), ALU.mult, ['osb', 'ss2h', 'sq2'], ['sq2'])
                                tt('dve', mixed[:, 4 * Q:4 * Q + 4, 512:1024], sq2.rearrange("p (a h) d -> p a (h d)", a=4),
                                   onormw[:, 512:1024].unsqueeze(1).to_broadcast([128, 4, 512]), ALU.mult, ['sq2', 'onormw'], ['mixed'])
                        return [s1, s2, s3, s4, s5, s6, s_g, s7]
                    items.append(mk())
        run_pipeline(items, [0, 1, 2, 3, 3, 4, 5, 5])


        S.barrier()
        A.release(m_ab)

        u2T = A.alloc([8, 2048], BF16)
        m_c1 = A.mark()
        wout = A.alloc([8, 1024], BF16)
        mT = A.alloc([8, 128], BF16)
        hbuf = A.alloc([1024], F32)
        S.dma('pool', wout, wout_d.rearrange("c p n -> p c n"), slot='wout', w=['wout'])
        memset('dve', ss, 0.0, ['ss'])
        for i in range(16):
            xb = xbuf[i % 2]
            ub = ubuf[i % 2]
            S.dma('sp', xb, x_d[b, 128 * i:128 * i + 128, :], slot='x%d' % (i % 2), w=[('xbuf', i % 2)])
            bk = i % 2
            for dc in range(8):
                tp(psb(bk)[:, dc * 128:(dc + 1) * 128], mixed[:, i, dc * 128:(dc + 1) * 128], ident, ['mixed', 'ident'], [('ps', bk)])
            cp('act', mT, psb(bk).rearrange("p (a b) -> p a b", a=8), [('ps', bk)], ['mT'])
            for half in range(2):
                pb = 2 + half
                for c in range(8):
                    mm(psf(pb), mT[:, c, :], wout[:, c, 512 * half:512 * half + 512], c == 0, c == 7, ['mT', 'wout'], [('ps', pb)])
                tt('dve', hbuf[:, 512 * half:512 * half + 512], psf(pb), xb[:, 512 * half:512 * half + 512], ALU.add, [('ps', pb), ('xbuf', i % 2)], ['hbuf'])
            S.dma('sp', out_d[b, 128 * i:128 * i + 128, :], hbuf, slot='hst', r=['hbuf'], w=[('outh', i)])
            act(junk, hbuf, AF.Square, ['hbuf'], ['junk', 'ss'], accum=ss[:, i:i + 1])
            cp('dve', rstd[:, i:i + 1], ss[:, i:i + 1], ['ss'], ['rstd'])
            rsq(rstd[:, i:i + 1], 1.0 / D, 'rstd')
            stt('dve', ub, hbuf, rstd[:, i:i + 1], normw2, ALU.mult, ALU.mult, ['hbuf', 'rstd', 'normw2'], [('ub', i % 2)])
            bk2 = 4 + i % 2
            for dc in range(8):
                tp(psb(bk2)[:, dc * 128:(dc + 1) * 128], ub[:, dc * 128:(dc + 1) * 128], ident, [('ub', i % 2), 'ident'], [('ps', bk2)])
            cp('act', u2T[:, :, 128 * i:128 * i + 128], psb(bk2).rearrange("p (a b) -> p a b", a=8), [('ps', bk2)], ['u2T'])
        S.barrier()
        A.release(m_seq)
        actT_lo = A.alloc([16, 1024], BF16)
        assert A.off == m_ab
        A.off = m_c1
        actT_hi = A.alloc([6, 1024], BF16)
        wdn = A.alloc([22, 1024], BF16)
        accg = [A.alloc([512], F32) for _ in range(2)]
        accv_ = [A.alloc([512], F32) for _ in range(2)]
        sil = [A.alloc([512], F32) for _ in range(2)]
        obuf = A.alloc([1024], F32)
        fx = A.alloc([8], F32)

        def actT(j):
            return actT_lo[:, j, :] if j < 16 else actT_hi[:, j - 16, :]

        memset('dve', halo, 0.0, ['halo'])
        for blk in range(2):
            S.dma('pool', wdn, wdn_d.rearrange("j p n -> p j n"), slot='wdn', w=['wdn'])
            for j in range(22):
                wg, wgk = w_get()
                wv, wvk = w_get()
                for tb in range(2):
                    n_ = (j * 2 + tb) % 2
                    col0 = 1024 * blk + 512 * tb
                    for which, wch, wk, pb, accs, fc in ((0, wg, wgk, 0 + n_, accg, j), (1, wv, wvk, 2 + n_, accv_, 22 + j)):
                        for dc in range(8):
                            mm(psf(pb), wch[:, dc, :], u2T[:, dc, col0:col0 + 512], dc == 0, dc == 7, [wk, 'u2T'], [('ps', pb)])
                        ac = accs[n_]
                        ak = ('acc', which, n_)
                        G = psf(pb)
                        S.op('act', lambda e, o=ac, i=G, fc=fc: e.activation(out=o, in_=i, func=AF.Identity, bias=convp[:, fc, 3:4], scale=convp[:, fc, 2:3]),
                             r=[('ps', pb), 'convp'], w=[ak])
                        stt('dve', ac[:, 1:512], G[:, 0:511], convp[:, fc, 1:2], ac[:, 1:512], ALU.mult, ALU.add, [('ps', pb), 'convp', ak], [ak])
                        stt('dve', ac[:, 2:512], G[:, 0:510], convp[:, fc, 0:1], ac[:, 2:512], ALU.mult, ALU.add, [('ps', pb), 'convp', ak], [ak])
                        stt('dve', ac[:, 0:1], halo[:, fc, 1:2], convp[:, fc, 1:2], ac[:, 0:1], ALU.mult, ALU.add, ['halo', 'convp', ak], [ak])
                        stt('dve', ac[:, 0:2], halo[:, fc, 0:2], convp[:, fc, 0:1], ac[:, 0:2], ALU.mult, ALU.add, ['halo', 'convp', ak], [ak])
                        cp('dve', halo[:, fc, :], G[:, 510:512], [('ps', pb)], ['halo'])
                    act(sil[n_], accg[n_], AF.Silu, [('acc', 0, n_)], [('sil', n_)])
                    tt('pool', actT(j)[:, 512 * tb:512 * tb + 512], sil[n_], accv_[n_], ALU.mult, [('sil', n_), ('acc', 1, n_)], ['actT'])
                w_issue()
                w_issue()
            memset('dve', ss, 0.0, ['ss'])
            for tl in range(8):
                i = 8 * blk + tl
                xb = xbuf[i % 2]
                S.dma('sp', xb, out_d[b, 128 * i:128 * i + 128, :], slot='x%d' % (i % 2), r=[('outh', i)], w=[('xbuf', i % 2)])
                for half in range(2):
                    pb = 4 + (2 * tl + half) % 4
                    for j in range(22):
                        mm(psf(pb), actT(j)[:, 128 * tl:128 * tl + 128], wdn[:, j, 512 * half:512 * half + 512], j == 0, j == 21, ['actT', 'wdn'], [('ps', pb)])
                    tt('dve', xb[:, 512 * half:512 * half + 512], psf(pb), xb[:, 512 * half:512 * half + 512], ALU.add, [('ps', pb), ('xbuf', i % 2)], [('xbuf', i % 2)])
                act(junk, xb, AF.Square, [('xbuf', i % 2)], ['junk', 'ss'], accum=ss[:, tl:tl + 1])
                cp('dve', fx[:, tl:tl + 1], ss[:, tl:tl + 1], ['ss'], ['fx'])
                rsq(fx[:, tl:tl + 1], 1.0 / D, 'fx')
                stt('dve', obuf, xb, fx[:, tl:tl + 1], normwf, ALU.mult, ALU.mult, [('xbuf', i % 2), 'fx', 'normwf'], ['obuf'])
                S.dma('sp', out_d[b, 128 * i:128 * i + 128, :], obuf, slot='ost', r=['obuf'], w=[('outf', i)])
        S.barrier()
        A.release(m_seq)

    S.barrier()
    build_program.info = {'peak': A.peak, 'ops': dict(S.cnt)}
    S.emit(nc, st)
    st.close()
    return nc


_CACHE = {}


def kernel(**inputs):
    x = np.ascontiguousarray(np.asarray(inputs['x'], dtype=np.float32))
    B = x.shape[0]
    nseq = B // NCORE
    w = _prep_weights(inputs)
    if nseq not in _CACHE:
        _CACHE[nseq] = build_program(nseq)
    nc = _CACHE[nseq]
    in_maps = []
    for c in range(NCORE):
        m = dict(w)
        m['x'] = np.ascontiguousarray(x[c * nseq:(c + 1) * nseq])
        in_maps.append(m)
    res = run_bass_kernel_spmd(nc, in_maps, core_ids=list(range(NCORE)))
    out = np.concatenate([np.asarray(r['out'], dtype=np.float32) for r in res.results], axis=0)
    return out
```

```python
import numpy as np
from contextlib import ExitStack
import concourse.bass as bass
import concourse.mybir as mybir
from concourse.bass_utils import run_bass_kernel_spmd

F32 = mybir.dt.float32
BF16 = mybir.dt.bfloat16
AF = mybir.ActivationFunctionType
ALU = mybir.AluOpType
AX = mybir.AxisListType

T = 2048
D = 1024
DFF = 2816
NCORE = 8
NEG = -30000.0
EPS = 1e-6
ENGS = ['pe', 'act', 'dve', 'pool', 'sp']
CH = 4096
NWS = 5


class Sched:
    def __init__(self):
        self.prog = {e: [] for e in ENGS}
        self.cnt = {e: 0 for e in ENGS}
        self.seen = {e: {} for e in ENGS}
        self.lastw = {}
        self.readers = {}
        self.dcnt = {}
        self.targets = {e: set() for e in ENGS}

    def _collect(self, r, w):
        deps = set()
        for k in r:
            if k in self.lastw:
                deps.add(self.lastw[k])
        for k in w:
            if k in self.lastw:
                deps.add(self.lastw[k])
            deps.update(self.readers.get(k, ()))
        return deps

    def _waits(self, eng, deps):
        best = {}
        for (src, v) in deps:
            if src == eng and eng == 'pe':
                continue
            if self.seen[eng].get(src, -1) >= v:
                continue
            best[src] = max(best.get(src, -1), v)
        out = []
        for src, v in best.items():
            self.seen[eng][src] = v
            out.append((src, v))
            if src in self.targets:
                self.targets[src].add(v)
        return out

    def _record(self, opid, r, w):
        for k in r:
            self.readers.setdefault(k, set()).add(opid)
        for k in w:
            self.lastw[k] = opid
            self.readers[k] = set()

    def op(self, eng, fn, r=(), w=()):
        waits = self._waits(eng, self._collect(r, w))
        i = self.cnt[eng]
        self.cnt[eng] += 1
        self.prog[eng].append((waits, fn, (eng, i)))
        self._record((eng, i), r, w)

    def dma(self, eng, out, in_, slot, r=(), w=()):
        waits = self._waits(eng, self._collect(r, w))
        n = self.dcnt.get(slot, 0) + 1
        self.dcnt[slot] = n
        src = ('dma', slot)
        self.prog[eng].append((waits, (lambda e, o=out, i=in_: e.dma_start(out=o, in_=i)), (src, n)))
        self._record((src, n), r, w)

    def barrier(self):
        latest = []
        for e in ENGS:
            if self.cnt[e] > 0:
                latest.append((e, self.cnt[e] - 1))
        for slot, n in self.dcnt.items():
            latest.append((('dma', slot), n))
        for e in ENGS:
            waits = self._waits(e, latest)
            if waits:
                self.prog[e].append((waits, None, None))
        self.lastw.clear()
        self.readers.clear()

    def emit(self, nc, st):
        sems = {}

        def getsem(key):
            if key not in sems:
                sems[key] = st.enter_context(nc.semaphore("s%d" % len(sems)))
            return sems[key]
        rank = {}
        for e in ENGS:
            tl = sorted(self.targets[e])
            rank[e] = {v: k for k, v in enumerate(tl)}

        def wait_of(src, v):
            if isinstance(src, tuple):
                return getsem(src), 16 * v
            k = rank[src][v]
            return getsem((src, k // CH)), k % CH + 1
        plan = {}
        for e in ENGS:
            lst = []
            for waits, fn, opid in self.prog[e]:
                ws = [wait_of(s, v) for (s, v) in waits]
                inc = None
                if fn is not None:
                    src, v = opid
                    if isinstance(src, tuple):
                        inc = (getsem(src), 16)
                    elif v in rank[src]:
                        k = rank[src][v]
                        inc = (getsem((src, k // CH)), 1)
                lst.append((ws, fn, inc))
            plan[e] = lst

        def run(name, e):
            for ws, fn, inc in plan[name]:
                for (s, v) in ws:
                    e.wait_ge(s, v)
                if fn is not None:
                    ins = fn(e)
                    if inc is not None:
                        ins.then_inc(inc[0], inc[1])
        block = st.enter_context(nc.Block())

        @block.tensor
        def _(e):
            run('pe', e)

        @block.scalar
        def _(e):
            run('act', e)

        @block.vector
        def _(e):
            run('dve', e)

        @block.gpsimd
        def _(e):
            run('pool', e)

        @block.sync
        def _(e):
            run('sp', e)


class Arena:
    def __init__(self, t, nelem_bf16):
        self.t = t
        self.n = nelem_bf16
        self.off = 0
        self.peak = 0

    def mark(self):
        return self.off

    def release(self, m):
        self.off = m

    def alloc(self, shape, dtype, parts=128):
        sz = 4 if dtype == F32 else 2
        n = 1
        for s in shape:
            n *= s
        nb = (n * sz + 3) // 4 * 4
        start = self.off
        self.off += nb
        self.peak = max(self.peak, self.off)
        assert self.off <= self.n * 2, ("arena overflow", self.off)
        ap = self.t[:, start // 2:(start + n * sz) // 2]
        if dtype == F32:
            ap = ap.bitcast(F32)
        if len(shape) == 2:
            ap = ap.rearrange("p (a b) -> p a b", a=shape[0])
        elif len(shape) == 3:
            ap = ap.rearrange("p (a b c) -> p a b c", a=shape[0], b=shape[1])
        if parts != 128:
            ap = ap[0:parts]
        return ap


def _t5_bucket_np(dist):
    n = np.maximum(dist, 0)
    nf = np.maximum(n, 1).astype(np.float32)
    lb = 16 + (np.log(nf / 16.0) / np.log(8.0) * 16.0).astype(np.int32)
    lb = np.minimum(lb, 31)
    return np.where(n < 16, n, lb)


def _win_chunks():
    ch = []
    for c in range(4):
        ch.append(list(range(128 * c, 128 * c + 128)))
    for g in range(2):
        ch.append(list(range(512 + 64 * g, 576 + 64 * g)) + list(range(640 + 64 * g, 704 + 64 * g)))
    for g in range(2):
        ch.append(list(range(768 + 64 * g, 832 + 64 * g)) + [-1] * 64)
        ch.append([-1] * 64 + list(range(768 + 64 * g, 832 + 64 * g)))
    for g in range(2):
        ch.append(list(range(1024 + 64 * g, 1088 + 64 * g)) + [-1] * 64)
        ch.append([-1] * 64 + list(range(1024 + 64 * g, 1088 + 64 * g)))
    ch.append(list(range(896, 1024)))
    ch.append(list(range(1152, 1280)))
    ch.append(list(range(1280, 1304)) + [-1] * 104)
    for c in range(4):
        ch.append(list(range(1304 + 128 * c, 1304 + 128 * c + 128)))
    for c in range(4):
        ch.append(list(range(1816 + 128 * c, 1816 + 128 * c + 128)))
    for c in range(4):
        ch.append(list(range(2328 + 128 * c, 2328 + 128 * c + 128)))
    return ch


def _host_consts():
    c = {}
    c['c_ident'] = np.eye(128, dtype=np.float32)
    j = np.arange(128)[:, None]
    s = np.arange(128)[None, :]
    c['c_tri'] = (j >= s).astype(np.float32)
    c['c_strict'] = np.where(s <= j, NEG, 0.0).astype(np.float32)
    c['c_wincorr'] = np.where(s >= j, NEG, 0.0).astype(np.float32)
    sel = np.zeros((32, 16, 128), np.float32)
    for cc in range(16):
        for sp in range(128):
            sel[2 * cc + sp // 64, cc, sp] = 1.0
    c['c_sele'] = sel.reshape(32, 16 * 128)
    n = np.arange(127)[:, None]
    sj = np.arange(32)[None, :]
    ovl = ((16 * n < 64 * (sj + 1)) & (16 * n + 32 > 64 * sj)).astype(np.float32)
    c['c_ovl'] = ovl
    addc = np.zeros((128, 16, 32), np.float32)
    for i in range(16):
        t = 128 * i + np.arange(128)
        cur = (t // 64)[:, None]
        jj = np.arange(32)[None, :]
        forced = (jj == 0) | (jj == cur) | (jj == cur - 1)
        a = np.where(forced, 1e6, 0.0)
        a = np.where(jj <= cur, a, -1e30)
        addc[:, i, :] = a
    c['c_addc'] = addc.reshape(128, 512)
    idx = np.zeros((128, 503), np.float32)
    msk = np.zeros((128, 503), np.float32)
    p = np.arange(128)[:, None]
    jp = np.arange(256)[None, :]
    d1 = jp - p
    idx[:, 0:256] = _t5_bucket_np(d1)
    msk[:, 0:256] = np.where(d1 < 0, NEG, 0.0)
    m = np.arange(247)[None, :] - 120
    d2 = p - 16 * m - 31
    idx[:, 256:503] = _t5_bucket_np(d2)
    msk[:, 256:503] = np.where(d2 < 0, NEG, 0.0)
    c['c_idx'] = idx
    c['c_mask'] = msk
    return c


def _prep_weights(inp):
    f = lambda a: np.ascontiguousarray(np.asarray(a, dtype=np.float32))
    w = {}
    w_in = f(inp['w_in'])[0]
    chs = _win_chunks()
    wr = np.zeros((len(chs), 128, 8, 128), np.float32)
    w3 = w_in.reshape(8, 128, 2840)
    for k, cols in enumerate(chs):
        cols = np.array(cols)
        ok = cols >= 0
        wr[k][:, :, ok] = np.transpose(w3[:, :, cols[ok]], (1, 0, 2))
    w['w_in_r'] = wr
    w_up = f(inp['w_up'])[0]
    w['w_up_r'] = np.ascontiguousarray(np.transpose(w_up.reshape(8, 128, 44, 128), (2, 1, 0, 3)))
    w['w_down_r'] = f(inp['w_down'])[0].reshape(22, 128, 1024)
    w['w_out_r'] = f(inp['w_out'])[0].reshape(8, 128, 1024)
    k1 = f(inp['cmp_k_w1'])[0].reshape(32, 64, 256).transpose(1, 0, 2)
    v1 = f(inp['cmp_v_w1'])[0].reshape(32, 64, 256).transpose(1, 0, 2)
    w['w1r'] = np.ascontiguousarray(np.concatenate([k1, v1], axis=0))
    k2 = f(inp['cmp_k_w2'])[0].reshape(2, 128, 64).transpose(1, 0, 2)
    w['w2k_r'] = np.ascontiguousarray(np.concatenate([k2, k2], axis=2))
    w['w2v_r'] = np.ascontiguousarray(f(inp['cmp_v_w2'])[0].reshape(2, 128, 64).transpose(1, 0, 2))
    w['posT'] = np.ascontiguousarray(np.concatenate([f(inp['cmp_pos_k'])[0].T, f(inp['cmp_pos_v'])[0].T], axis=0))
    w['norm1_w'] = f(inp['norm1_w']).reshape(1, 1024)
    w['norm2_w'] = f(inp['norm2_w']).reshape(1, 1024)
    w['final_w'] = f(inp['final_norm_w']).reshape(1, 1024)
    w['onorm_w'] = np.concatenate([f(inp['nsa_out_norm_w']).reshape(1, 512), f(inp['sb_out_norm_w']).reshape(1, 512)], axis=1)
    w['gate_b'] = f(inp['gate_b']).reshape(1, 24)
    w['rel_bias'] = f(inp['rel_bias']).reshape(1, 256)
    cw = f(inp['conv_w'])[0]
    cb = f(inp['conv_b'])[0]
    cp = np.stack([cw[0], cw[1], cw[2], cb], axis=1).reshape(44, 128, 4).transpose(1, 0, 2)
    w['convp'] = np.ascontiguousarray(cp)
    w.update(_host_consts())
    return w


def build_program(nseq):
    nc = bass.Bass("TRN2", target_bir_lowering=False)
    S = Sched()
    dr = {}

    def din(name, shape):
        dr[name] = nc.dram_tensor(name, list(shape), F32, kind="ExternalInput").ap()
        return dr[name]
    x_d = din('x', (nseq, T, D))
    win_d = din('w_in_r', (29, 128, 8, 128))
    wup_d = din('w_up_r', (44, 128, 8, 128))
    wdn_d = din('w_down_r', (22, 128, 1024))
    wout_d = din('w_out_r', (8, 128, 1024))
    w1_d = din('w1r', (128, 32, 256))
    w2k_d = din('w2k_r', (128, 2, 128))
    w2v_d = din('w2v_r', (128, 2, 64))
    posT_d = din('posT', (128, 32))
    n1_d = din('norm1_w', (1, 1024))
    n2_d = din('norm2_w', (1, 1024))
    nf_d = din('final_w', (1, 1024))
    on_d = din('onorm_w', (1, 1024))
    gb_d = din('gate_b', (1, 24))
    rb_d = din('rel_bias', (1, 256))
    cp_d = din('convp', (128, 44, 4))
    cid_d = din('c_ident', (128, 128))
    ctri_d = din('c_tri', (128, 128))
    cstr_d = din('c_strict', (128, 128))
    cwc_d = din('c_wincorr', (128, 128))
    csel_d = din('c_sele', (32, 2048))
    covl_d = din('c_ovl', (127, 32))
    cadd_d = din('c_addc', (128, 512))
    cidx_d = din('c_idx', (128, 503))
    cmsk_d = din('c_mask', (128, 503))
    out_d = nc.dram_tensor("out", [nseq, T, D], F32, kind="ExternalOutput").ap()

    st = ExitStack()
    NEL = 105000
    arena_t = st.enter_context(nc.sbuf_tensor("arena", [128, NEL], BF16))
    A = Arena(arena_t, NEL)
    PS = [st.enter_context(nc.psum_tensor("ps%d" % i, [128, 512], F32)) for i in range(8)]

    def psf(b):
        return PS[b][:]

    def psb(b):
        return PS[b][:].bitcast(BF16)

    def mm(out, lhsT, rhs, start, stop, r, w, skip=False):
        S.op('pe', lambda e, o=out, l=lhsT, rr=rhs, s0=start, s1=stop, sk=skip: e.matmul(o, lhsT=l, rhs=rr, start=s0, stop=s1, skip_group_check=sk), r=r, w=w)

    def tp(out, in_, idn, r, w):
        S.op('pe', lambda e, o=out, i=in_, d=idn: e.transpose(o, i, d), r=r, w=w)

    def act(out, in_, func, r, w, bias=None, scale=None, accum=None, eng='act'):
        kw = {}
        if bias is not None:
            kw['bias'] = bias
        if scale is not None:
            kw['scale'] = scale
        if accum is not None:
            kw['accum_out'] = accum
        S.op('act', lambda e, o=out, i=in_, f=func, k=kw: e.activation(out=o, in_=i, func=f, **k), r=r, w=w)

    def tt(eng, out, in0, in1, op, r, w):
        S.op(eng, lambda e, o=out, a=in0, b=in1, p=op: e.tensor_tensor(out=o, in0=a, in1=b, op=p), r=r, w=w)

    def ts(eng, out, in0, s1, s2, op0, op1, r, w):
        if s2 is None:
            S.op(eng, lambda e, o=out, a=in0, s=s1, p=op0: e.tensor_single_scalar(out=o, in_=a, scalar=s, op=p), r=r, w=w)
        else:
            S.op(eng, lambda e, o=out, a=in0, x1=s1, x2=s2, p0=op0, p1=op1: e.tensor_scalar(out=o, in0=a, scalar1=x1, scalar2=x2, op0=p0, op1=p1), r=r, w=w)

    def stt(eng, out, in0, scalar, in1, op0, op1, r, w):
        S.op(eng, lambda e, o=out, a=in0, s=scalar, b=in1, p0=op0, p1=op1: e.scalar_tensor_tensor(out=o, in0=a, scalar=s, in1=b, op0=p0, op1=p1), r=r, w=w)

    def cp(eng, out, in_, r, w):
        if eng == 'act':
            S.op('act', lambda e, o=out, i=in_: e.copy(out=o, in_=i), r=r, w=w)
        else:
            S.op(eng, lambda e, o=out, i=in_: e.tensor_copy(out=o, in_=i), r=r, w=w)


    def rsq(vec, scale, key):
        act(vec, vec, AF.Sqrt, [key], [key], bias=EPS, scale=scale)
        S.op('dve', lambda e, v=vec: e.reciprocal(out=v, in_=v), r=[key], w=[key])

    def run_pipeline(items, lags):
        n = len(items)
        L = max(lags)
        for s_ in range(n + L):
            for j_, lag in enumerate(lags):
                k = s_ - lag
                if 0 <= k < n:
                    items[k][j_]()

    def memset(eng, ap, val, w):
        S.op(eng, lambda e, a=ap, v=val: e.memset(a, v), r=(), w=w)

    ident = A.alloc([128], BF16)
    tri = A.alloc([128], BF16)
    strict = A.alloc([128], BF16)
    wincorr = A.alloc([128], BF16)
    onescol = A.alloc([2], BF16)
    sele = A.alloc([16, 128], BF16)
    addc = A.alloc([16, 32], F32)
    corr = A.alloc([8, 256], BF16)
    bcm = A.alloc([8, 247], BF16)
    vcA = A.alloc([2, 97], BF16)
    b31c = A.alloc([8], F32)
    gateb = A.alloc([24], F32)
    convp = A.alloc([44, 4], F32)
    normw1 = A.alloc([1024], F32)
    normw2 = A.alloc([1024], F32)
    normwf = A.alloc([1024], F32)
    onormw = A.alloc([1024], F32)
    w2k = A.alloc([2, 128], BF16)
    w2v = A.alloc([2, 64], BF16)
    cbias = A.alloc([4], F32)
    posT = A.alloc([32], BF16)
    ws = [A.alloc([1024], BF16) for _ in range(NWS)]
    xbuf = [A.alloc([1024], F32) for _ in range(2)]
    ubuf = [A.alloc([1024], BF16) for _ in range(2)]
    junk = A.alloc([1024], BF16)
    ss = A.alloc([16], F32)
    rstd = A.alloc([16], F32)
    halo = A.alloc([44, 2], F32)
    m_common = A.mark()

    def ld(eng, dst, src, slot, wkey):
        S.dma(eng, dst, src, slot=slot, w=[wkey])
    ld('pool', ident, cid_d, 'c0', 'ident')
    ld('pool', tri, ctri_d, 'c1', 'tri')
    ld('pool', strict, cstr_d, 'c2', 'strict')
    ld('pool', wincorr, cwc_d, 'c3', 'wincorr')
    memset('dve', sele, 0.0, ['sele'])
    ld('pool', sele[0:32].rearrange("p a b -> p (a b)"), csel_d, 'c4', 'sele')
    ld('sp', addc.rearrange("p a b -> p (a b)"), cadd_d, 'c5', 'addc')
    ld('pool', vcA[0:127, 0, 65:97], covl_d, 'c6', 'vcA')
    ld('pool', vcA[0:127, 1, 65:97], covl_d, 'c7', 'vcA')
    ld('sp', gateb, gb_d[0:1, :].partition_broadcast(128), 'c8', 'gateb')
    ld('sp', convp.rearrange("p a b -> p (a b)"), cp_d.rearrange("p a b -> p (a b)"), 'c9', 'convp')
    ld('sp', normw1, n1_d[0:1, :].partition_broadcast(128), 'c10', 'normw1')
    ld('sp', normw2, n2_d[0:1, :].partition_broadcast(128), 'c11', 'normw2')
    ld('sp', normwf, nf_d[0:1, :].partition_broadcast(128), 'c12', 'normwf')
    ld('sp', onormw, on_d[0:1, :].partition_broadcast(128), 'c13', 'onormw')
    ld('pool', w2k.rearrange("p a b -> p (a b)"), w2k_d.rearrange("p a b -> p (a b)"), 'c14', 'w2k')
    ld('pool', w2v.rearrange("p a b -> p (a b)"), w2v_d.rearrange("p a b -> p (a b)"), 'c15', 'w2v')
    ld('pool', posT, posT_d, 'c16', 'posT')
    memset('dve', onescol, 1.0, ['onescol'])
    memset('dve', vcA[:, :, 64:65], 1.0, ['vcA'])
    memset('dve', junk, 0.0, ['junk'])

    m0 = A.mark()
    RB = A.alloc([32, 8], F32)
    idxt = A.alloc([503], F32)
    mskt = A.alloc([503], F32)
    accb = A.alloc([8, 503], F32)
    eqm = A.alloc([503], F32)
    ld('sp', RB.rearrange("p a b -> p (a b)"), rb_d[0:1, :].partition_broadcast(128), 'c17', 'RB')
    ld('sp', idxt, cidx_d, 'c18', 'idxt')
    ld('sp', mskt, cmsk_d, 'c19', 'mskt')
    cp('dve', b31c, RB[:, 31, :], ['RB'], ['b31c'])
    tt('dve', RB, RB, b31c.unsqueeze(1).to_broadcast([128, 32, 8]), ALU.subtract, ['RB', 'b31c'], ['RB'])
    memset('dve', accb, 0.0, ['accb'])
    for k in range(31):
        ts('dve', eqm, idxt, float(k), None, ALU.is_equal, None, ['idxt'], ['eqm'])
        for h in range(8):
            stt('dve', accb[:, h, :], eqm, RB[:, k, h:h + 1], accb[:, h, :], ALU.mult, ALU.add, ['eqm', 'RB', 'accb'], ['accb'])
    for h in range(8):
        tt('dve', corr[:, h, :], accb[:, h, 0:256], mskt[:, 0:256], ALU.add, ['accb', 'mskt'], ['corr'])
        tt('dve', bcm[:, h, :], accb[:, h, 256:503], mskt[:, 256:503], ALU.add, ['accb', 'mskt'], ['bcm'])
    S.barrier()
    A.release(m0)

    plan = []
    for b in range(nseq):
        for k in range(29):
            plan.append(win_d[k].rearrange("p a b -> p (a b)"))
        for blk in range(2):
            for j in range(22):
                plan.append(wup_d[j].rearrange("p a b -> p (a b)"))
                plan.append(wup_d[22 + j].rearrange("p a b -> p (a b)"))
    wstate = {'issued': 0, 'next': 0}

    def w_issue():
        k = wstate['issued']
        if k < len(plan):
            S.dma('pool', ws[k % NWS], plan[k], slot='ws%d' % (k % NWS), w=[('ws', k % NWS)])
            wstate['issued'] = k + 1

    def w_get():
        k = wstate['next']
        wstate['next'] = k + 1
        assert k < wstate['issued']
        return ws[k % NWS].rearrange("p (a b) -> p a b", a=8), ('ws', k % NWS)

    for _ in range(NWS):
        w_issue()


    for b in range(nseq):
        m_seq = A.mark()
        mixed = A.alloc([16, 1024], BF16)
        m_ab = A.mark()
        uT = A.alloc([8, 2048], BF16)
        qkv_m = A.mark()

        memset('dve', ss, 0.0, ['ss'])
        for i in range(16):
            xb = xbuf[i % 2]
            ub = ubuf[i % 2]
            S.dma('sp', xb, x_d[b, 128 * i:128 * i + 128, :], slot='x%d' % (i % 2), w=[('xbuf', i % 2)])
            act(junk, xb, AF.Square, [('xbuf', i % 2)], ['junk', 'ss'], accum=ss[:, i:i + 1])
            cp('dve', rstd[:, i:i + 1], ss[:, i:i + 1], ['ss'], ['rstd'])
            rsq(rstd[:, i:i + 1], 1.0 / D, 'rstd')
            stt('dve', ub, xb, rstd[:, i:i + 1], normw1, ALU.mult, ALU.mult, [('xbuf', i % 2), 'rstd', 'normw1'], [('ub', i % 2)])
            bk = i % 2
            for dc in range(8):
                tp(psb(bk)[:, dc * 128:(dc + 1) * 128], ub[:, dc * 128:(dc + 1) * 128], ident, [('ub', i % 2), 'ident'], [('ps', bk)])
            cp('act', uT[:, :, 128 * i:128 * i + 128], psb(bk).rearrange("p (a b) -> p a b", a=8), [('ps', bk)], ['uT'])

        def proj_F(dst_fn, scale, keyw):
            wch, wk = w_get()
            for Q in range(4):
                bk = 2 + (proj_F.n % 4)
                proj_F.n += 1
                for dc in range(8):
                    mm(psf(bk), wch[:, dc, :], uT[:, dc, 512 * Q:512 * Q + 512], dc == 0, dc == 7, [wk, 'uT'], [('ps', bk)])
                dst = dst_fn(Q)
                if proj_F.n % 2 == 0:
                    act(dst, psf(bk), AF.Copy, [('ps', bk)], [keyw], scale=scale)
                else:
                    ts('dve', dst, psf(bk), scale, None, ALU.mult, None, [('ps', bk)], [keyw])
            w_issue()
        proj_F.n = 0

        def proj_T(evac_fn, ncols=128):
            wch, wk = w_get()
            for tg in range(4):
                bk = 2 + (proj_F.n % 4)
                proj_F.n += 1
                for tl in range(4):
                    i = 4 * tg + tl
                    for dc in range(8):
                        mm(psf(bk)[:, tl * 128:tl * 128 + ncols], uT[:, dc, 128 * i:128 * i + 128], wch[:, dc, 0:ncols], dc == 0, dc == 7, [wk, 'uT'], [('ps', bk)])
                evac_fn(tg, bk)
            w_issue()

        qnT = A.alloc([4, 2048], BF16)
        ksT = A.alloc([4, 2048], BF16)
        kwT = A.alloc([4, 2048], BF16)
        vsA = A.alloc([16, 130], BF16)
        vwA = A.alloc([16, 130], BF16)
        gT = A.alloc([16, 24], F32)
        kcmpT = A.alloc([2, 127], BF16)
        work_m = A.mark()
        kcvcT = A.alloc([2, 2048], BF16)
        memset('dve', vsA, 1.0, ['vsA'])
        memset('dve', vwA, 1.0, ['vwA'])
        for c in range(4):
            proj_F(lambda Q, c=c: qnT[:, c, 512 * Q:512 * Q + 512], 0.125, 'qnT')
        for g in range(2):
            proj_F(lambda Q, g=g: kcvcT[:, g, 512 * Q:512 * Q + 512], 1.0, 'kcvcT')
        for gh in range(4):
            proj_F(lambda Q, gh=gh: ksT[:, gh, 512 * Q:512 * Q + 512], 1.0, 'ksT')
        for gh in range(4):
            proj_F(lambda Q, gh=gh: kwT[:, gh, 512 * Q:512 * Q + 512], 1.0, 'kwT')

        def evac_v(dstA, key):
            def f(tg, bk):
                src = psf(bk).rearrange("p (a g d) -> p a g d", a=4, g=2)
                dst = dstA[:, 4 * tg:4 * tg + 4, :].rearrange("p a (g e) -> p a g e", g=2)[:, :, :, 0:64]
                cp('dve', dst, src, [('ps', bk)], [key])
            return f
        proj_T(evac_v(vsA, 'vsA'))
        proj_T(evac_v(vwA, 'vwA'))

        def evac_g(tg, bk):
            src = psf(bk).rearrange("p (a c) -> p a c", a=4)[:, :, 0:24]
            dst = gT[:, 4 * tg:4 * tg + 4, :]
            tt('dve', dst, src, gateb.unsqueeze(1).to_broadcast([128, 4, 24]), ALU.add, [('ps', bk), 'gateb'], ['gT'])
            act(dst, dst, AF.Sigmoid, ['gT'], ['gT'])
        proj_T(evac_g, ncols=24)

        w1sb = A.alloc([32, 256], BF16)
        S.dma('pool', w1sb.rearrange("p a b -> p (a b)"), w1_d.rearrange("p a b -> p (a b)"), slot='w1', w=['w1sb'])
        geluT = A.alloc([2, 127], BF16)
        gx = A.alloc([127], F32)
        gt_ = A.alloc([127], F32)
        for kv in range(2):
            rows = slice(64 * kv, 64 * kv + 64)
            for hcc in range(2):
                bk = 0
                for i in range(32):
                    mm(psf(bk)[:, 0:1], w1sb[rows, i, hcc * 128:hcc * 128 + 128], posT[rows, i:i + 1], i == 0, i == 31, ['w1sb', 'posT'], [('ps', bk)])
                cp('dve', cbias[:, kv * 2 + hcc:kv * 2 + hcc + 1], psf(bk)[:, 0:1], [('ps', bk)], ['cbias'])
        for g in range(2):
            for kv in range(2):
                rows = slice(64 * kv, 64 * kv + 64)
                for hcc in range(2):
                    bk = hcc
                    for i in range(32):
                        mm(psf(bk)[:, 0:127], w1sb[rows, i, hcc * 128:hcc * 128 + 128], kcvcT[rows, g, i:i + 16 * 126 + 1:16], i == 0, i == 31, ['w1sb', 'kcvcT'], [('ps', bk)])
                    ts('dve', gx, psf(bk)[:, 0:127], cbias[:, kv * 2 + hcc:kv * 2 + hcc + 1], None, ALU.add, None, [('ps', bk), 'cbias'], ['gx'])
                    tt('dve', gt_, gx, gx, ALU.mult, ['gx'], ['gt'])
                    ts('dve', gt_, gt_, 0.044715, 1.0, ALU.mult, ALU.add, ['gt'], ['gt'])
                    tt('dve', gt_, gt_, gx, ALU.mult, ['gt', 'gx'], ['gt'])
                    act(gt_, gt_, AF.Sigmoid, ['gt'], ['gt'], scale=1.5957691216057308)
                    tt('dve', geluT[:, hcc, :], gx, gt_, ALU.mult, ['gx', 'gt'], ['geluT'])
                bk = 2
                if kv == 0:
                    for hcc in range(2):
                        mm(psf(bk)[:, 0:127], w2k[:, hcc, :], geluT[:, hcc, :], hcc == 0, hcc == 1, ['w2k', 'geluT'], [('ps', bk)])
                    cp('dve', kcmpT[:, g, :], psf(bk)[:, 0:127], [('ps', bk)], ['kcmpT'])
                else:
                    for hcc in range(2):
                        mm(psf(bk)[0:127, 0:64], geluT[:, hcc, :], w2v[:, hcc, :], hcc == 0, hcc == 1, ['w2v', 'geluT'], [('ps', bk)])
                    cp('dve', vcA[0:127, g, 0:64], psf(bk)[0:127, 0:64], [('ps', bk)], ['vcA'])
        S.barrier()
        A.release(work_m)
        kcmp_keep = kcmpT
        att_m = A.mark()

        Pc = [A.alloc([127], BF16) for _ in range(2)]
        PcT = [A.alloc([128], BF16) for _ in range(2)]
        PT = [A.alloc([512], BF16) for _ in range(3)]
        onsa2 = [A.alloc([16, 64], F32) for _ in range(2)]
        tmpo = A.alloc([4, 64], F32)
        impa2 = [A.alloc([4, 32], F32) for _ in range(2)]
        score = A.alloc([4, 32], F32)
        top8 = A.alloc([8], F32)
        thr = A.alloc([1], F32)
        negm = A.alloc([4, 32], BF16)
        negmT2 = [A.alloc([512], BF16) for _ in range(2)]
        rd = A.alloc([4], F32)
        sc = A.alloc([4], F32)
        sqt = A.alloc([16, 64], F32)
        ssh = A.alloc([16], F32)
        obn = [0]
        build_program.nsa_off = A.off
        for g in range(2):
            memset('dve', negmT2[g], 0.0, [('negmT', g)])

        def finalize(ob, h, branch, Q, with_imp=False):
            g_ = h // 4
            onsa = onsa2[g_]
            impa = impa2[g_]
            O = psf(ob).rearrange("p (a c) -> p a c", a=4)
            r_ = h % 4
            ts('dve', rd, O[:, :, 64], 1e-30, None, ALU.max, None, [('ps', ob)], ['rd'])
            S.op('dve', lambda e: e.reciprocal(out=rd, in_=rd), r=['rd'], w=['rd'])
            tt('dve', sc, rd, gT[:, 4 * Q:4 * Q + 4, 3 * h + branch], ALU.mult, ['rd', 'gT'], ['sc'])
            tt('dve', tmpo, O[:, :, 0:64], sc.unsqueeze(2).to_broadcast([128, 4, 64]), ALU.mult, [('ps', ob), 'sc'], ['tmpo'])
            ov = onsa.rearrange("p (a r) d -> p a r d", a=4)[:, :, r_, :]
            tt('dve', ov, ov, tmpo, ALU.add, ['tmpo', ('onsa', g_)], [('onsa', g_)])
            if with_imp:
                tt('dve', score, O[:, :, 65:97], rd.unsqueeze(2).to_broadcast([128, 4, 32]), ALU.mult, [('ps', ob), 'rd'], ['score'])
                tt('dve', impa, impa, score, ALU.add, ['score', ('impa', g_)], [('impa', g_)])

        def headnorm_nsa(Q, g_):
            onsa = onsa2[g_]
            tt('dve', sqt, onsa, onsa, ALU.mult, [('onsa', g_)], ['sqt'])
            S.op('dve', lambda e: e.tensor_reduce(out=ssh, in_=sqt, axis=AX.X, op=ALU.add), r=['sqt'], w=['ssh'])
            rsq(ssh, 1.0 / 64, 'ssh')
            tt('dve', sqt, onsa, ssh.unsqueeze(2).to_broadcast([128, 16, 64]), ALU.mult, [('onsa', g_), 'ssh', 'sqt'], ['sqt'])
            tt('dve', mixed[:, 4 * Q:4 * Q + 4, 256 * g_:256 * g_ + 256], sqt.rearrange("p (a r) d -> p a (r d)", a=4),
               onormw[:, 256 * g_:256 * g_ + 256].unsqueeze(1).to_broadcast([128, 4, 256]), ALU.mult, ['sqt', 'onormw'], ['mixed'])

        for Q in range(4):
            for g in range(2):
                memset('dve', onsa2[g], 0.0, [('onsa', g)])
                memset('dve', impa2[g], 0.0, [('impa', g)])
            items = []
            for g in range(2):
                for r_ in range(4):
                    h = 4 * g + r_
                    ob = 6 + (obn[0] % 2)
                    obn[0] += 1
                    for tl in range(4):
                        k_ = len(items)
                        def mk(g=g, h=h, ob=ob, tl=tl, k_=k_, Q=Q):
                            hf = slice(64 * (h % 2), 64 * (h % 2) + 64)
                            i = 4 * Q + tl
                            bk = k_ % 2
                            pc = Pc[k_ % 2]
                            pct = PcT[k_ % 2]
                            tb_ = 3 + (k_ % 2)
                            O = psf(ob).rearrange("p (a c) -> p a c", a=4)

                            def s1():
                                mm(psf(bk)[:, 0:127], qnT[hf, h // 2, 128 * i:128 * i + 128], kcmp_keep[hf, g, :], True, False, ['qnT', 'kcmpT'], [('ps', bk)])
                                mm(psf(bk)[:, 0:127], ident, bcm[:, h, 120 - 8 * i:120 - 8 * i + 127], False, True, ['ident', 'bcm'], [('ps', bk)])

                            def s2():
                                act(pc, psf(bk)[:, 0:127], AF.Exp, [('ps', bk), 'b31c'], [('Pc', k_ % 2)], bias=b31c[:, h:h + 1])

                            def s3():
                                tp(psb(tb_)[0:127, 0:128], pc, ident, [('Pc', k_ % 2), 'ident'], [('ps', tb_)])

                            def s4():
                                cp('dve', pct[0:127, :], psb(tb_)[0:127, 0:128], [('ps', tb_)], [('PcT', k_ % 2)])

                            def s5():
                                mm(O[:, tl, 0:97], pct[0:127, :], vcA[0:127, g, :], True, True, [('PcT', k_ % 2), 'vcA'], [('ps', ob)])
                                if tl == 3:
                                    finalize(ob, h, 0, Q, with_imp=True)
                            return [s1, s2, s3, s4, s5]
                        items.append(mk())
            run_pipeline(items, [0, 1, 2, 3, 4])
            for g in range(2):
                tt('dve', score, impa2[g], addc[:, 4 * Q:4 * Q + 4, :], ALU.add, [('impa', g), 'addc'], ['score'])
                for tl in range(4):
                    S.op('dve', lambda e, tl=tl: e.max(out=top8, in_=score[:, tl, :]), r=['score'], w=['top8'])
                    ts('dve', thr, top8[:, 7:8], -5e29, None, ALU.max, None, ['top8'], ['thr'])
                    ts('dve', negm[:, tl, :], score[:, tl, :], thr[:, 0:1], NEG, ALU.is_lt, ALU.mult, ['score', 'thr'], ['negm'])
                    tp(psb(2)[0:32, tl * 128:tl * 128 + 128], negm[:, tl, :], ident, ['negm', 'ident'], [('ps', 2)])
                cp('dve', negmT2[g][0:32, :], psb(2)[0:32, 0:512], [('ps', 2)], [('negmT', g)])
            items = []
            for g in range(2):
                for branch in (2, 1):
                    kT = ksT if branch == 1 else kwT
                    vA = vsA if branch == 1 else vwA
                    kkey = 'ksT' if branch == 1 else 'kwT'
                    vkey = 'vsA' if branch == 1 else 'vwA'
                    for r_ in range(4):
                        h = 4 * g + r_
                        ob = 6 + (obn[0] % 2)
                        obn[0] += 1
                        c_lo = 0 if branch == 1 else max(0, 4 * Q - 4)
                        for c in range(c_lo, 4 * Q + 4):
                            k_ = len(items)
                            last_item = (branch == 1 and r_ == 3 and c == 4 * Q + 3)
                            def mk(g=g, branch=branch, kT=kT, vA=vA, kkey=kkey, vkey=vkey, h=h, ob=ob, c=c, c_lo=c_lo, k_=k_, Q=Q, last_item=last_item):
                                hf = slice(64 * (h % 2), 64 * (h % 2) + 64)
                                O = psf(ob).rearrange("p (a c) -> p a c", a=4)
                                qt_lo = max(c, 4 * Q)
                                qt_hi = 4 * Q + 3 if branch == 1 else min(c + 4, 4 * Q + 3)
                                lo = 128 * (qt_lo - 4 * Q)
                                hi = 128 * (qt_hi - 4 * Q + 1)
                                bk = 3 + (k_ % 3)
                                pt = PT[k_ % 3]
                                pk = ('PT', k_ % 3)

                                def s1():
                                    extra = []
                                    if branch == 1:
                                        extra.append(('m', lo, hi))
                                    for qt in range(qt_lo, qt_hi + 1):
                                        o_ = qt - c
                                        cl = 128 * (qt - 4 * Q)
                                        if o_ <= 1:
                                            extra.append(('c', cl, o_))
                                        if branch == 2 and o_ == 4:
                                            extra.append(('w', cl, 0))
                                    mm(psf(bk)[:, lo:hi], kT[:, 2 * g + (h % 2), 128 * c:128 * c + 128], qnT[:, h // 2, 512 * Q + lo:512 * Q + hi], True, len(extra) == 0, [kkey, 'qnT'], [('ps', bk)])
                                    for j_, ex in enumerate(extra):
                                        last = j_ == len(extra) - 1
                                        if ex[0] == 'm':
                                            mm(psf(bk)[:, lo:hi], sele[:, c, :], negmT2[g][:, lo:hi], False, last, ['sele', ('negmT', g)], [('ps', bk)])
                                        elif ex[0] == 'c':
                                            mm(psf(bk)[:, ex[1]:ex[1] + 128], ident, corr[:, h, 128 * ex[2]:128 * ex[2] + 128], False, last, ['ident', 'corr'], [('ps', bk)])
                                        else:
                                            mm(psf(bk)[:, ex[1]:ex[1] + 128], ident, wincorr, False, last, ['ident', 'wincorr'], [('ps', bk)])

                                def s2():
                                    act(pt[:, lo:hi], psf(bk)[:, lo:hi], AF.Exp, [('ps', bk), 'b31c'], [pk], bias=b31c[:, h:h + 1])

                                def s3():
                                    if c == c_lo:
                                        memset('dve', psf(ob), 0.0, [('ps', ob)])
                                    for qt in range(qt_lo, qt_hi + 1):
                                        tl = qt - 4 * Q
                                        mm(O[:, tl, 0:65], pt[:, 128 * tl:128 * tl + 128], vA[:, c, 65 * g:65 * g + 65], False, c == qt, [pk, vkey], [('ps', ob)], skip=True)
                                    if c == 4 * Q + 3:
                                        finalize(ob, h, branch, Q)
                                    if last_item:
                                        headnorm_nsa(Q, g)
                                return [s1, s2, s3]
                            items.append(mk())
            run_pipeline(items, [0, 1, 2])


        S.barrier()
        A.release(qkv_m)

        qsT = A.alloc([4, 2048], BF16)
        ksbT = A.alloc([4, 2048], BF16)
        vS = A.alloc([16, 512], BF16)
        for c in range(4):
            proj_F(lambda Q, c=c: qsT[:, c, 512 * Q:512 * Q + 512], 0.125, 'qsT')
        for c in range(4):
            proj_F(lambda Q, c=c: ksbT[:, c, 512 * Q:512 * Q + 512], 1.0, 'ksbT')
        for c in range(4):
            def evac_vs(tg, bk, c=c):
                src = psf(bk).rearrange("p (a d) -> p a d", a=4)
                cp('dve', vS[:, 4 * tg:4 * tg + 4, 128 * c:128 * c + 128], src, [('ps', bk)], ['vS'])
            proj_T(evac_vs)

        Eb = [A.alloc([512], BF16) for _ in range(3)]
        SPb = [A.alloc([512], BF16) for _ in range(4)]
        Xb = [A.alloc([512], BF16) for _ in range(2)]
        Pb = [A.alloc([512], BF16) for _ in range(2)]
        osb = A.alloc([32, 64], F32)
        carry = A.alloc([4], F32)
        gsc = A.alloc([4], F32)
        tmps = A.alloc([4, 64], F32)
        sq2 = A.alloc([32, 64], F32)
        ss2h = A.alloc([32], F32)
        build_program.sb_off = A.off
        items = []
        for Q in range(4):
            for h in range(8):
                for c in range(4 * Q + 3, -1, -1):
                    k_ = len(items)
                    def mk(Q=Q, h=h, c=c, k_=k_):
                        hf = slice(64 * (h % 2), 64 * (h % 2) + 64)
                        accv = osb.rearrange("p (a h) d -> p a h d", a=4)[:, :, h, :]
                        qt_lo = max(c, 4 * Q)
                        lo = 128 * (qt_lo - 4 * Q)
                        hi = 512
                        zb = k_ % 2
                        cb_ = 2 + k_ % 2
                        rb_ = 4 + k_ % 2
                        E = Eb[k_ % 3]
                        SP = SPb[k_ % 4]
                        X = Xb[k_ % 2]
                        P = Pb[k_ % 2]
                        kE, kS, kX, kP = ('E', k_ % 3), ('SP', k_ % 4), ('X', k_ % 2), ('P', k_ % 2)
                        diag = c >= 4 * Q
                        R = psf(rb_).rearrange("p (a c) -> p a c", a=4)
                        tl0 = qt_lo - 4 * Q
                        first = (c == 4 * Q + 3)

                        def s_g():
                            if not first:
                                act(gsc, carry, AF.Exp, ['carry'], ['gsc'], scale=-1.0)

                        def s1():
                            mm(psf(zb)[:, lo:hi], ksbT[hf, h // 2, 128 * c:128 * c + 128], qsT[hf, h // 2, 512 * Q + lo:512 * Q + hi], True, not diag, ['ksbT', 'qsT'], [('ps', zb)])
                            if diag:
                                mm(psf(zb)[:, lo:lo + 128], ident, strict, False, True, ['ident', 'strict'], [('ps', zb)])

                        def s2():
                            act(E[:, lo:hi], psf(zb)[:, lo:hi], AF.Exp, [('ps', zb)], [kE])
                            act(SP[:, lo:hi], E[:, lo:hi], AF.Ln, [kE], [kS], bias=1.0)

                        def s3():
                            mm(psf(cb_)[:, lo:hi], tri, SP[:, lo:hi], True, True, ['tri', kS], [('ps', cb_)])

                        def s4():
                            act(X[:, lo:hi], psf(cb_)[:, lo:hi], AF.Exp, [('ps', cb_)], [kX], scale=-1.0)

                        def s5():
                            tt('dve', P[:, lo:hi], E[:, lo:hi], X[:, lo:hi], ALU.mult, [kE, kX], [kP])

                        def s6():
                            for tl in range(tl0, 4):
                                mm(R[:, tl, 0:64], P[:, 128 * tl:128 * tl + 128], vS[:, c, 64 * h:64 * h + 64], True, True, [kP, 'vS'], [('ps', rb_)])
                                mm(R[:, tl, 64:65], SP[:, 128 * tl:128 * tl + 128], onescol[:, 0:1], True, True, [kS, 'onescol'], [('ps', rb_)])

                        def s7():
                            if first and h == 0:
                                memset('dve', osb, 0.0, ['osb'])
                            if first:
                                memset('dve', carry, 0.0, ['carry'])
                                memset('dve', gsc, 1.0, ['gsc'])
                            tt('dve', tmps[:, tl0:4, :], R[:, tl0:4, 0:64], gsc[:, tl0:4].unsqueeze(2).to_broadcast([128, 4 - tl0, 64]), ALU.mult, [('ps', rb_), 'gsc'], ['tmps'])
                            tt('dve', accv[:, tl0:4, :], accv[:, tl0:4, :], tmps[:, tl0:4, :], ALU.add, ['tmps', 'osb'], ['osb'])
                            if c > 0:
                                tt('dve', carry[:, tl0:4], carry[:, tl0:4], R[:, tl0:4, 64], ALU.add, [('ps', rb_), 'carry'], ['carry'])
                            if c == 0 and h == 7:
                                tt('dve', sq2, osb, osb, ALU.mult, ['osb'], ['sq2'])
                                S.op('dve', lambda e: e.tensor_reduce(out=ss2h, in_=sq2, axis=AX.X, op=ALU.add), r=['sq2'], w=['ss2h'])
                                rsq(ss2h, 1.0 / 64, 'ss2h')
                                tt('dve', sq2, osb, ss2h.unsqueeze(2).to_broadcast([128, 32, 64]), ALU.mult, ['osb', 'ss2h', 'sq2'], ['sq2'])
                                tt('dve', mixed[:, 4 * Q:4 * Q + 4, 512:1024], sq2.rearrange("p (a h) d -> p a (h d)", a=4),
                                   onormw[:, 512:1024].unsqueeze(1).to_broadcast([128, 4, 512]), ALU.mult, ['sq2', 'onormw'], ['mixed'])
                        return [s1, s2, s3, s4, s5, s6, s_g, s7]
                    items.append(mk())
        run_pipeline(items, [0, 1, 2, 3, 3, 4, 5, 5])


        S.barrier()
        A.release(m_ab)

        u2T = A.alloc([8, 2048], BF16)
        m_c1 = A.mark()
        wout = A.alloc([8, 1024], BF16)
        mT = A.alloc([8, 128], BF16)
        hbuf = A.alloc([1024], F32)
        S.dma('pool', wout, wout_d.rearrange("c p n -> p c n"), slot='wout', w=['wout'])
        memset('dve', ss, 0.0, ['ss'])
        for i in range(16):
            xb = xbuf[i % 2]
            ub = ubuf[i % 2]
            S.dma('sp', xb, x_d[b, 128 * i:128 * i + 128, :], slot='x%d' % (i % 2), w=[('xbuf', i % 2)])
            bk = i % 2
            for dc in range(8):
                tp(psb(bk)[:, dc * 128:(dc + 1) * 128], mixed[:, i, dc * 128:(dc + 1) * 128], ident, ['mixed', 'ident'], [('ps', bk)])
            cp('act', mT, psb(bk).rearrange("p (a b) -> p a b", a=8), [('ps', bk)], ['mT'])
            for half in range(2):
                pb = 2 + half
                for c in range(8):
                    mm(psf(pb), mT[:, c, :], wout[:, c, 512 * half:512 * half + 512], c == 0, c == 7, ['mT', 'wout'], [('ps', pb)])
                tt('dve', hbuf[:, 512 * half:512 * half + 512], psf(pb), xb[:, 512 * half:512 * half + 512], ALU.add, [('ps', pb), ('xbuf', i % 2)], ['hbuf'])
            S.dma('sp', out_d[b, 128 * i:128 * i + 128, :], hbuf, slot='hst', r=['hbuf'], w=[('outh', i)])
            act(junk, hbuf, AF.Square, ['hbuf'], ['junk', 'ss'], accum=ss[:, i:i + 1])
            cp('dve', rstd[:, i:i + 1], ss[:, i:i + 1], ['ss'], ['rstd'])
            rsq(rstd[:, i:i + 1], 1.0 / D, 'rstd')
            stt('dve', ub, hbuf, rstd[:, i:i + 1], normw2, ALU.mult, ALU.mult, ['hbuf', 'rstd', 'normw2'], [('ub', i % 2)])
            bk2 = 4 + i % 2
            for dc in range(8):
                tp(psb(bk2)[:, dc * 128:(dc + 1) * 128], ub[:, dc * 128:(dc + 1) * 128], ident, [('ub', i % 2), 'ident'], [('ps', bk2)])
            cp('act', u2T[:, :, 128 * i:128 * i + 128], psb(bk2).rearrange("p (a b) -> p a b", a=8), [('ps', bk2)], ['u2T'])
        S.barrier()
        A.release(m_seq)
        actT_lo = A.alloc([16, 1024], BF16)
        assert A.off == m_ab
        A.off = m_c1
        actT_hi = A.alloc([6, 1024], BF16)
        wdn = A.alloc([22, 1024], BF16)
        accg = [A.alloc([512], F32) for _ in range(2)]
        accv_ = [A.alloc([512], F32) for _ in range(2)]
        sil = [A.alloc([512], F32) for _ in range(2)]
        obuf = A.alloc([1024], F32)
        fx = A.alloc([8], F32)

        def actT(j):
            return actT_lo[:, j, :] if j < 16 else actT_hi[:, j - 16, :]

        memset('dve', halo, 0.0, ['halo'])
        for blk in range(2):
            S.dma('pool', wdn, wdn_d.rearrange("j p n -> p j n"), slot='wdn', w=['wdn'])
            for j in range(22):
                wg, wgk = w_get()
                wv, wvk = w_get()
                for tb in range(2):
                    n_ = (j * 2 + tb) % 2
                    col0 = 1024 * blk + 512 * tb
                    for which, wch, wk, pb, accs, fc in ((0, wg, wgk, 0 + n_, accg, j), (1, wv, wvk, 2 + n_, accv_, 22 + j)):
                        for dc in range(8):
                            mm(psf(pb), wch[:, dc, :], u2T[:, dc, col0:col0 + 512], dc == 0, dc == 7, [wk, 'u2T'], [('ps', pb)])
                        ac = accs[n_]
                        ak = ('acc', which, n_)
                        G = psf(pb)
                        S.op('act', lambda e, o=ac, i=G, fc=fc: e.activation(out=o, in_=i, func=AF.Identity, bias=convp[:, fc, 3:4], scale=convp[:, fc, 2:3]),
                             r=[('ps', pb), 'convp'], w=[ak])
                        stt('dve', ac[:, 1:512], G[:, 0:511], convp[:, fc, 1:2], ac[:, 1:512], ALU.mult, ALU.add, [('ps', pb), 'convp', ak], [ak])
                        stt('dve', ac[:, 2:512], G[:, 0:510], convp[:, fc, 0:1], ac[:, 2:512], ALU.mult, ALU.add, [('ps', pb), 'convp', ak], [ak])
                        stt('dve', ac[:, 0:1], halo[:, fc, 1:2], convp[:, fc, 1:2], ac[:, 0:1], ALU.mult, ALU.add, ['halo', 'convp', ak], [ak])
                        stt('dve', ac[:, 0:2], halo[:, fc, 0:2], convp[:, fc, 0:1], ac[:, 0:2], ALU.mult, ALU.add, ['halo', 'convp', ak], [ak])
                        cp('dve', halo[:, fc, :], G[:, 510:512], [('ps', pb)], ['halo'])
                    act(sil[n_], accg[n_], AF.Silu, [('acc', 0, n_)], [('sil', n_)])
                    tt('pool', actT(j)[:, 512 * tb:512 * tb + 512], sil[n_], accv_[n_], ALU.mult, [('sil', n_), ('acc', 1, n_)], ['actT'])
                w_issue()
                w_issue()
            memset('dve', ss, 0.0, ['ss'])
            for tl in range(8):
                i = 8 * blk + tl
                xb = xbuf[i % 2]
                S.dma('sp', xb, out_d[b, 128 * i:128 * i + 128, :], slot='x%d' % (i % 2), r=[('outh', i)], w=[('xbuf', i % 2)])
                for half in range(2):
                    pb = 4 + (2 * tl + half) % 4
                    for j in range(22):
                        mm(psf(pb), actT(j)[:, 128 * tl:128 * tl + 128], wdn[:, j, 512 * half:512 * half + 512], j == 0, j == 21, ['actT', 'wdn'], [('ps', pb)])
                    tt('dve', xb[:, 512 * half:512 * half + 512], psf(pb), xb[:, 512 * half:512 * half + 512], ALU.add, [('ps', pb), ('xbuf', i % 2)], [('xbuf', i % 2)])
                act(junk, xb, AF.Square, [('xbuf', i % 2)], ['junk', 'ss'], accum=ss[:, tl:tl + 1])
                cp('dve', fx[:, tl:tl + 1], ss[:, tl:tl + 1], ['ss'], ['fx'])
                rsq(fx[:, tl:tl + 1], 1.0 / D, 'fx')
                stt('dve', obuf, xb, fx[:, tl:tl + 1], normwf, ALU.mult, ALU.mult, [('xbuf', i % 2), 'fx', 'normwf'], ['obuf'])
                S.dma('sp', out_d[b, 128 * i:128 * i + 128, :], obuf, slot='ost', r=['obuf'], w=[('outf', i)])
        S.barrier()
        A.release(m_seq)

    S.barrier()
    build_program.info = {'peak': A.peak, 'ops': dict(S.cnt)}
    S.emit(nc, st)
    st.close()
    return nc


_CACHE = {}


def kernel(**inputs):
    x = np.ascontiguousarray(np.asarray(inputs['x'], dtype=np.float32))
    B = x.shape[0]
    nseq = B // NCORE
    w = _prep_weights(inputs)
    if nseq not in _CACHE:
        _CACHE[nseq] = build_program(nseq)
    nc = _CACHE[nseq]
    in_maps = []
    for c in range(NCORE):
        m = dict(w)
        m['x'] = np.ascontiguousarray(x[c * nseq:(c + 1) * nseq])
        in_maps.append(m)
    res = run_bass_kernel_spmd(nc, in_maps, core_ids=list(range(NCORE)))
    out = np.concatenate([np.asarray(r['out'], dtype=np.float32) for r in res.results], axis=0)
    return out
```

```python
import numpy as np
from contextlib import ExitStack
import concourse.bass as bass
import concourse.mybir as mybir
from concourse.bass_utils import run_bass_kernel_spmd

F32 = mybir.dt.float32
BF16 = mybir.dt.bfloat16
AF = mybir.ActivationFunctionType
ALU = mybir.AluOpType
AX = mybir.AxisListType

T = 2048
D = 1024
DFF = 2816
NCORE = 8
NEG = -30000.0
EPS = 1e-6
ENGS = ['pe', 'act', 'dve', 'pool', 'sp']
CH = 4096
NWS = 5


class Sched:
    def __init__(self):
        self.prog = {e: [] for e in ENGS}
        self.cnt = {e: 0 for e in ENGS}
        self.seen = {e: {} for e in ENGS}
        self.lastw = {}
        self.readers = {}
        self.dcnt = {}
        self.targets = {e: set() for e in ENGS}

    def _collect(self, r, w):
        deps = set()
        for k in r:
            if k in self.lastw:
                deps.add(self.lastw[k])
        for k in w:
            if k in self.lastw:
                deps.add(self.lastw[k])
            deps.update(self.readers.get(k, ()))
        return deps

    def _waits(self, eng, deps):
        best = {}
        for (src, v) in deps:
            if src == eng and eng == 'pe':
                continue
            if self.seen[eng].get(src, -1) >= v:
                continue
            best[src] = max(best.get(src, -1), v)
        out = []
        for src, v in best.items():
            self.seen[eng][src] = v
            out.append((src, v))
            if src in self.targets:
                self.targets[src].add(v)
        return out

    def _record(self, opid, r, w):
        for k in r:
            self.readers.setdefault(k, set()).add(opid)
        for k in w:
            self.lastw[k] = opid
            self.readers[k] = set()

    def op(self, eng, fn, r=(), w=()):
        waits = self._waits(eng, self._collect(r, w))
        i = self.cnt[eng]
        self.cnt[eng] += 1
        self.prog[eng].append((waits, fn, (eng, i)))
        self._record((eng, i), r, w)

    def dma(self, eng, out, in_, slot, r=(), w=()):
        waits = self._waits(eng, self._collect(r, w))
        n = self.dcnt.get(slot, 0) + 1
        self.dcnt[slot] = n
        src = ('dma', slot)
        self.prog[eng].append((waits, (lambda e, o=out, i=in_: e.dma_start(out=o, in_=i)), (src, n)))
        self._record((src, n), r, w)

    def barrier(self):
        latest = []
        for e in ENGS:
            if self.cnt[e] > 0:
                latest.append((e, self.cnt[e] - 1))
        for slot, n in self.dcnt.items():
            latest.append((('dma', slot), n))
        for e in ENGS:
            waits = self._waits(e, latest)
            if waits:
                self.prog[e].append((waits, None, None))
        self.lastw.clear()
        self.readers.clear()

    def emit(self, nc, st):
        sems = {}

        def getsem(key):
            if key not in sems:
                sems[key] = st.enter_context(nc.semaphore("s%d" % len(sems)))
            return sems[key]
        rank = {}
        for e in ENGS:
            tl = sorted(self.targets[e])
            rank[e] = {v: k for k, v in enumerate(tl)}

        def wait_of(src, v):
            if isinstance(src, tuple):
                return getsem(src), 16 * v
            k = rank[src][v]
            return getsem((src, k // CH)), k % CH + 1
        plan = {}
        for e in ENGS:
            lst = []
            for waits, fn, opid in self.prog[e]:
                ws = [wait_of(s, v) for (s, v) in waits]
                inc = None
                if fn is not None:
                    src, v = opid
                    if isinstance(src, tuple):
                        inc = (getsem(src), 16)
                    elif v in rank[src]:
                        k = rank[src][v]
                        inc = (getsem((src, k // CH)), 1)
                lst.append((ws, fn, inc))
            plan[e] = lst

        def run(name, e):
            for ws, fn, inc in plan[name]:
                for (s, v) in ws:
                    e.wait_ge(s, v)
                if fn is not None:
                    ins = fn(e)
                    if inc is not None:
                        ins.then_inc(inc[0], inc[1])
        block = st.enter_context(nc.Block())

        @block.tensor
        def _(e):
            run('pe', e)

        @block.scalar
        def _(e):
            run('act', e)

        @block.vector
        def _(e):
            run('dve', e)

        @block.gpsimd
        def _(e):
            run('pool', e)

        @block.sync
        def _(e):
            run('sp', e)


class Arena:
    def __init__(self, t, nelem_bf16):
        self.t = t
        self.n = nelem_bf16
        self.off = 0
        self.peak = 0

    def mark(self):
        return self.off

    def release(self, m):
        self.off = m

    def alloc(self, shape, dtype, parts=128):
        sz = 4 if dtype == F32 else 2
        n = 1
        for s in shape:
            n *= s
        nb = (n * sz + 3) // 4 * 4
        start = self.off
        self.off += nb
        self.peak = max(self.peak, self.off)
        assert self.off <= self.n * 2, ("arena overflow", self.off)
        ap = self.t[:, start // 2:(start + n * sz) // 2]
        if dtype == F32:
            ap = ap.bitcast(F32)
        if len(shape) == 2:
            ap = ap.rearrange("p (a b) -> p a b", a=shape[0])
        elif len(shape) == 3:
            ap = ap.rearrange("p (a b c) -> p a b c", a=shape[0], b=shape[1])
        if parts != 128:
            ap = ap[0:parts]
        return ap


def _t5_bucket_np(dist):
    n = np.maximum(dist, 0)
    nf = np.maximum(n, 1).astype(np.float32)
    lb = 16 + (np.log(nf / 16.0) / np.log(8.0) * 16.0).astype(np.int32)
    lb = np.minimum(lb, 31)
    return np.where(n < 16, n, lb)


def _win_chunks():
    ch = []
    for c in range(4):
        ch.append(list(range(128 * c, 128 * c + 128)))
    for g in range(2):
        ch.append(list(range(512 + 64 * g, 576 + 64 * g)) + list(range(640 + 64 * g, 704 + 64 * g)))
    for g in range(2):
        ch.append(list(range(768 + 64 * g, 832 + 64 * g)) + [-1] * 64)
        ch.append([-1] * 64 + list(range(768 + 64 * g, 832 + 64 * g)))
    for g in range(2):
        ch.append(list(range(1024 + 64 * g, 1088 + 64 * g)) + [-1] * 64)
        ch.append([-1] * 64 + list(range(1024 + 64 * g, 1088 + 64 * g)))
    ch.append(list(range(896, 1024)))
    ch.append(list(range(1152, 1280)))
    ch.append(list(range(1280, 1304)) + [-1] * 104)
    for c in range(4):
        ch.append(list(range(1304 + 128 * c, 1304 + 128 * c + 128)))
    for c in range(4):
        ch.append(list(range(1816 + 128 * c, 1816 + 128 * c + 128)))
    for c in range(4):
        ch.append(list(range(2328 + 128 * c, 2328 + 128 * c + 128)))
    return ch


def _host_consts():
    c = {}
    c['c_ident'] = np.eye(128, dtype=np.float32)
    j = np.arange(128)[:, None]
    s = np.arange(128)[None, :]
    c['c_tri'] = (j >= s).astype(np.float32)
    c['c_strict'] = np.where(s <= j, NEG, 0.0).astype(np.float32)
    c['c_wincorr'] = np.where(s >= j, NEG, 0.0).astype(np.float32)
    sel = np.zeros((32, 16, 128), np.float32)
    for cc in range(16):
        for sp in range(128):
            sel[2 * cc + sp // 64, cc, sp] = 1.0
    c['c_sele'] = sel.reshape(32, 16 * 128)
    n = np.arange(127)[:, None]
    sj = np.arange(32)[None, :]
    ovl = ((16 * n < 64 * (sj + 1)) & (16 * n + 32 > 64 * sj)).astype(np.float32)
    c['c_ovl'] = ovl
    addc = np.zeros((128, 16, 32), np.float32)
    for i in range(16):
        t = 128 * i + np.arange(128)
        cur = (t // 64)[:, None]
        jj = np.arange(32)[None, :]
        forced = (jj == 0) | (jj == cur) | (jj == cur - 1)
        a = np.where(forced, 1e6, 0.0)
        a = np.where(jj <= cur, a, -1e30)
        addc[:, i, :] = a
    c['c_addc'] = addc.reshape(128, 512)
    idx = np.zeros((128, 503), np.float32)
    msk = np.zeros((128, 503), np.float32)
    p = np.arange(128)[:, None]
    jp = np.arange(256)[None, :]
    d1 = jp - p
    idx[:, 0:256] = _t5_bucket_np(d1)
    msk[:, 0:256] = np.where(d1 < 0, NEG, 0.0)
    m = np.arange(247)[None, :] - 120
    d2 = p - 16 * m - 31
    idx[:, 256:503] = _t5_bucket_np(d2)
    msk[:, 256:503] = np.where(d2 < 0, NEG, 0.0)
    c['c_idx'] = idx
    c['c_mask'] = msk
    return c


def _prep_weights(inp):
    f = lambda a: np.ascontiguousarray(np.asarray(a, dtype=np.float32))
    w = {}
    w_in = f(inp['w_in'])[0]
    chs = _win_chunks()
    wr = np.zeros((len(chs), 128, 8, 128), np.float32)
    w3 = w_in.reshape(8, 128, 2840)
    for k, cols in enumerate(chs):
        cols = np.array(cols)
        ok = cols >= 0
        wr[k][:, :, ok] = np.transpose(w3[:, :, cols[ok]], (1, 0, 2))
    w['w_in_r'] = wr
    w_up = f(inp['w_up'])[0]
    w['w_up_r'] = np.ascontiguousarray(np.transpose(w_up.reshape(8, 128, 44, 128), (2, 1, 0, 3)))
    w['w_down_r'] = f(inp['w_down'])[0].reshape(22, 128, 1024)
    w['w_out_r'] = f(inp['w_out'])[0].reshape(8, 128, 1024)
    k1 = f(inp['cmp_k_w1'])[0].reshape(32, 64, 256).transpose(1, 0, 2)
    v1 = f(inp['cmp_v_w1'])[0].reshape(32, 64, 256).transpose(1, 0, 2)
    w['w1r'] = np.ascontiguousarray(np.concatenate([k1, v1], axis=0))
    k2 = f(inp['cmp_k_w2'])[0].reshape(2, 128, 64).transpose(1, 0, 2)
    w['w2k_r'] = np.ascontiguousarray(np.concatenate([k2, k2], axis=2))
    w['w2v_r'] = np.ascontiguousarray(f(inp['cmp_v_w2'])[0].reshape(2, 128, 64).transpose(1, 0, 2))
    w['posT'] = np.ascontiguousarray(np.concatenate([f(inp['cmp_pos_k'])[0].T, f(inp['cmp_pos_v'])[0].T], axis=0))
    w['norm1_w'] = f(inp['norm1_w']).reshape(1, 1024)
    w['norm2_w'] = f(inp['norm2_w']).reshape(1, 1024)
    w['final_w'] = f(inp['final_norm_w']).reshape(1, 1024)
    w['onorm_w'] = np.concatenate([f(inp['nsa_out_norm_w']).reshape(1, 512), f(inp['sb_out_norm_w']).reshape(1, 512)], axis=1)
    w['gate_b'] = f(inp['gate_b']).reshape(1, 24)
    w['rel_bias'] = f(inp['rel_bias']).reshape(1, 256)
    cw = f(inp['conv_w'])[0]
    cb = f(inp['conv_b'])[0]
    cp = np.stack([cw[0], cw[1], cw[2], cb], axis=1).reshape(44, 128, 4).transpose(1, 0, 2)
    w['convp'] = np.ascontiguousarray(cp)
    w.update(_host_consts())
    return w


def build_program(nseq):
    nc = bass.Bass("TRN2", target_bir_lowering=False)
    S = Sched()
    dr = {}

    def din(name, shape):
        dr[name] = nc.dram_tensor(name, list(shape), F32, kind="ExternalInput").ap()
        return dr[name]
    x_d = din('x', (nseq, T, D))
    win_d = din('w_in_r', (29, 128, 8, 128))
    wup_d = din('w_up_r', (44, 128, 8, 128))
    wdn_d = din('w_down_r', (22, 128, 1024))
    wout_d = din('w_out_r', (8, 128, 1024))
    w1_d = din('w1r', (128, 32, 256))
    w2k_d = din('w2k_r', (128, 2, 128))
    w2v_d = din('w2v_r', (128, 2, 64))
    posT_d = din('posT', (128, 32))
    n1_d = din('norm1_w', (1, 1024))
    n2_d = din('norm2_w', (1, 1024))
    nf_d = din('final_w', (1, 1024))
    on_d = din('onorm_w', (1, 1024))
    gb_d = din('gate_b', (1, 24))
    rb_d = din('rel_bias', (1, 256))
    cp_d = din('convp', (128, 44, 4))
    cid_d = din('c_ident', (128, 128))
    ctri_d = din('c_tri', (128, 128))
    cstr_d = din('c_strict', (128, 128))
    cwc_d = din('c_wincorr', (128, 128))
    csel_d = din('c_sele', (32, 2048))
    covl_d = din('c_ovl', (127, 32))
    cadd_d = din('c_addc', (128, 512))
    cidx_d = din('c_idx', (128, 503))
    cmsk_d = din('c_mask', (128, 503))
    out_d = nc.dram_tensor("out", [nseq, T, D], F32, kind="ExternalOutput").ap()

    st = ExitStack()
    NEL = 105000
    arena_t = st.enter_context(nc.sbuf_tensor("arena", [128, NEL], BF16))
    A = Arena(arena_t, NEL)
    PS = [st.enter_context(nc.psum_tensor("ps%d" % i, [128, 512], F32)) for i in range(8)]

    def psf(b):
        return PS[b][:]

    def psb(b):
        return PS[b][:].bitcast(BF16)

    def mm(out, lhsT, rhs, start, stop, r, w, skip=False):
        S.op('pe', lambda e, o=out, l=lhsT, rr=rhs, s0=start, s1=stop, sk=skip: e.matmul(o, lhsT=l, rhs=rr, start=s0, stop=s1, skip_group_check=sk), r=r, w=w)

    def tp(out, in_, idn, r, w):
        S.op('pe', lambda e, o=out, i=in_, d=idn: e.transpose(o, i, d), r=r, w=w)

    def act(out, in_, func, r, w, bias=None, scale=None, accum=None, eng='act'):
        kw = {}
        if bias is not None:
            kw['bias'] = bias
        if scale is not None:
            kw['scale'] = scale
        if accum is not None:
            kw['accum_out'] = accum
        S.op('act', lambda e, o=out, i=in_, f=func, k=kw: e.activation(out=o, in_=i, func=f, **k), r=r, w=w)

    def tt(eng, out, in0, in1, op, r, w):
        S.op(eng, lambda e, o=out, a=in0, b=in1, p=op: e.tensor_tensor(out=o, in0=a, in1=b, op=p), r=r, w=w)

    def ts(eng, out, in0, s1, s2, op0, op1, r, w):
        if s2 is None:
            S.op(eng, lambda e, o=out, a=in0, s=s1, p=op0: e.tensor_single_scalar(out=o, in_=a, scalar=s, op=p), r=r, w=w)
        else:
            S.op(eng, lambda e, o=out, a=in0, x1=s1, x2=s2, p0=op0, p1=op1: e.tensor_scalar(out=o, in0=a, scalar1=x1, scalar2=x2, op0=p0, op1=p1), r=r, w=w)

    def stt(eng, out, in0, scalar, in1, op0, op1, r, w):
        S.op(eng, lambda e, o=out, a=in0, s=scalar, b=in1, p0=op0, p1=op1: e.scalar_tensor_tensor(out=o, in0=a, scalar=s, in1=b, op0=p0, op1=p1), r=r, w=w)

    def cp(eng, out, in_, r, w):
        if eng == 'act':
            S.op('act', lambda e, o=out, i=in_: e.copy(out=o, in_=i), r=r, w=w)
        else:
            S.op(eng, lambda e, o=out, i=in_: e.tensor_copy(out=o, in_=i), r=r, w=w)


    def rsq(vec, scale, key):
        act(vec, vec, AF.Sqrt, [key], [key], bias=EPS, scale=scale)
        S.op('dve', lambda e, v=vec: e.reciprocal(out=v, in_=v), r=[key], w=[key])

    def run_pipeline(items, lags):
        n = len(items)
        L = max(lags)
        for s_ in range(n + L):
            for j_, lag in enumerate(lags):
                k = s_ - lag
                if 0 <= k < n:
                    items[k][j_]()

    def memset(eng, ap, val, w):
        S.op(eng, lambda e, a=ap, v=val: e.memset(a, v), r=(), w=w)

    ident = A.alloc([128], BF16)
    tri = A.alloc([128], BF16)
    strict = A.alloc([128], BF16)
    wincorr = A.alloc([128], BF16)
    onescol = A.alloc([2], BF16)
    sele = A.alloc([16, 128], BF16)
    addc = A.alloc([16, 32], F32)
    corr = A.alloc([8, 256], BF16)
    bcm = A.alloc([8, 247], BF16)
    vcA = A.alloc([2, 97], BF16)
    b31c = A.alloc([8], F32)
    gateb = A.alloc([24], F32)
    convp = A.alloc([44, 4], F32)
    normw1 = A.alloc([1024], F32)
    normw2 = A.alloc([1024], F32)
    normwf = A.alloc([1024], F32)
    onormw = A.alloc([1024], F32)
    w2k = A.alloc([2, 128], BF16)
    w2v = A.alloc([2, 64], BF16)
    cbias = A.alloc([4], F32)
    posT = A.alloc([32], BF16)
    ws = [A.alloc([1024], BF16) for _ in range(NWS)]
    xbuf = [A.alloc([1024], F32) for _ in range(2)]
    ubuf = [A.alloc([1024], BF16) for _ in range(2)]
    junk = A.alloc([1024], BF16)
    ss = A.alloc([16], F32)
    rstd = A.alloc([16], F32)
    halo = A.alloc([44, 2], F32)
    m_common = A.mark()

    def ld(eng, dst, src, slot, wkey):
        S.dma(eng, dst, src, slot=slot, w=[wkey])
    ld('pool', ident, cid_d, 'c0', 'ident')
    ld('pool', tri, ctri_d, 'c1', 'tri')
    ld('pool', strict, cstr_d, 'c2', 'strict')
    ld('pool', wincorr, cwc_d, 'c3', 'wincorr')
    memset('dve', sele, 0.0, ['sele'])
    ld('pool', sele[0:32].rearrange("p a b -> p (a b)"), csel_d, 'c4', 'sele')
    ld('sp', addc.rearrange("p a b -> p (a b)"), cadd_d, 'c5', 'addc')
    ld('pool', vcA[0:127, 0, 65:97], covl_d, 'c6', 'vcA')
    ld('pool', vcA[0:127, 1, 65:97], covl_d, 'c7', 'vcA')
    ld('sp', gateb, gb_d[0:1, :].partition_broadcast(128), 'c8', 'gateb')
    ld('sp', convp.rearrange("p a b -> p (a b)"), cp_d.rearrange("p a b -> p (a b)"), 'c9', 'convp')
    ld('sp', normw1, n1_d[0:1, :].partition_broadcast(128), 'c10', 'normw1')
    ld('sp', normw2, n2_d[0:1, :].partition_broadcast(128), 'c11', 'normw2')
    ld('sp', normwf, nf_d[0:1, :].partition_broadcast(128), 'c12', 'normwf')
    ld('sp', onormw, on_d[0:1, :].partition_broadcast(128), 'c13', 'onormw')
    ld('pool', w2k.rearrange("p a b -> p (a b)"), w2k_d.rearrange("p a b -> p (a b)"), 'c14', 'w2k')
    ld('pool', w2v.rearrange("p a b -> p (a b)"), w2v_d.rearrange("p a b -> p (a b)"), 'c15', 'w2v')
    ld('pool', posT, posT_d, 'c16', 'posT')
    memset('dve', onescol, 1.0, ['onescol'])
    memset('dve', vcA[:, :, 64:65], 1.0, ['vcA'])
    memset('dve', junk, 0.0, ['junk'])

    m0 = A.mark()
    RB = A.alloc([32, 8], F32)
    idxt = A.alloc([503], F32)
    mskt = A.alloc([503], F32)
    accb = A.alloc([8, 503], F32)
    eqm = A.alloc([503], F32)
    ld('sp', RB.rearrange("p a b -> p (a b)"), rb_d[0:1, :].partition_broadcast(128), 'c17', 'RB')
    ld('sp', idxt, cidx_d, 'c18', 'idxt')
    ld('sp', mskt, cmsk_d, 'c19', 'mskt')
    cp('dve', b31c, RB[:, 31, :], ['RB'], ['b31c'])
    tt('dve', RB, RB, b31c.unsqueeze(1).to_broadcast([128, 32, 8]), ALU.subtract, ['RB', 'b31c'], ['RB'])
    memset('dve', accb, 0.0, ['accb'])
    for k in range(31):
        ts('dve', eqm, idxt, float(k), None, ALU.is_equal, None, ['idxt'], ['eqm'])
        for h in range(8):
            stt('dve', accb[:, h, :], eqm, RB[:, k, h:h + 1], accb[:, h, :], ALU.mult, ALU.add, ['eqm', 'RB', 'accb'], ['accb'])
    for h in range(8):
        tt('dve', corr[:, h, :], accb[:, h, 0:256], mskt[:, 0:256], ALU.add, ['accb', 'mskt'], ['corr'])
        tt('dve', bcm[:, h, :], accb[:, h, 256:503], mskt[:, 256:503], ALU.add, ['accb', 'mskt'], ['bcm'])
    S.barrier()
    A.release(m0)

    plan = []
    for b in range(nseq):
        for k in range(29):
            plan.append(win_d[k].rearrange("p a b -> p (a b)"))
        for blk in range(2):
            for j in range(22):
                plan.append(wup_d[j].rearrange("p a b -> p (a b)"))
                plan.append(wup_d[22 + j].rearrange("p a b -> p (a b)"))
    wstate = {'issued': 0, 'next': 0}

    def w_issue():
        k = wstate['issued']
        if k < len(plan):
            S.dma('pool', ws[k % NWS], plan[k], slot='ws%d' % (k % NWS), w=[('ws', k % NWS)])
            wstate['issued'] = k + 1

    def w_get():
        k = wstate['next']
        wstate['next'] = k + 1
        assert k < wstate['issued']
        return ws[k % NWS].rearrange("p (a b) -> p a b", a=8), ('ws', k % NWS)

    for _ in range(NWS):
        w_issue()


    for b in range(nseq):
        m_seq = A.mark()
        mixed = A.alloc([16, 1024], BF16)
        m_ab = A.mark()
        uT = A.alloc([8, 2048], BF16)
        qkv_m = A.mark()

        memset('dve', ss, 0.0, ['ss'])
        for i in range(16):
            xb = xbuf[i % 2]
            ub = ubuf[i % 2]
            S.dma('sp', xb, x_d[b, 128 * i:128 * i + 128, :], slot='x%d' % (i % 2), w=[('xbuf', i % 2)])
            act(junk, xb, AF.Square, [('xbuf', i % 2)], ['junk', 'ss'], accum=ss[:, i:i + 1])
            cp('dve', rstd[:, i:i + 1], ss[:, i:i + 1], ['ss'], ['rstd'])
            rsq(rstd[:, i:i + 1], 1.0 / D, 'rstd')
            stt('dve', ub, xb, rstd[:, i:i + 1], normw1, ALU.mult, ALU.mult, [('xbuf', i % 2), 'rstd', 'normw1'], [('ub', i % 2)])
            bk = i % 2
            for dc in range(8):
                tp(psb(bk)[:, dc * 128:(dc + 1) * 128], ub[:, dc * 128:(dc + 1) * 128], ident, [('ub', i % 2), 'ident'], [('ps', bk)])
            cp('act', uT[:, :, 128 * i:128 * i + 128], psb(bk).rearrange("p (a b) -> p a b", a=8), [('ps', bk)], ['uT'])

        def proj_F(dst_fn, scale, keyw):
            wch, wk = w_get()
            for Q in range(4):
                bk = 2 + (proj_F.n % 4)
                proj_F.n += 1
                for dc in range(8):
                    mm(psf(bk), wch[:, dc, :], uT[:, dc, 512 * Q:512 * Q + 512], dc == 0, dc == 7, [wk, 'uT'], [('ps', bk)])
                dst = dst_fn(Q)
                if proj_F.n % 2 == 0:
                    act(dst, psf(bk), AF.Copy, [('ps', bk)], [keyw], scale=scale)
                else:
                    ts('dve', dst, psf(bk), scale, None, ALU.mult, None, [('ps', bk)], [keyw])
            w_issue()
        proj_F.n = 0

        def proj_T(evac_fn, ncols=128):
            wch, wk = w_get()
            for tg in range(4):
                bk = 2 + (proj_F.n % 4)
                proj_F.n += 1
                for tl in range(4):
                    i = 4 * tg + tl
                    for dc in range(8):
                        mm(psf(bk)[:, tl * 128:tl * 128 + ncols], uT[:, dc, 128 * i:128 * i + 128], wch[:, dc, 0:ncols], dc == 0, dc == 7, [wk, 'uT'], [('ps', bk)])
                evac_fn(tg, bk)
            w_issue()

        qnT = A.alloc([4, 2048], BF16)
        ksT = A.alloc([4, 2048], BF16)
        kwT = A.alloc([4, 2048], BF16)
        vsA = A.alloc([16, 130], BF16)
        vwA = A.alloc([16, 130], BF16)
        gT = A.alloc([16, 24], F32)
        kcmpT = A.alloc([2, 127], BF16)
        work_m = A.mark()
        kcvcT = A.alloc([2, 2048], BF16)
        memset('dve', vsA, 1.0, ['vsA'])
        memset('dve', vwA, 1.0, ['vwA'])
        for c in range(4):
            proj_F(lambda Q, c=c: qnT[:, c, 512 * Q:512 * Q + 512], 0.125, 'qnT')
        for g in range(2):
            proj_F(lambda Q, g=g: kcvcT[:, g, 512 * Q:512 * Q + 512], 1.0, 'kcvcT')
        for gh in range(4):
            proj_F(lambda Q, gh=gh: ksT[:, gh, 512 * Q:512 * Q + 512], 1.0, 'ksT')
        for gh in range(4):
            proj_F(lambda Q, gh=gh: kwT[:, gh, 512 * Q:512 * Q + 512], 1.0, 'kwT')

        def evac_v(dstA, key):
            def f(tg, bk):
                src = psf(bk).rearrange("p (a g d) -> p a g d", a=4, g=2)
                dst = dstA[:, 4 * tg:4 * tg + 4, :].rearrange("p a (g e) -> p a g e", g=2)[:, :, :, 0:64]
                cp('dve', dst, src, [('ps', bk)], [key])
            return f
        proj_T(evac_v(vsA, 'vsA'))
        proj_T(evac_v(vwA, 'vwA'))

        def evac_g(tg, bk):
            src = psf(bk).rearrange("p (a c) -> p a c", a=4)[:, :, 0:24]
            dst = gT[:, 4 * tg:4 * tg + 4, :]
            tt('dve', dst, src, gateb.unsqueeze(1).to_broadcast([128, 4, 24]), ALU.add, [('ps', bk), 'gateb'], ['gT'])
            act(dst, dst, AF.Sigmoid, ['gT'], ['gT'])
        proj_T(evac_g, ncols=24)

        w1sb = A.alloc([32, 256], BF16)
        S.dma('pool', w1sb.rearrange("p a b -> p (a b)"), w1_d.rearrange("p a b -> p (a b)"), slot='w1', w=['w1sb'])
        geluT = A.alloc([2, 127], BF16)
        gx = A.alloc([127], F32)
        gt_ = A.alloc([127], F32)
        for kv in range(2):
            rows = slice(64 * kv, 64 * kv + 64)
            for hcc in range(2):
                bk = 0
                for i in range(32):
                    mm(psf(bk)[:, 0:1], w1sb[rows, i, hcc * 128:hcc * 128 + 128], posT[rows, i:i + 1], i == 0, i == 31, ['w1sb', 'posT'], [('ps', bk)])
                cp('dve', cbias[:, kv * 2 + hcc:kv * 2 + hcc + 1], psf(bk)[:, 0:1], [('ps', bk)], ['cbias'])
        for g in range(2):
            for kv in range(2):
                rows = slice(64 * kv, 64 * kv + 64)
                for hcc in range(2):
                    bk = hcc
                    for i in range(32):
                        mm(psf(bk)[:, 0:127], w1sb[rows, i, hcc * 128:hcc * 128 + 128], kcvcT[rows, g, i:i + 16 * 126 + 1:16], i == 0, i == 31, ['w1sb', 'kcvcT'], [('ps', bk)])
                    ts('dve', gx, psf(bk)[:, 0:127], cbias[:, kv * 2 + hcc:kv * 2 + hcc + 1], None, ALU.add, None, [('ps', bk), 'cbias'], ['gx'])
                    tt('dve', gt_, gx, gx, ALU.mult, ['gx'], ['gt'])
                    ts('dve', gt_, gt_, 0.044715, 1.0, ALU.mult, ALU.add, ['gt'], ['gt'])
                    tt('dve', gt_, gt_, gx, ALU.mult, ['gt', 'gx'], ['gt'])
                    act(gt_, gt_, AF.Sigmoid, ['gt'], ['gt'], scale=1.5957691216057308)
                    tt('dve', geluT[:, hcc, :], gx, gt_, ALU.mult, ['gx', 'gt'], ['geluT'])
                bk = 2
                if kv == 0:
                    for hcc in range(2):
                        mm(psf(bk)[:, 0:127], w2k[:, hcc, :], geluT[:, hcc, :], hcc == 0, hcc == 1, ['w2k', 'geluT'], [('ps', bk)])
                    cp('dve', kcmpT[:, g, :], psf(bk)[:, 0:127], [('ps', bk)], ['kcmpT'])
                else:
                    for hcc in range(2):
                        mm(psf(bk)[0:127, 0:64], geluT[:, hcc, :], w2v[:, hcc, :], hcc == 0, hcc == 1, ['w2v', 'geluT'], [('ps', bk)])
                    cp('dve', vcA[0:127, g, 0:64], psf(bk)[0:127, 0:64], [('ps', bk)], ['vcA'])
        S.barrier()
        A.release(work_m)
        kcmp_keep = kcmpT
        att_m = A.mark()

        Pc = [A.alloc([127], BF16) for _ in range(2)]
        PcT = [A.alloc([128], BF16) for _ in range(2)]
        PT = [A.alloc([512], BF16) for _ in range(3)]
        onsa2 = [A.alloc([16, 64], F32) for _ in range(2)]
        tmpo = A.alloc([4, 64], F32)
        impa2 = [A.alloc([4, 32], F32) for _ in range(2)]
        score = A.alloc([4, 32], F32)
        top8 = A.alloc([8], F32)
        thr = A.alloc([1], F32)
        negm = A.alloc([4, 32], BF16)
        negmT2 = [A.alloc([512], BF16) for _ in range(2)]
        rd = A.alloc([4], F32)
        sc = A.alloc([4], F32)
        sqt = A.alloc([16, 64], F32)
        ssh = A.alloc([16], F32)
        obn = [0]
        build_program.nsa_off = A.off
        for g in range(2):
            memset('dve', negmT2[g], 0.0, [('negmT', g)])

        def finalize(ob, h, branch, Q, with_imp=False):
            g_ = h // 4
            onsa = onsa2[g_]
            impa = impa2[g_]
            O = psf(ob).rearrange("p (a c) -> p a c", a=4)
            r_ = h % 4
            ts('dve', rd, O[:, :, 64], 1e-30, None, ALU.max, None, [('ps', ob)], ['rd'])
            S.op('dve', lambda e: e.reciprocal(out=rd, in_=rd), r=['rd'], w=['rd'])
            tt('dve', sc, rd, gT[:, 4 * Q:4 * Q + 4, 3 * h + branch], ALU.mult, ['rd', 'gT'], ['sc'])
            tt('dve', tmpo, O[:, :, 0:64], sc.unsqueeze(2).to_broadcast([128, 4, 64]), ALU.mult, [('ps', ob), 'sc'], ['tmpo'])
            ov = onsa.rearrange("p (a r) d -> p a r d", a=4)[:, :, r_, :]
            tt('dve', ov, ov, tmpo, ALU.add, ['tmpo', ('onsa', g_)], [('onsa', g_)])
            if with_imp:
                tt('dve', score, O[:, :, 65:97], rd.unsqueeze(2).to_broadcast([128, 4, 32]), ALU.mult, [('ps', ob), 'rd'], ['score'])
                tt('dve', impa, impa, score, ALU.add, ['score', ('impa', g_)], [('impa', g_)])

        def headnorm_nsa(Q, g_):
            onsa = onsa2[g_]
            tt('dve', sqt, onsa, onsa, ALU.mult, [('onsa', g_)], ['sqt'])
            S.op('dve', lambda e: e.tensor_reduce(out=ssh, in_=sqt, axis=AX.X, op=ALU.add), r=['sqt'], w=['ssh'])
            rsq(ssh, 1.0 / 64, 'ssh')
            tt('dve', sqt, onsa, ssh.unsqueeze(2).to_broadcast([128, 16, 64]), ALU.mult, [('onsa', g_), 'ssh', 'sqt'], ['sqt'])
            tt('dve', mixed[:, 4 * Q:4 * Q + 4, 256 * g_:256 * g_ + 256], sqt.rearrange("p (a r) d -> p a (r d)", a=4),
               onormw[:, 256 * g_:256 * g_ + 256].unsqueeze(1).to_broadcast([128, 4, 256]), ALU.mult, ['sqt', 'onormw'], ['mixed'])

        for Q in range(4):
            for g in range(2):
                memset('dve', onsa2[g], 0.0, [('onsa', g)])
                memset('dve', impa2[g], 0.0, [('impa', g)])
            items = []
            for g in range(2):
                for r_ in range(4):
                    h = 4 * g + r_
                    ob = 6 + (obn[0] % 2)
                    obn[0] += 1
                    for tl in range(4):
                        k_ = len(items)
                        def mk(g=g, h=h, ob=ob, tl=tl, k_=k_, Q=Q):
                            hf = slice(64 * (h % 2), 64 * (h % 2) + 64)
                            i = 4 * Q + tl
                            bk = k_ % 2
                            pc = Pc[k_ % 2]
                            pct = PcT[k_ % 2]
                            tb_ = 3 + (k_ % 2)
                            O = psf(ob).rearrange("p (a c) -> p a c", a=4)

                            def s1():
                                mm(psf(bk)[:, 0:127], qnT[hf, h // 2, 128 * i:128 * i + 128], kcmp_keep[hf, g, :], True, False, ['qnT', 'kcmpT'], [('ps', bk)])
                                mm(psf(bk)[:, 0:127], ident, bcm[:, h, 120 - 8 * i:120 - 8 * i + 127], False, True, ['ident', 'bcm'], [('ps', bk)])

                            def s2():
                                act(pc, psf(bk)[:, 0:127], AF.Exp, [('ps', bk), 'b31c'], [('Pc', k_ % 2)], bias=b31c[:, h:h + 1])

                            def s3():
                                tp(psb(tb_)[0:127, 0:128], pc, ident, [('Pc', k_ % 2), 'ident'], [('ps', tb_)])

                            def s4():
                                cp('dve', pct[0:127, :], psb(tb_)[0:127, 0:128], [('ps', tb_)], [('PcT', k_ % 2)])

                            def s5():
                                mm(O[:, tl, 0:97], pct[0:127, :], vcA[0:127, g, :], True, True, [('PcT', k_ % 2), 'vcA'], [('ps', ob)])
                                if tl == 3:
                                    finalize(ob, h, 0, Q, with_imp=True)
                            return [s1, s2, s3, s4, s5]
                        items.append(mk())
            run_pipeline(items, [0, 1, 2, 3, 4])
            for g in range(2):
                tt('dve', score, impa2[g], addc[:, 4 * Q:4 * Q + 4, :], ALU.add, [('impa', g), 'addc'], ['score'])
                for tl in range(4):
                    S.op('dve', lambda e, tl=tl: e.max(out=top8, in_=score[:, tl, :]), r=['score'], w=['top8'])
                    ts('dve', thr, top8[:, 7:8], -5e29, None, ALU.max, None, ['top8'], ['thr'])
                    ts('dve', negm[:, tl, :], score[:, tl, :], thr[:, 0:1], NEG, ALU.is_lt, ALU.mult, ['score', 'thr'], ['negm'])
                    tp(psb(2)[0:32, tl * 128:tl * 128 + 128], negm[:, tl, :], ident, ['negm', 'ident'], [('ps', 2)])
                cp('dve', negmT2[g][0:32, :], psb(2)[0:32, 0:512], [('ps', 2)], [('negmT', g)])
            items = []
            for g in range(2):
                for branch in (2, 1):
                    kT = ksT if branch == 1 else kwT
                    vA = vsA if branch == 1 else vwA
                    kkey = 'ksT' if branch == 1 else 'kwT'
                    vkey = 'vsA' if branch == 1 else 'vwA'
                    for r_ in range(4):
                        h = 4 * g + r_
                        ob = 6 + (obn[0] % 2)
                        obn[0] += 1
                        c_lo = 0 if branch == 1 else max(0, 4 * Q - 4)
                        very_first = (g == 0 and branch == 2 and r_ == 0)
                        very_last = (g == 1 and branch == 1 and r_ == 3)
                        for c in range(c_lo, 4 * Q + 4):
                            k_ = len(items)
                            last_item = (branch == 1 and r_ == 3 and c == 4 * Q + 3)
                            def mk(g=g, branch=branch, kT=kT, vA=vA, kkey=kkey, vkey=vkey, h=h, ob=ob, c=c, c_lo=c_lo, k_=k_, Q=Q, last_item=last_item, very_first=very_first, very_last=very_last):
                                hf = slice(64 * (h % 2), 64 * (h % 2) + 64)
                                O = psf(ob).rearrange("p (a c) -> p a c", a=4)
                                qt_lo = max(c, 4 * Q)
                                qt_hi = 4 * Q + 3 if branch == 1 else min(c + 4, 4 * Q + 3)
                                lo = 128 * (qt_lo - 4 * Q)
                                hi = 128 * (qt_hi - 4 * Q + 1)
                                bk = 3 + (k_ % 3)
                                pt = PT[k_ % 3]
                                pk = ('PT', k_ % 3)

                                def s1():
                                    extra = []
                                    if branch == 1:
                                        extra.append(('m', lo, hi))
                                    for qt in range(qt_lo, qt_hi + 1):
                                        o_ = qt - c
                                        cl = 128 * (qt - 4 * Q)
                                        if o_ <= 1:
                                            extra.append(('c', cl, o_))
                                        if branch == 2 and o_ == 4:
                                            extra.append(('w', cl, 0))
                                    mm(psf(bk)[:, lo:hi], kT[:, 2 * g + (h % 2), 128 * c:128 * c + 128], qnT[:, h // 2, 512 * Q + lo:512 * Q + hi], True, len(extra) == 0, [kkey, 'qnT'], [('ps', bk)])
                                    for j_, ex in enumerate(extra):
                                        last = j_ == len(extra) - 1
                                        if ex[0] == 'm':
                                            mm(psf(bk)[:, lo:hi], sele[:, c, :], negmT2[g][:, lo:hi], False, last, ['sele', ('negmT', g)], [('ps', bk)])
                                        elif ex[0] == 'c':
                                            mm(psf(bk)[:, ex[1]:ex[1] + 128], ident, corr[:, h, 128 * ex[2]:128 * ex[2] + 128], False, last, ['ident', 'corr'], [('ps', bk)])
                                        else:
                                            mm(psf(bk)[:, ex[1]:ex[1] + 128], ident, wincorr, False, last, ['ident', 'wincorr'], [('ps', bk)])

                                def s2():
                                    act(pt[:, lo:hi], psf(bk)[:, lo:hi], AF.Exp, [('ps', bk), 'b31c'], [pk], bias=b31c[:, h:h + 1])

                                def s3():
                                    if c == c_lo:
                                        if very_first:
                                            memset('dve', psf(ob), 0.0, [('ps', ob)])
                                        if not very_last:
                                            ob_next = 6 + ((ob - 6 + 1) % 2)
                                            memset('dve', psf(ob_next), 0.0, [('ps', ob_next)])
                                    for qt in range(qt_lo, qt_hi + 1):
                                        tl = qt - 4 * Q
                                        mm(O[:, tl, 0:65], pt[:, 128 * tl:128 * tl + 128], vA[:, c, 65 * g:65 * g + 65], False, c == qt, [pk, vkey], [('ps', ob)], skip=True)
                                    if c == 4 * Q + 3:
                                        finalize(ob, h, branch, Q)
                                    if last_item:
                                        headnorm_nsa(Q, g)
                                return [s1, s2, s3]
                            items.append(mk())
            run_pipeline(items, [0, 1, 2])


        S.barrier()
        A.release(qkv_m)

        qsT = A.alloc([4, 2048], BF16)
        ksbT = A.alloc([4, 2048], BF16)
        vS = A.alloc([16, 512], BF16)
        for c in range(4):
            proj_F(lambda Q, c=c: qsT[:, c, 512 * Q:512 * Q + 512], 0.125, 'qsT')
        for c in range(4):
            proj_F(lambda Q, c=c: ksbT[:, c, 512 * Q:512 * Q + 512], 1.0, 'ksbT')
        for c in range(4):
            def evac_vs(tg, bk, c=c):
                src = psf(bk).rearrange("p (a d) -> p a d", a=4)
                cp('dve', vS[:, 4 * tg:4 * tg + 4, 128 * c:128 * c + 128], src, [('ps', bk)], ['vS'])
            proj_T(evac_vs)

        Eb = [A.alloc([512], BF16) for _ in range(3)]
        SPb = [A.alloc([512], BF16) for _ in range(4)]
        Xb = [A.alloc([512], BF16) for _ in range(2)]
        Pb = [A.alloc([512], BF16) for _ in range(2)]
        osb = A.alloc([32, 64], F32)
        carry = A.alloc([4], F32)
        gsc = A.alloc([4], F32)
        tmps = A.alloc([4, 64], F32)
        sq2 = A.alloc([32, 64], F32)
        ss2h = A.alloc([32], F32)
        build_program.sb_off = A.off
        items = []
        for Q in range(4):
            for h in range(8):
                for c in range(4 * Q + 3, -1, -1):
                    k_ = len(items)
                    def mk(Q=Q, h=h, c=c, k_=k_):
                        hf = slice(64 * (h % 2), 64 * (h % 2) + 64)
                        accv = osb.rearrange("p (a h) d -> p a h d", a=4)[:, :, h, :]
                        qt_lo = max(c, 4 * Q)
                        lo = 128 * (qt_lo - 4 * Q)
                        hi = 512
                        zb = k_ % 2
                        cb_ = 2 + k_ % 2
                        rb_ = 4 + k_ % 2
                        E = Eb[k_ % 3]
                        SP = SPb[k_ % 4]
                        X = Xb[k_ % 2]
                        P = Pb[k_ % 2]
                        kE, kS, kX, kP = ('E', k_ % 3), ('SP', k_ % 4), ('X', k_ % 2), ('P', k_ % 2)
                        diag = c >= 4 * Q
                        R = psf(rb_).rearrange("p (a c) -> p a c", a=4)
                        tl0 = qt_lo - 4 * Q
                        first = (c == 4 * Q + 3)

                        def s_g():
                            if not first:
                                act(gsc, carry, AF.Exp, ['carry'], ['gsc'], scale=-1.0)

                        def s1():
                            mm(psf(zb)[:, lo:hi], ksbT[hf, h // 2, 128 * c:128 * c + 128], qsT[hf, h // 2, 512 * Q + lo:512 * Q + hi], True, not diag, ['ksbT', 'qsT'], [('ps', zb)])
                            if diag:
                                mm(psf(zb)[:, lo:lo + 128], ident, strict, False, True, ['ident', 'strict'], [('ps', zb)])

                        def s2():
                            act(E[:, lo:hi], psf(zb)[:, lo:hi], AF.Exp, [('ps', zb)], [kE])

                        def s2b():
                            act(SP[:, lo:hi], E[:, lo:hi], AF.Ln, [kE], [kS], bias=1.0)

                        def s3():
                            mm(psf(cb_)[:, lo:hi], tri, SP[:, lo:hi], True, True, ['tri', kS], [('ps', cb_)])

                        def s4():
                            act(X[:, lo:hi], psf(cb_)[:, lo:hi], AF.Exp, [('ps', cb_)], [kX], scale=-1.0)

                        def s5():
                            tt('dve', P[:, lo:hi], E[:, lo:hi], X[:, lo:hi], ALU.mult, [kE, kX], [kP])

                        def s6():
                            for tl in range(tl0, 4):
                                mm(R[:, tl, 0:64], P[:, 128 * tl:128 * tl + 128], vS[:, c, 64 * h:64 * h + 64], True, True, [kP, 'vS'], [('ps', rb_)])
                                mm(R[:, tl, 64:65], SP[:, 128 * tl:128 * tl + 128], onescol[:, 0:1], True, True, [kS, 'onescol'], [('ps', rb_)])

                        def s7():
                            if first and h == 0:
                                memset('dve', osb, 0.0, ['osb'])
                            if first:
                                memset('dve', carry, 0.0, ['carry'])
                                memset('dve', gsc, 1.0, ['gsc'])
                            tt('dve', tmps[:, tl0:4, :], R[:, tl0:4, 0:64], gsc[:, tl0:4].unsqueeze(2).to_broadcast([128, 4 - tl0, 64]), ALU.mult, [('ps', rb_), 'gsc'], ['tmps'])
                            tt('dve', accv[:, tl0:4, :], accv[:, tl0:4, :], tmps[:, tl0:4, :], ALU.add, ['tmps', 'osb'], ['osb'])
                            if c > 0:
                                tt('dve', carry[:, tl0:4], carry[:, tl0:4], R[:, tl0:4, 64], ALU.add, [('ps', rb_), 'carry'], ['carry'])
                            if c == 0 and h == 7:
                                tt('dve', sq2, osb, osb, ALU.mult, ['osb'], ['sq2'])
                                S.op('dve', lambda e: e.tensor_reduce(out=ss2h, in_=sq2, axis=AX.X, op=ALU.add), r=['sq2'], w=['ss2h'])
                                rsq(ss2h, 1.0 / 64, 'ss2h')
                                tt('dve', sq2, osb, ss2h.unsqueeze(2).to_broadcast([128, 32, 64]), ALU.mult, ['osb', 'ss2h', 'sq2'], ['sq2'])
                                tt('dve', mixed[:, 4 * Q:4 * Q + 4, 512:1024], sq2.rearrange("p (a h) d -> p a (h d)", a=4),
                                   onormw[:, 512:1024].unsqueeze(1).to_broadcast([128, 4, 512]), ALU.mult, ['sq2', 'onormw'], ['mixed'])
                        return [s1, s2, s3, s4, s2b, s5, s6, s_g, s7]
                    items.append(mk())
        run_pipeline(items, [0, 1, 2, 3, 1, 3, 4, 5, 5])


        S.barrier()
        A.release(m_ab)

        u2T = A.alloc([8, 2048], BF16)
        m_c1 = A.mark()
        wout = A.alloc([8, 1024], BF16)
        mT = A.alloc([8, 128], BF16)
        hbuf = A.alloc([1024], F32)
        S.dma('pool', wout, wout_d.rearrange("c p n -> p c n"), slot='wout', w=['wout'])
        memset('dve', ss, 0.0, ['ss'])
        for i in range(16):
            xb = xbuf[i % 2]
            ub = ubuf[i % 2]
            S.dma('sp', xb, x_d[b, 128 * i:128 * i + 128, :], slot='x%d' % (i % 2), w=[('xbuf', i % 2)])
            bk = i % 2
            for dc in range(8):
                tp(psb(bk)[:, dc * 128:(dc + 1) * 128], mixed[:, i, dc * 128:(dc + 1) * 128], ident, ['mixed', 'ident'], [('ps', bk)])
            cp('act', mT, psb(bk).rearrange("p (a b) -> p a b", a=8), [('ps', bk)], ['mT'])
            for half in range(2):
                pb = 2 + half
                for c in range(8):
                    mm(psf(pb), mT[:, c, :], wout[:, c, 512 * half:512 * half + 512], c == 0, c == 7, ['mT', 'wout'], [('ps', pb)])
                tt('dve', hbuf[:, 512 * half:512 * half + 512], psf(pb), xb[:, 512 * half:512 * half + 512], ALU.add, [('ps', pb), ('xbuf', i % 2)], ['hbuf'])
            S.dma('sp', out_d[b, 128 * i:128 * i + 128, :], hbuf, slot='hst', r=['hbuf'], w=[('outh', i)])
            act(junk, hbuf, AF.Square, ['hbuf'], ['junk', 'ss'], accum=ss[:, i:i + 1])
            cp('dve', rstd[:, i:i + 1], ss[:, i:i + 1], ['ss'], ['rstd'])
            rsq(rstd[:, i:i + 1], 1.0 / D, 'rstd')
            stt('dve', ub, hbuf, rstd[:, i:i + 1], normw2, ALU.mult, ALU.mult, ['hbuf', 'rstd', 'normw2'], [('ub', i % 2)])
            bk2 = 4 + i % 2
            for dc in range(8):
                tp(psb(bk2)[:, dc * 128:(dc + 1) * 128], ub[:, dc * 128:(dc + 1) * 128], ident, [('ub', i % 2), 'ident'], [('ps', bk2)])
            cp('act', u2T[:, :, 128 * i:128 * i + 128], psb(bk2).rearrange("p (a b) -> p a b", a=8), [('ps', bk2)], ['u2T'])
        S.barrier()
        A.release(m_seq)
        actT_lo = A.alloc([16, 1024], BF16)
        assert A.off == m_ab
        A.off = m_c1
        actT_hi = A.alloc([6, 1024], BF16)
        wdn = A.alloc([22, 1024], BF16)
        accg = [A.alloc([512], F32) for _ in range(2)]
        accv_ = [A.alloc([512], F32) for _ in range(2)]
        sil = [A.alloc([512], F32) for _ in range(2)]
        obuf = A.alloc([1024], F32)
        fx = A.alloc([8], F32)

        def actT(j):
            return actT_lo[:, j, :] if j < 16 else actT_hi[:, j - 16, :]

        memset('dve', halo, 0.0, ['halo'])
        for blk in range(2):
            S.dma('pool', wdn, wdn_d.rearrange("j p n -> p j n"), slot='wdn', w=['wdn'])
            for j in range(22):
                wg, wgk = w_get()
                wv, wvk = w_get()
                for tb in range(2):
                    n_ = (j * 2 + tb) % 2
                    col0 = 1024 * blk + 512 * tb
                    for which, wch, wk, pb, accs, fc in ((0, wg, wgk, 0 + n_, accg, j), (1, wv, wvk, 2 + n_, accv_, 22 + j)):
                        for dc in range(8):
                            mm(psf(pb), wch[:, dc, :], u2T[:, dc, col0:col0 + 512], dc == 0, dc == 7, [wk, 'u2T'], [('ps', pb)])
                        ac = accs[n_]
                        ak = ('acc', which, n_)
                        G = psf(pb)
                        S.op('act', lambda e, o=ac, i=G, fc=fc: e.activation(out=o, in_=i, func=AF.Identity, bias=convp[:, fc, 3:4], scale=convp[:, fc, 2:3]),
                             r=[('ps', pb), 'convp'], w=[ak])
                        stt('dve', ac[:, 1:512], G[:, 0:511], convp[:, fc, 1:2], ac[:, 1:512], ALU.mult, ALU.add, [('ps', pb), 'convp', ak], [ak])
                        stt('dve', ac[:, 2:512], G[:, 0:510], convp[:, fc, 0:1], ac[:, 2:512], ALU.mult, ALU.add, [('ps', pb), 'convp', ak], [ak])
                        stt('dve', ac[:, 0:1], halo[:, fc, 1:2], convp[:, fc, 1:2], ac[:, 0:1], ALU.mult, ALU.add, ['halo', 'convp', ak], [ak])
                        stt('dve', ac[:, 0:2], halo[:, fc, 0:2], convp[:, fc, 0:1], ac[:, 0:2], ALU.mult, ALU.add, ['halo', 'convp', ak], [ak])
                        cp('dve', halo[:, fc, :], G[:, 510:512], [('ps', pb)], ['halo'])
                    act(sil[n_], accg[n_], AF.Silu, [('acc', 0, n_)], [('sil', n_)])
                    tt('pool', actT(j)[:, 512 * tb:512 * tb + 512], sil[n_], accv_[n_], ALU.mult, [('sil', n_), ('acc', 1, n_)], ['actT'])
                w_issue()
                w_issue()
            memset('dve', ss, 0.0, ['ss'])
            for tl in range(8):
                i = 8 * blk + tl
                xb = xbuf[i % 2]
                S.dma('sp', xb, out_d[b, 128 * i:128 * i + 128, :], slot='x%d' % (i % 2), r=[('outh', i)], w=[('xbuf', i % 2)])
                for half in range(2):
                    pb = 4 + (2 * tl + half) % 4
                    for j in range(22):
                        mm(psf(pb), actT(j)[:, 128 * tl:128 * tl + 128], wdn[:, j, 512 * half:512 * half + 512], j == 0, j == 21, ['actT', 'wdn'], [('ps', pb)])
                    tt('dve', xb[:, 512 * half:512 * half + 512], psf(pb), xb[:, 512 * half:512 * half + 512], ALU.add, [('ps', pb), ('xbuf', i % 2)], [('xbuf', i % 2)])
                act(junk, xb, AF.Square, [('xbuf', i % 2)], ['junk', 'ss'], accum=ss[:, tl:tl + 1])
                cp('dve', fx[:, tl:tl + 1], ss[:, tl:tl + 1], ['ss'], ['fx'])
                rsq(fx[:, tl:tl + 1], 1.0 / D, 'fx')
                stt('dve', obuf, xb, fx[:, tl:tl + 1], normwf, ALU.mult, ALU.mult, [('xbuf', i % 2), 'fx', 'normwf'], ['obuf'])
                S.dma('sp', out_d[b, 128 * i:128 * i + 128, :], obuf, slot='ost', r=['obuf'], w=[('outf', i)])
        S.barrier()
        A.release(m_seq)

    S.barrier()
    build_program.info = {'peak': A.peak, 'ops': dict(S.cnt)}
    S.emit(nc, st)
    st.close()
    return nc


_CACHE = {}


def kernel(**inputs):
    x = np.ascontiguousarray(np.asarray(inputs['x'], dtype=np.float32))
    B = x.shape[0]
    nseq = B // NCORE
    w = _prep_weights(inputs)
    if nseq not in _CACHE:
        _CACHE[nseq] = build_program(nseq)
    nc = _CACHE[nseq]
    in_maps = []
    for c in range(NCORE):
        m = dict(w)
        m['x'] = np.ascontiguousarray(x[c * nseq:(c + 1) * nseq])
        in_maps.append(m)
    res = run_bass_kernel_spmd(nc, in_maps, core_ids=list(range(NCORE)))
    out = np.concatenate([np.asarray(r['out'], dtype=np.float32) for r in res.results], axis=0)
    return out
```

```python
import numpy as np
from contextlib import ExitStack
import concourse.bass as bass
import concourse.mybir as mybir
from concourse.bass_utils import run_bass_kernel_spmd

F32 = mybir.dt.float32
BF16 = mybir.dt.bfloat16
AF = mybir.ActivationFunctionType
ALU = mybir.AluOpType
AX = mybir.AxisListType

T = 2048
D = 1024
DFF = 2816
NCORE = 8
NEG = -30000.0
EPS = 1e-6
ENGS = ['pe', 'act', 'dve', 'pool', 'sp']
CH = 4096
NWS = 5


class Sched:
    def __init__(self):
        self.prog = {e: [] for e in ENGS}
        self.cnt = {e: 0 for e in ENGS}
        self.seen = {e: {} for e in ENGS}
        self.lastw = {}
        self.readers = {}
        self.dcnt = {}
        self.targets = {e: set() for e in ENGS}

    def _collect(self, r, w):
        deps = set()
        for k in r:
            if k in self.lastw:
                deps.add(self.lastw[k])
        for k in w:
            if k in self.lastw:
                deps.add(self.lastw[k])
            deps.update(self.readers.get(k, ()))
        return deps

    def _waits(self, eng, deps):
        best = {}
        for (src, v) in deps:
            if src == eng and eng == 'pe':
                continue
            if self.seen[eng].get(src, -1) >= v:
                continue
            best[src] = max(best.get(src, -1), v)
        out = []
        for src, v in best.items():
            self.seen[eng][src] = v
            out.append((src, v))
            if src in self.targets:
                self.targets[src].add(v)
        return out

    def _record(self, opid, r, w):
        for k in r:
            self.readers.setdefault(k, set()).add(opid)
        for k in w:
            self.lastw[k] = opid
            self.readers[k] = set()

    def op(self, eng, fn, r=(), w=()):
        waits = self._waits(eng, self._collect(r, w))
        i = self.cnt[eng]
        self.cnt[eng] += 1
        self.prog[eng].append((waits, fn, (eng, i)))
        self._record((eng, i), r, w)

    def dma(self, eng, out, in_, slot, r=(), w=()):
        waits = self._waits(eng, self._collect(r, w))
        n = self.dcnt.get(slot, 0) + 1
        self.dcnt[slot] = n
        src = ('dma', slot)
        self.prog[eng].append((waits, (lambda e, o=out, i=in_: e.dma_start(out=o, in_=i)), (src, n)))
        self._record((src, n), r, w)

    def barrier(self):
        latest = []
        for e in ENGS:
            if self.cnt[e] > 0:
                latest.append((e, self.cnt[e] - 1))
        for slot, n in self.dcnt.items():
            latest.append((('dma', slot), n))
        for e in ENGS:
            waits = self._waits(e, latest)
            if waits:
                self.prog[e].append((waits, None, None))
        self.lastw.clear()
        self.readers.clear()

    def emit(self, nc, st):
        sems = {}

        def getsem(key):
            if key not in sems:
                sems[key] = st.enter_context(nc.semaphore("s%d" % len(sems)))
            return sems[key]
        rank = {}
        for e in ENGS:
            tl = sorted(self.targets[e])
            rank[e] = {v: k for k, v in enumerate(tl)}

        def wait_of(src, v):
            if isinstance(src, tuple):
                return getsem(src), 16 * v
            k = rank[src][v]
            return getsem((src, k // CH)), k % CH + 1
        plan = {}
        for e in ENGS:
            lst = []
            for waits, fn, opid in self.prog[e]:
                ws = [wait_of(s, v) for (s, v) in waits]
                inc = None
                if fn is not None:
                    src, v = opid
                    if isinstance(src, tuple):
                        inc = (getsem(src), 16)
                    elif v in rank[src]:
                        k = rank[src][v]
                        inc = (getsem((src, k // CH)), 1)
                lst.append((ws, fn, inc))
            plan[e] = lst

        def run(name, e):
            for ws, fn, inc in plan[name]:
                for (s, v) in ws:
                    e.wait_ge(s, v)
                if fn is not None:
                    ins = fn(e)
                    if inc is not None:
                        ins.then_inc(inc[0], inc[1])
        block = st.enter_context(nc.Block())

        @block.tensor
        def _(e):
            run('pe', e)

        @block.scalar
        def _(e):
            run('act', e)

        @block.vector
        def _(e):
            run('dve', e)

        @block.gpsimd
        def _(e):
            run('pool', e)

        @block.sync
        def _(e):
            run('sp', e)


class Arena:
    def __init__(self, t, nelem_bf16):
        self.t = t
        self.n = nelem_bf16
        self.off = 0
        self.peak = 0

    def mark(self):
        return self.off

    def release(self, m):
        self.off = m

    def alloc(self, shape, dtype, parts=128):
        sz = 4 if dtype == F32 else 2
        n = 1
        for s in shape:
            n *= s
        nb = (n * sz + 3) // 4 * 4
        start = self.off
        self.off += nb
        self.peak = max(self.peak, self.off)
        assert self.off <= self.n * 2, ("arena overflow", self.off)
        ap = self.t[:, start // 2:(start + n * sz) // 2]
        if dtype == F32:
            ap = ap.bitcast(F32)
        if len(shape) == 2:
            ap = ap.rearrange("p (a b) -> p a b", a=shape[0])
        elif len(shape) == 3:
            ap = ap.rearrange("p (a b c) -> p a b c", a=shape[0], b=shape[1])
        if parts != 128:
            ap = ap[0:parts]
        return ap


def _t5_bucket_np(dist):
    n = np.maximum(dist, 0)
    nf = np.maximum(n, 1).astype(np.float32)
    lb = 16 + (np.log(nf / 16.0) / np.log(8.0) * 16.0).astype(np.int32)
    lb = np.minimum(lb, 31)
    return np.where(n < 16, n, lb)


def _win_chunks():
    ch = []
    for c in range(4):
        ch.append(list(range(128 * c, 128 * c + 128)))
    for g in range(2):
        ch.append(list(range(512 + 64 * g, 576 + 64 * g)) + list(range(640 + 64 * g, 704 + 64 * g)))
    for g in range(2):
        ch.append(list(range(768 + 64 * g, 832 + 64 * g)) + [-1] * 64)
        ch.append([-1] * 64 + list(range(768 + 64 * g, 832 + 64 * g)))
    for g in range(2):
        ch.append(list(range(1024 + 64 * g, 1088 + 64 * g)) + [-1] * 64)
        ch.append([-1] * 64 + list(range(1024 + 64 * g, 1088 + 64 * g)))
    ch.append(list(range(896, 1024)))
    ch.append(list(range(1152, 1280)))
    ch.append(list(range(1280, 1304)) + [-1] * 104)
    for c in range(4):
        ch.append(list(range(1304 + 128 * c, 1304 + 128 * c + 128)))
    for c in range(4):
        ch.append(list(range(1816 + 128 * c, 1816 + 128 * c + 128)))
    for c in range(4):
        ch.append(list(range(2328 + 128 * c, 2328 + 128 * c + 128)))
    return ch


def _host_consts():
    c = {}
    c['c_ident'] = np.eye(128, dtype=np.float32)
    j = np.arange(128)[:, None]
    s = np.arange(128)[None, :]
    c['c_tri'] = (j >= s).astype(np.float32)
    c['c_strict'] = np.where(s <= j, NEG, 0.0).astype(np.float32)
    c['c_wincorr'] = np.where(s >= j, NEG, 0.0).astype(np.float32)
    sel = np.zeros((32, 16, 128), np.float32)
    for cc in range(16):
        for sp in range(128):
            sel[2 * cc + sp // 64, cc, sp] = 1.0
    c['c_sele'] = sel.reshape(32, 16 * 128)
    n = np.arange(127)[:, None]
    sj = np.arange(32)[None, :]
    ovl = ((16 * n < 64 * (sj + 1)) & (16 * n + 32 > 64 * sj)).astype(np.float32)
    c['c_ovl'] = ovl
    addc = np.zeros((128, 16, 32), np.float32)
    for i in range(16):
        t = 128 * i + np.arange(128)
        cur = (t // 64)[:, None]
        jj = np.arange(32)[None, :]
        forced = (jj == 0) | (jj == cur) | (jj == cur - 1)
        a = np.where(forced, 1e6, 0.0)
        a = np.where(jj <= cur, a, -1e30)
        addc[:, i, :] = a
    c['c_addc'] = addc.reshape(128, 512)
    idx = np.zeros((128, 503), np.float32)
    msk = np.zeros((128, 503), np.float32)
    p = np.arange(128)[:, None]
    jp = np.arange(256)[None, :]
    d1 = jp - p
    idx[:, 0:256] = _t5_bucket_np(d1)
    msk[:, 0:256] = np.where(d1 < 0, NEG, 0.0)
    m = np.arange(247)[None, :] - 120
    d2 = p - 16 * m - 31
    idx[:, 256:503] = _t5_bucket_np(d2)
    msk[:, 256:503] = np.where(d2 < 0, NEG, 0.0)
    c['c_idx'] = idx
    c['c_mask'] = msk
    return c


def _prep_weights(inp):
    f = lambda a: np.ascontiguousarray(np.asarray(a, dtype=np.float32))
    w = {}
    w_in = f(inp['w_in'])[0]
    chs = _win_chunks()
    wr = np.zeros((len(chs), 128, 8, 128), np.float32)
    w3 = w_in.reshape(8, 128, 2840)
    for k, cols in enumerate(chs):
        cols = np.array(cols)
        ok = cols >= 0
        wr[k][:, :, ok] = np.transpose(w3[:, :, cols[ok]], (1, 0, 2))
    w['w_in_r'] = wr
    w_up = f(inp['w_up'])[0]
    w['w_up_r'] = np.ascontiguousarray(np.transpose(w_up.reshape(8, 128, 44, 128), (2, 1, 0, 3)))
    w['w_down_r'] = f(inp['w_down'])[0].reshape(22, 128, 1024)
    w['w_out_r'] = f(inp['w_out'])[0].reshape(8, 128, 1024)
    k1 = f(inp['cmp_k_w1'])[0].reshape(32, 64, 256).transpose(1, 0, 2)
    v1 = f(inp['cmp_v_w1'])[0].reshape(32, 64, 256).transpose(1, 0, 2)
    w['w1r'] = np.ascontiguousarray(np.concatenate([k1, v1], axis=0))
    k2 = f(inp['cmp_k_w2'])[0].reshape(2, 128, 64).transpose(1, 0, 2)
    w['w2k_r'] = np.ascontiguousarray(np.concatenate([k2, k2], axis=2))
    w['w2v_r'] = np.ascontiguousarray(f(inp['cmp_v_w2'])[0].reshape(2, 128, 64).transpose(1, 0, 2))
    w['posT'] = np.ascontiguousarray(np.concatenate([f(inp['cmp_pos_k'])[0].T, f(inp['cmp_pos_v'])[0].T], axis=0))
    w['norm1_w'] = f(inp['norm1_w']).reshape(1, 1024)
    w['norm2_w'] = f(inp['norm2_w']).reshape(1, 1024)
    w['final_w'] = f(inp['final_norm_w']).reshape(1, 1024)
    w['onorm_w'] = np.concatenate([f(inp['nsa_out_norm_w']).reshape(1, 512), f(inp['sb_out_norm_w']).reshape(1, 512)], axis=1)
    w['gate_b'] = f(inp['gate_b']).reshape(1, 24)
    w['rel_bias'] = f(inp['rel_bias']).reshape(1, 256)
    cw = f(inp['conv_w'])[0]
    cb = f(inp['conv_b'])[0]
    cp = np.stack([cw[0], cw[1], cw[2], cb], axis=1).reshape(44, 128, 4).transpose(1, 0, 2)
    w['convp'] = np.ascontiguousarray(cp)
    w.update(_host_consts())
    return w


def build_program(nseq):
    nc = bass.Bass("TRN2", target_bir_lowering=False)
    S = Sched()
    dr = {}

    def din(name, shape):
        dr[name] = nc.dram_tensor(name, list(shape), F32, kind="ExternalInput").ap()
        return dr[name]
    x_d = din('x', (nseq, T, D))
    win_d = din('w_in_r', (29, 128, 8, 128))
    wup_d = din('w_up_r', (44, 128, 8, 128))
    wdn_d = din('w_down_r', (22, 128, 1024))
    wout_d = din('w_out_r', (8, 128, 1024))
    w1_d = din('w1r', (128, 32, 256))
    w2k_d = din('w2k_r', (128, 2, 128))
    w2v_d = din('w2v_r', (128, 2, 64))
    posT_d = din('posT', (128, 32))
    n1_d = din('norm1_w', (1, 1024))
    n2_d = din('norm2_w', (1, 1024))
    nf_d = din('final_w', (1, 1024))
    on_d = din('onorm_w', (1, 1024))
    gb_d = din('gate_b', (1, 24))
    rb_d = din('rel_bias', (1, 256))
    cp_d = din('convp', (128, 44, 4))
    cid_d = din('c_ident', (128, 128))
    ctri_d = din('c_tri', (128, 128))
    cstr_d = din('c_strict', (128, 128))
    cwc_d = din('c_wincorr', (128, 128))
    csel_d = din('c_sele', (32, 2048))
    covl_d = din('c_ovl', (127, 32))
    cadd_d = din('c_addc', (128, 512))
    cidx_d = din('c_idx', (128, 503))
    cmsk_d = din('c_mask', (128, 503))
    out_d = nc.dram_tensor("out", [nseq, T, D], F32, kind="ExternalOutput").ap()

    st = ExitStack()
    NEL = 105000
    arena_t = st.enter_context(nc.sbuf_tensor("arena", [128, NEL], BF16))
    A = Arena(arena_t, NEL)
    PS = [st.enter_context(nc.psum_tensor("ps%d" % i, [128, 512], F32)) for i in range(8)]

    def psf(b):
        return PS[b][:]

    def psb(b):
        return PS[b][:].bitcast(BF16)

    def mm(out, lhsT, rhs, start, stop, r, w, skip=False):
        S.op('pe', lambda e, o=out, l=lhsT, rr=rhs, s0=start, s1=stop, sk=skip: e.matmul(o, lhsT=l, rhs=rr, start=s0, stop=s1, skip_group_check=sk), r=r, w=w)

    def tp(out, in_, idn, r, w):
        S.op('pe', lambda e, o=out, i=in_, d=idn: e.transpose(o, i, d), r=r, w=w)

    def act(out, in_, func, r, w, bias=None, scale=None, accum=None, eng='act'):
        kw = {}
        if bias is not None:
            kw['bias'] = bias
        if scale is not None:
            kw['scale'] = scale
        if accum is not None:
            kw['accum_out'] = accum
        S.op('act', lambda e, o=out, i=in_, f=func, k=kw: e.activation(out=o, in_=i, func=f, **k), r=r, w=w)

    def tt(eng, out, in0, in1, op, r, w):
        S.op(eng, lambda e, o=out, a=in0, b=in1, p=op: e.tensor_tensor(out=o, in0=a, in1=b, op=p), r=r, w=w)

    def ts(eng, out, in0, s1, s2, op0, op1, r, w):
        if s2 is None:
            S.op(eng, lambda e, o=out, a=in0, s=s1, p=op0: e.tensor_single_scalar(out=o, in_=a, scalar=s, op=p), r=r, w=w)
        else:
            S.op(eng, lambda e, o=out, a=in0, x1=s1, x2=s2, p0=op0, p1=op1: e.tensor_scalar(out=o, in0=a, scalar1=x1, scalar2=x2, op0=p0, op1=p1), r=r, w=w)

    def stt(eng, out, in0, scalar, in1, op0, op1, r, w):
        S.op(eng, lambda e, o=out, a=in0, s=scalar, b=in1, p0=op0, p1=op1: e.scalar_tensor_tensor(out=o, in0=a, scalar=s, in1=b, op0=p0, op1=p1), r=r, w=w)

    def cp(eng, out, in_, r, w):
        if eng == 'act':
            S.op('act', lambda e, o=out, i=in_: e.copy(out=o, in_=i), r=r, w=w)
        else:
            S.op(eng, lambda e, o=out, i=in_: e.tensor_copy(out=o, in_=i), r=r, w=w)


    def rsq(vec, scale, key):
        act(vec, vec, AF.Sqrt, [key], [key], bias=EPS, scale=scale)
        S.op('dve', lambda e, v=vec: e.reciprocal(out=v, in_=v), r=[key], w=[key])

    def run_pipeline(items, lags):
        n = len(items)
        L = max(lags)
        for s_ in range(n + L):
            for j_, lag in enumerate(lags):
                k = s_ - lag
                if 0 <= k < n:
                    items[k][j_]()

    def memset(eng, ap, val, w):
        S.op(eng, lambda e, a=ap, v=val: e.memset(a, v), r=(), w=w)

    ident = A.alloc([128], BF16)
    tri = A.alloc([128], BF16)
    strict = A.alloc([128], BF16)
    wincorr = A.alloc([128], BF16)
    onescol = A.alloc([2], BF16)
    sele = A.alloc([16, 128], BF16)
    addc = A.alloc([16, 32], F32)
    corr = A.alloc([8, 256], BF16)
    bcm = A.alloc([8, 247], BF16)
    vcA = A.alloc([2, 97], BF16)
    b31c = A.alloc([8], F32)
    gateb = A.alloc([24], F32)
    convp = A.alloc([44, 4], F32)
    normw1 = A.alloc([1024], F32)
    normw2 = A.alloc([1024], F32)
    normwf = A.alloc([1024], F32)
    onormw = A.alloc([1024], F32)
    w2k = A.alloc([2, 128], BF16)
    w2v = A.alloc([2, 64], BF16)
    cbias = A.alloc([4], F32)
    posT = A.alloc([32], BF16)
    ws = [A.alloc([1024], BF16) for _ in range(NWS)]
    xbuf = [A.alloc([1024], F32) for _ in range(2)]
    ubuf = [A.alloc([1024], BF16) for _ in range(2)]
    junk = A.alloc([1024], BF16)
    ss = A.alloc([16], F32)
    rstd = A.alloc([16], F32)
    halo = A.alloc([44, 2], F32)
    m_common = A.mark()

    def ld(eng, dst, src, slot, wkey):
        S.dma(eng, dst, src, slot=slot, w=[wkey])
    ld('pool', ident, cid_d, 'c0', 'ident')
    ld('pool', tri, ctri_d, 'c1', 'tri')
    ld('pool', strict, cstr_d, 'c2', 'strict')
    ld('pool', wincorr, cwc_d, 'c3', 'wincorr')
    memset('dve', sele, 0.0, ['sele'])
    ld('pool', sele[0:32].rearrange("p a b -> p (a b)"), csel_d, 'c4', 'sele')
    ld('sp', addc.rearrange("p a b -> p (a b)"), cadd_d, 'c5', 'addc')
    ld('pool', vcA[0:127, 0, 65:97], covl_d, 'c6', 'vcA')
    ld('pool', vcA[0:127, 1, 65:97], covl_d, 'c7', 'vcA')
    ld('sp', gateb, gb_d[0:1, :].partition_broadcast(128), 'c8', 'gateb')
    ld('sp', convp.rearrange("p a b -> p (a b)"), cp_d.rearrange("p a b -> p (a b)"), 'c9', 'convp')
    ld('sp', normw1, n1_d[0:1, :].partition_broadcast(128), 'c10', 'normw1')
    ld('sp', normw2, n2_d[0:1, :].partition_broadcast(128), 'c11', 'normw2')
    ld('sp', normwf, nf_d[0:1, :].partition_broadcast(128), 'c12', 'normwf')
    ld('sp', onormw, on_d[0:1, :].partition_broadcast(128), 'c13', 'onormw')
    ld('pool', w2k.rearrange("p a b -> p (a b)"), w2k_d.rearrange("p a b -> p (a b)"), 'c14', 'w2k')
    ld('pool', w2v.rearrange("p a b -> p (a b)"), w2v_d.rearrange("p a b -> p (a b)"), 'c15', 'w2v')
    ld('pool', posT, posT_d, 'c16', 'posT')
    memset('dve', onescol, 1.0, ['onescol'])
    memset('dve', vcA[:, :, 64:65], 1.0, ['vcA'])
    memset('dve', junk, 0.0, ['junk'])

    m0 = A.mark()
    RB = A.alloc([32, 8], F32)
    idxt = A.alloc([503], F32)
    mskt = A.alloc([503], F32)
    accb = A.alloc([8, 503], F32)
    eqm = A.alloc([503], F32)
    ld('sp', RB.rearrange("p a b -> p (a b)"), rb_d[0:1, :].partition_broadcast(128), 'c17', 'RB')
    ld('sp', idxt, cidx_d, 'c18', 'idxt')
    ld('sp', mskt, cmsk_d, 'c19', 'mskt')
    cp('dve', b31c, RB[:, 31, :], ['RB'], ['b31c'])
    tt('dve', RB, RB, b31c.unsqueeze(1).to_broadcast([128, 32, 8]), ALU.subtract, ['RB', 'b31c'], ['RB'])
    memset('dve', accb, 0.0, ['accb'])
    for k in range(31):
        ts('dve', eqm, idxt, float(k), None, ALU.is_equal, None, ['idxt'], ['eqm'])
        for h in range(8):
            stt('dve', accb[:, h, :], eqm, RB[:, k, h:h + 1], accb[:, h, :], ALU.mult, ALU.add, ['eqm', 'RB', 'accb'], ['accb'])
    for h in range(8):
        tt('dve', corr[:, h, :], accb[:, h, 0:256], mskt[:, 0:256], ALU.add, ['accb', 'mskt'], ['corr'])
        tt('dve', bcm[:, h, :], accb[:, h, 256:503], mskt[:, 256:503], ALU.add, ['accb', 'mskt'], ['bcm'])
    S.barrier()
    A.release(m0)

    plan = []
    for b in range(nseq):
        for k in range(29):
            plan.append(win_d[k].rearrange("p a b -> p (a b)"))
        for blk in range(2):
            for j in range(22):
                plan.append(wup_d[j].rearrange("p a b -> p (a b)"))
                plan.append(wup_d[22 + j].rearrange("p a b -> p (a b)"))
    wstate = {'issued': 0, 'next': 0}

    def w_issue():
        k = wstate['issued']
        if k < len(plan):
            S.dma('pool', ws[k % NWS], plan[k], slot='ws%d' % (k % NWS), w=[('ws', k % NWS)])
            wstate['issued'] = k + 1

    def w_get():
        k = wstate['next']
        wstate['next'] = k + 1
        assert k < wstate['issued']
        return ws[k % NWS].rearrange("p (a b) -> p a b", a=8), ('ws', k % NWS)

    for _ in range(NWS):
        w_issue()


    for b in range(nseq):
        m_seq = A.mark()
        mixed = A.alloc([16, 1024], BF16)
        m_ab = A.mark()
        uT = A.alloc([8, 2048], BF16)
        qkv_m = A.mark()

        memset('dve', ss, 0.0, [('ss', i_) for i_ in range(16)])
        items = []
        for i in range(16):
            def mk(i=i):
                xb = xbuf[i % 2]
                ub = ubuf[i % 2]
                bk = i % 2

                def sa():
                    S.dma('sp', xb, x_d[b, 128 * i:128 * i + 128, :], slot='x%d' % (i % 2), w=[('xbuf', i % 2)])
                    act(junk, xb, AF.Square, [('xbuf', i % 2)], [('ss', i)], accum=ss[:, i:i + 1])
                    cp('dve', rstd[:, i:i + 1], ss[:, i:i + 1], [('ss', i)], [('rstd', i)])
                    rsq(rstd[:, i:i + 1], 1.0 / D, ('rstd', i))
                    stt('dve', ub, xb, rstd[:, i:i + 1], normw1, ALU.mult, ALU.mult, [('xbuf', i % 2), ('rstd', i), 'normw1'], [('ub', i % 2)])

                def sb_():
                    for dc in range(8):
                        tp(psb(bk)[:, dc * 128:(dc + 1) * 128], ub[:, dc * 128:(dc + 1) * 128], ident, [('ub', i % 2), 'ident'], [('ps', bk)])

                def sc_():
                    cp('act', uT[:, :, 128 * i:128 * i + 128], psb(bk).rearrange("p (a b) -> p a b", a=8), [('ps', bk)], ['uT'])
                return [sa, sb_, sc_]
            items.append(mk())
        run_pipeline(items, [0, 1, 2])

        def proj_F(dst_fn, scale, keyw):
            wch, wk = w_get()
            for Q in range(4):
                bk = 2 + (proj_F.n % 4)
                proj_F.n += 1
                for dc in range(8):
                    mm(psf(bk), wch[:, dc, :], uT[:, dc, 512 * Q:512 * Q + 512], dc == 0, dc == 7, [wk, 'uT'], [('ps', bk)])
                dst = dst_fn(Q)
                if proj_F.n % 2 == 0:
                    act(dst, psf(bk), AF.Copy, [('ps', bk)], [keyw], scale=scale)
                else:
                    ts('dve', dst, psf(bk), scale, None, ALU.mult, None, [('ps', bk)], [keyw])
            w_issue()
        proj_F.n = 0

        def proj_T(evac_fn, ncols=128):
            wch, wk = w_get()
            for tg in range(4):
                bk = 2 + (proj_F.n % 4)
                proj_F.n += 1
                for tl in range(4):
                    i = 4 * tg + tl
                    for dc in range(8):
                        mm(psf(bk)[:, tl * 128:tl * 128 + ncols], uT[:, dc, 128 * i:128 * i + 128], wch[:, dc, 0:ncols], dc == 0, dc == 7, [wk, 'uT'], [('ps', bk)])
                evac_fn(tg, bk)
            w_issue()

        qnT = A.alloc([4, 2048], BF16)
        ksT = A.alloc([4, 2048], BF16)
        kwT = A.alloc([4, 2048], BF16)
        vsA = A.alloc([16, 130], BF16)
        vwA = A.alloc([16, 130], BF16)
        gT = A.alloc([16, 24], F32)
        kcmpT = A.alloc([2, 127], BF16)
        work_m = A.mark()
        kcvcT = A.alloc([2, 2048], BF16)
        memset('dve', vsA, 1.0, ['vsA'])
        memset('dve', vwA, 1.0, ['vwA'])
        for c in range(4):
            proj_F(lambda Q, c=c: qnT[:, c, 512 * Q:512 * Q + 512], 0.125, 'qnT')
        for g in range(2):
            proj_F(lambda Q, g=g: kcvcT[:, g, 512 * Q:512 * Q + 512], 1.0, 'kcvcT')
        for gh in range(4):
            proj_F(lambda Q, gh=gh: ksT[:, gh, 512 * Q:512 * Q + 512], 1.0, 'ksT')
        for gh in range(4):
            proj_F(lambda Q, gh=gh: kwT[:, gh, 512 * Q:512 * Q + 512], 1.0, 'kwT')

        def evac_v(dstA, key):
            def f(tg, bk):
                src = psf(bk).rearrange("p (a g d) -> p a g d", a=4, g=2)
                dst = dstA[:, 4 * tg:4 * tg + 4, :].rearrange("p a (g e) -> p a g e", g=2)[:, :, :, 0:64]
                cp('dve', dst, src, [('ps', bk)], [key])
            return f
        proj_T(evac_v(vsA, 'vsA'))
        proj_T(evac_v(vwA, 'vwA'))

        def evac_g(tg, bk):
            src = psf(bk).rearrange("p (a c) -> p a c", a=4)[:, :, 0:24]
            dst = gT[:, 4 * tg:4 * tg + 4, :]
            tt('dve', dst, src, gateb.unsqueeze(1).to_broadcast([128, 4, 24]), ALU.add, [('ps', bk), 'gateb'], ['gT'])
            act(dst, dst, AF.Sigmoid, ['gT'], ['gT'])
        proj_T(evac_g, ncols=24)

        w1sb = A.alloc([32, 256], BF16)
        S.dma('pool', w1sb.rearrange("p a b -> p (a b)"), w1_d.rearrange("p a b -> p (a b)"), slot='w1', w=['w1sb'])
        geluT = A.alloc([2, 127], BF16)
        gx = A.alloc([127], F32)
        gt_ = A.alloc([127], F32)
        for kv in range(2):
            rows = slice(64 * kv, 64 * kv + 64)
            for hcc in range(2):
                bk = 0
                for i in range(32):
                    mm(psf(bk)[:, 0:1], w1sb[rows, i, hcc * 128:hcc * 128 + 128], posT[rows, i:i + 1], i == 0, i == 31, ['w1sb', 'posT'], [('ps', bk)])
                cp('dve', cbias[:, kv * 2 + hcc:kv * 2 + hcc + 1], psf(bk)[:, 0:1], [('ps', bk)], ['cbias'])
        for g in range(2):
            for kv in range(2):
                rows = slice(64 * kv, 64 * kv + 64)
                for hcc in range(2):
                    bk = hcc
                    for i in range(32):
                        mm(psf(bk)[:, 0:127], w1sb[rows, i, hcc * 128:hcc * 128 + 128], kcvcT[rows, g, i:i + 16 * 126 + 1:16], i == 0, i == 31, ['w1sb', 'kcvcT'], [('ps', bk)])
                    ts('dve', gx, psf(bk)[:, 0:127], cbias[:, kv * 2 + hcc:kv * 2 + hcc + 1], None, ALU.add, None, [('ps', bk), 'cbias'], ['gx'])
                    tt('dve', gt_, gx, gx, ALU.mult, ['gx'], ['gt'])
                    ts('dve', gt_, gt_, 0.044715, 1.0, ALU.mult, ALU.add, ['gt'], ['gt'])
                    tt('dve', gt_, gt_, gx, ALU.mult, ['gt', 'gx'], ['gt'])
                    act(gt_, gt_, AF.Sigmoid, ['gt'], ['gt'], scale=1.5957691216057308)
                    tt('dve', geluT[:, hcc, :], gx, gt_, ALU.mult, ['gx', 'gt'], ['geluT'])
                bk = 2
                if kv == 0:
                    for hcc in range(2):
                        mm(psf(bk)[:, 0:127], w2k[:, hcc, :], geluT[:, hcc, :], hcc == 0, hcc == 1, ['w2k', 'geluT'], [('ps', bk)])
                    cp('dve', kcmpT[:, g, :], psf(bk)[:, 0:127], [('ps', bk)], ['kcmpT'])
                else:
                    for hcc in range(2):
                        mm(psf(bk)[0:127, 0:64], geluT[:, hcc, :], w2v[:, hcc, :], hcc == 0, hcc == 1, ['w2v', 'geluT'], [('ps', bk)])
                    cp('dve', vcA[0:127, g, 0:64], psf(bk)[0:127, 0:64], [('ps', bk)], ['vcA'])
        S.barrier()
        A.release(work_m)
        kcmp_keep = kcmpT
        att_m = A.mark()

        Pc = [A.alloc([127], BF16) for _ in range(2)]
        PcT = [A.alloc([128], BF16) for _ in range(2)]
        PT = [A.alloc([512], BF16) for _ in range(3)]
        onsa2 = [A.alloc([16, 64], F32) for _ in range(2)]
        tmpo = A.alloc([4, 64], F32)
        impa2 = [A.alloc([4, 32], F32) for _ in range(2)]
        score = A.alloc([4, 32], F32)
        top8 = A.alloc([8], F32)
        thr = A.alloc([1], F32)
        negm = A.alloc([4, 32], BF16)
        negmT2 = [A.alloc([512], BF16) for _ in range(2)]
        rd = A.alloc([4], F32)
        sc = A.alloc([4], F32)
        sqt = A.alloc([16, 64], F32)
        ssh = A.alloc([16], F32)
        obn = [0]
        build_program.nsa_off = A.off
        for g in range(2):
            memset('dve', negmT2[g], 0.0, [('negmT', g)])

        def finalize(ob, h, branch, Q, with_imp=False):
            g_ = h // 4
            onsa = onsa2[g_]
            impa = impa2[g_]
            O = psf(ob).rearrange("p (a c) -> p a c", a=4)
            r_ = h % 4
            ts('dve', rd, O[:, :, 64], 1e-30, None, ALU.max, None, [('ps', ob)], ['rd'])
            S.op('dve', lambda e: e.reciprocal(out=rd, in_=rd), r=['rd'], w=['rd'])
            tt('dve', sc, rd, gT[:, 4 * Q:4 * Q + 4, 3 * h + branch], ALU.mult, ['rd', 'gT'], ['sc'])
            tt('dve', tmpo, O[:, :, 0:64], sc.unsqueeze(2).to_broadcast([128, 4, 64]), ALU.mult, [('ps', ob), 'sc'], ['tmpo'])
            ov = onsa.rearrange("p (a r) d -> p a r d", a=4)[:, :, r_, :]
            tt('dve', ov, ov, tmpo, ALU.add, ['tmpo', ('onsa', g_)], [('onsa', g_)])
            if with_imp:
                tt('dve', score, O[:, :, 65:97], rd.unsqueeze(2).to_broadcast([128, 4, 32]), ALU.mult, [('ps', ob), 'rd'], ['score'])
                tt('dve', impa, impa, score, ALU.add, ['score', ('impa', g_)], [('impa', g_)])

        def headnorm_nsa(Q, g_):
            onsa = onsa2[g_]
            tt('dve', sqt, onsa, onsa, ALU.mult, [('onsa', g_)], ['sqt'])
            S.op('dve', lambda e: e.tensor_reduce(out=ssh, in_=sqt, axis=AX.X, op=ALU.add), r=['sqt'], w=['ssh'])
            rsq(ssh, 1.0 / 64, 'ssh')
            tt('dve', sqt, onsa, ssh.unsqueeze(2).to_broadcast([128, 16, 64]), ALU.mult, [('onsa', g_), 'ssh', 'sqt'], ['sqt'])
            tt('dve', mixed[:, 4 * Q:4 * Q + 4, 256 * g_:256 * g_ + 256], sqt.rearrange("p (a r) d -> p a (r d)", a=4),
               onormw[:, 256 * g_:256 * g_ + 256].unsqueeze(1).to_broadcast([128, 4, 256]), ALU.mult, ['sqt', 'onormw'], ['mixed'])

        for Q in range(4):
            for g in range(2):
                memset('dve', onsa2[g], 0.0, [('onsa', g)])
                memset('dve', impa2[g], 0.0, [('impa', g)])
            items = []
            for g in range(2):
                for r_ in range(4):
                    h = 4 * g + r_
                    ob = 6 + (obn[0] % 2)
                    obn[0] += 1
                    for tl in range(4):
                        k_ = len(items)
                        def mk(g=g, h=h, ob=ob, tl=tl, k_=k_, Q=Q):
                            hf = slice(64 * (h % 2), 64 * (h % 2) + 64)
                            i = 4 * Q + tl
                            bk = k_ % 2
                            pc = Pc[k_ % 2]
                            pct = PcT[k_ % 2]
                            tb_ = 3 + (k_ % 2)
                            O = psf(ob).rearrange("p (a c) -> p a c", a=4)

                            def s1():
                                mm(psf(bk)[:, 0:127], qnT[hf, h // 2, 128 * i:128 * i + 128], kcmp_keep[hf, g, :], True, False, ['qnT', 'kcmpT'], [('ps', bk)])
                                mm(psf(bk)[:, 0:127], ident, bcm[:, h, 120 - 8 * i:120 - 8 * i + 127], False, True, ['ident', 'bcm'], [('ps', bk)])

                            def s2():
                                act(pc, psf(bk)[:, 0:127], AF.Exp, [('ps', bk), 'b31c'], [('Pc', k_ % 2)], bias=b31c[:, h:h + 1])

                            def s3():
                                tp(psb(tb_)[0:127, 0:128], pc, ident, [('Pc', k_ % 2), 'ident'], [('ps', tb_)])

                            def s4():
                                cp('dve', pct[0:127, :], psb(tb_)[0:127, 0:128], [('ps', tb_)], [('PcT', k_ % 2)])

                            def s5():
                                mm(O[:, tl, 0:97], pct[0:127, :], vcA[0:127, g, :], True, True, [('PcT', k_ % 2), 'vcA'], [('ps', ob)])
                                if tl == 3:
                                    finalize(ob, h, 0, Q, with_imp=True)
                            return [s1, s2, s3, s4, s5]
                        items.append(mk())
            run_pipeline(items, [0, 1, 2, 3, 4])
            for g in range(2):
                tt('dve', score, impa2[g], addc[:, 4 * Q:4 * Q + 4, :], ALU.add, [('impa', g), 'addc'], ['score'])
                for tl in range(4):
                    S.op('dve', lambda e, tl=tl: e.max(out=top8, in_=score[:, tl, :]), r=['score'], w=['top8'])
                    ts('dve', thr, top8[:, 7:8], -5e29, None, ALU.max, None, ['top8'], ['thr'])
                    ts('dve', negm[:, tl, :], score[:, tl, :], thr[:, 0:1], NEG, ALU.is_lt, ALU.mult, ['score', 'thr'], ['negm'])
                    tp(psb(2)[0:32, tl * 128:tl * 128 + 128], negm[:, tl, :], ident, ['negm', 'ident'], [('ps', 2)])
                cp('dve', negmT2[g][0:32, :], psb(2)[0:32, 0:512], [('ps', 2)], [('negmT', g)])
            items = []
            for g in range(2):
                for branch in (2, 1):
                    kT = ksT if branch == 1 else kwT
                    vA = vsA if branch == 1 else vwA
                    kkey = 'ksT' if branch == 1 else 'kwT'
                    vkey = 'vsA' if branch == 1 else 'vwA'
                    for r_ in range(4):
                        h = 4 * g + r_
                        ob = 6 + (obn[0] % 2)
                        obn[0] += 1
                        c_lo = 0 if branch == 1 else max(0, 4 * Q - 4)
                        very_first = (g == 0 and branch == 2 and r_ == 0)
                        very_last = (g == 1 and branch == 1 and r_ == 3)
                        for c in range(c_lo, 4 * Q + 4):
                            k_ = len(items)
                            last_item = (branch == 1 and r_ == 3 and c == 4 * Q + 3)
                            def mk(g=g, branch=branch, kT=kT, vA=vA, kkey=kkey, vkey=vkey, h=h, ob=ob, c=c, c_lo=c_lo, k_=k_, Q=Q, last_item=last_item, very_first=very_first, very_last=very_last):
                                hf = slice(64 * (h % 2), 64 * (h % 2) + 64)
                                O = psf(ob).rearrange("p (a c) -> p a c", a=4)
                                qt_lo = max(c, 4 * Q)
                                qt_hi = 4 * Q + 3 if branch == 1 else min(c + 4, 4 * Q + 3)
                                lo = 128 * (qt_lo - 4 * Q)
                                hi = 128 * (qt_hi - 4 * Q + 1)
                                bk = 3 + (k_ % 3)
                                pt = PT[k_ % 3]
                                pk = ('PT', k_ % 3)

                                def s1():
                                    extra = []
                                    if branch == 1:
                                        extra.append(('m', lo, hi))
                                    for qt in range(qt_lo, qt_hi + 1):
                                        o_ = qt - c
                                        cl = 128 * (qt - 4 * Q)
                                        if o_ <= 1:
                                            extra.append(('c', cl, o_))
                                        if branch == 2 and o_ == 4:
                                            extra.append(('w', cl, 0))
                                    mm(psf(bk)[:, lo:hi], kT[:, 2 * g + (h % 2), 128 * c:128 * c + 128], qnT[:, h // 2, 512 * Q + lo:512 * Q + hi], True, len(extra) == 0, [kkey, 'qnT'], [('ps', bk)])
                                    for j_, ex in enumerate(extra):
                                        last = j_ == len(extra) - 1
                                        if ex[0] == 'm':
                                            mm(psf(bk)[:, lo:hi], sele[:, c, :], negmT2[g][:, lo:hi], False, last, ['sele', ('negmT', g)], [('ps', bk)])
                                        elif ex[0] == 'c':
                                            mm(psf(bk)[:, ex[1]:ex[1] + 128], ident, corr[:, h, 128 * ex[2]:128 * ex[2] + 128], False, last, ['ident', 'corr'], [('ps', bk)])
                                        else:
                                            mm(psf(bk)[:, ex[1]:ex[1] + 128], ident, wincorr, False, last, ['ident', 'wincorr'], [('ps', bk)])

                                def s2():
                                    act(pt[:, lo:hi], psf(bk)[:, lo:hi], AF.Exp, [('ps', bk), 'b31c'], [pk], bias=b31c[:, h:h + 1])

                                def s3():
                                    if c == c_lo:
                                        if very_first:
                                            memset('dve', psf(ob), 0.0, [('ps', ob)])
                                        if not very_last:
                                            ob_next = 6 + ((ob - 6 + 1) % 2)
                                            memset('dve', psf(ob_next), 0.0, [('ps', ob_next)])
                                    for qt in range(qt_lo, qt_hi + 1):
                                        tl = qt - 4 * Q
                                        mm(O[:, tl, 0:65], pt[:, 128 * tl:128 * tl + 128], vA[:, c, 65 * g:65 * g + 65], False, c == qt, [pk, vkey], [('ps', ob)], skip=True)
                                    if c == 4 * Q + 3:
                                        finalize(ob, h, branch, Q)
                                    if last_item:
                                        headnorm_nsa(Q, g)
                                return [s1, s2, s3]
                            items.append(mk())
            run_pipeline(items, [0, 1, 2])


        S.barrier()
        A.release(qkv_m)

        qsT = A.alloc([4, 2048], BF16)
        ksbT = A.alloc([4, 2048], BF16)
        vS = A.alloc([16, 512], BF16)
        for c in range(4):
            proj_F(lambda Q, c=c: qsT[:, c, 512 * Q:512 * Q + 512], 0.125, 'qsT')
        for c in range(4):
            proj_F(lambda Q, c=c: ksbT[:, c, 512 * Q:512 * Q + 512], 1.0, 'ksbT')
        for c in range(4):
            def evac_vs(tg, bk, c=c):
                src = psf(bk).rearrange("p (a d) -> p a d", a=4)
                cp('dve', vS[:, 4 * tg:4 * tg + 4, 128 * c:128 * c + 128], src, [('ps', bk)], ['vS'])
            proj_T(evac_vs)

        Eb = [A.alloc([512], BF16) for _ in range(3)]
        SPb = [A.alloc([512], BF16) for _ in range(4)]
        Xb = [A.alloc([512], BF16) for _ in range(2)]
        Pb = [A.alloc([512], BF16) for _ in range(2)]
        osb = A.alloc([32, 64], F32)
        carry = A.alloc([4], F32)
        gsc = A.alloc([4], F32)
        tmps = A.alloc([4, 64], F32)
        sq2 = A.alloc([32, 64], F32)
        ss2h = A.alloc([32], F32)
        build_program.sb_off = A.off
        items = []
        for Q in range(4):
            for h in range(8):
                for c in range(4 * Q + 3, -1, -1):
                    k_ = len(items)
                    def mk(Q=Q, h=h, c=c, k_=k_):
                        hf = slice(64 * (h % 2), 64 * (h % 2) + 64)
                        accv = osb.rearrange("p (a h) d -> p a h d", a=4)[:, :, h, :]
                        qt_lo = max(c, 4 * Q)
                        lo = 128 * (qt_lo - 4 * Q)
                        hi = 512
                        zb = k_ % 2
                        cb_ = 2 + k_ % 2
                        rb_ = 4 + k_ % 2
                        E = Eb[k_ % 3]
                        SP = SPb[k_ % 4]
                        X = Xb[k_ % 2]
                        P = Pb[k_ % 2]
                        kE, kS, kX, kP = ('E', k_ % 3), ('SP', k_ % 4), ('X', k_ % 2), ('P', k_ % 2)
                        diag = c >= 4 * Q
                        R = psf(rb_).rearrange("p (a c) -> p a c", a=4)
                        tl0 = qt_lo - 4 * Q
                        first = (c == 4 * Q + 3)

                        def s_g():
                            if not first:
                                act(gsc, carry, AF.Exp, ['carry'], ['gsc'], scale=-1.0)

                        def s1():
                            mm(psf(zb)[:, lo:hi], ksbT[hf, h // 2, 128 * c:128 * c + 128], qsT[hf, h // 2, 512 * Q + lo:512 * Q + hi], True, not diag, ['ksbT', 'qsT'], [('ps', zb)])
                            if diag:
                                mm(psf(zb)[:, lo:lo + 128], ident, strict, False, True, ['ident', 'strict'], [('ps', zb)])

                        def s2():
                            act(E[:, lo:hi], psf(zb)[:, lo:hi], AF.Exp, [('ps', zb)], [kE])

                        def s2b():
                            act(SP[:, lo:hi], E[:, lo:hi], AF.Ln, [kE], [kS], bias=1.0)

                        def s3():
                            mm(psf(cb_)[:, lo:hi], tri, SP[:, lo:hi], True, True, ['tri', kS], [('ps', cb_)])

                        def s4():
                            act(X[:, lo:hi], psf(cb_)[:, lo:hi], AF.Exp, [('ps', cb_)], [kX], scale=-1.0)

                        def s5():
                            tt('dve', P[:, lo:hi], E[:, lo:hi], X[:, lo:hi], ALU.mult, [kE, kX], [kP])

                        def s6():
                            for tl in range(tl0, 4):
                                mm(R[:, tl, 0:64], P[:, 128 * tl:128 * tl + 128], vS[:, c, 64 * h:64 * h + 64], True, True, [kP, 'vS'], [('ps', rb_)])
                                mm(R[:, tl, 64:65], SP[:, 128 * tl:128 * tl + 128], onescol[:, 0:1], True, True, [kS, 'onescol'], [('ps', rb_)])

                        def s7():
                            if first and h == 0:
                                memset('dve', osb, 0.0, ['osb'])
                            if first:
                                memset('dve', carry, 0.0, ['carry'])
                                memset('dve', gsc, 1.0, ['gsc'])
                            tt('dve', tmps[:, tl0:4, :], R[:, tl0:4, 0:64], gsc[:, tl0:4].unsqueeze(2).to_broadcast([128, 4 - tl0, 64]), ALU.mult, [('ps', rb_), 'gsc'], ['tmps'])
                            tt('dve', accv[:, tl0:4, :], accv[:, tl0:4, :], tmps[:, tl0:4, :], ALU.add, ['tmps', 'osb'], ['osb'])
                            if c > 0:
                                tt('dve', carry[:, tl0:4], carry[:, tl0:4], R[:, tl0:4, 64], ALU.add, [('ps', rb_), 'carry'], ['carry'])
                            if c == 0 and h == 7:
                                tt('dve', sq2, osb, osb, ALU.mult, ['osb'], ['sq2'])
                                S.op('dve', lambda e: e.tensor_reduce(out=ss2h, in_=sq2, axis=AX.X, op=ALU.add), r=['sq2'], w=['ss2h'])
                                rsq(ss2h, 1.0 / 64, 'ss2h')
                                tt('dve', sq2, osb, ss2h.unsqueeze(2).to_broadcast([128, 32, 64]), ALU.mult, ['osb', 'ss2h', 'sq2'], ['sq2'])
                                tt('dve', mixed[:, 4 * Q:4 * Q + 4, 512:1024], sq2.rearrange("p (a h) d -> p a (h d)", a=4),
                                   onormw[:, 512:1024].unsqueeze(1).to_broadcast([128, 4, 512]), ALU.mult, ['sq2', 'onormw'], ['mixed'])
                        return [s1, s2, s3, s4, s2b, s5, s6, s_g, s7]
                    items.append(mk())
        run_pipeline(items, [0, 1, 2, 3, 1, 3, 4, 5, 5])


        S.barrier()
        A.release(m_ab)

        u2T = A.alloc([8, 2048], BF16)
        m_c1 = A.mark()
        wout = A.alloc([8, 1024], BF16)
        mT2 = [A.alloc([8, 128], BF16) for _ in range(2)]
        hbuf2 = [A.alloc([1024], F32) for _ in range(2)]
        S.dma('pool', wout, wout_d.rearrange("c p n -> p c n"), slot='wout', w=['wout'])
        memset('dve', ss, 0.0, [('ss', i_) for i_ in range(16)])
        items = []
        for i in range(16):
            def mk(i=i):
                xb = xbuf[i % 2]
                ub = ubuf[i % 2]
                mT = mT2[i % 2]
                hbuf = hbuf2[i % 2]
                bk = i % 2
                bk2 = 4 + i % 2
                kh = ('hbuf', i % 2)
                km = ('mT', i % 2)

                def sa():
                    for dc in range(8):
                        tp(psb(bk)[:, dc * 128:(dc + 1) * 128], mixed[:, i, dc * 128:(dc + 1) * 128], ident, ['mixed', 'ident'], [('ps', bk)])

                def sb_():
                    cp('act', mT, psb(bk).rearrange("p (a b) -> p a b", a=8), [('ps', bk)], [km])

                def sc_():
                    S.dma('sp', xb, x_d[b, 128 * i:128 * i + 128, :], slot='x%d' % (i % 2), w=[('xbuf', i % 2)])
                    for half in range(2):
                        pb = 2 + half
                        for c in range(8):
                            mm(psf(pb), mT[:, c, :], wout[:, c, 512 * half:512 * half + 512], c == 0, c == 7, [km, 'wout'], [('ps', pb)])
                        tt('dve', hbuf[:, 512 * half:512 * half + 512], psf(pb), xb[:, 512 * half:512 * half + 512], ALU.add, [('ps', pb), ('xbuf', i % 2)], [kh])
                    S.dma('sp', out_d[b, 128 * i:128 * i + 128, :], hbuf, slot='hst%d' % (i % 2), r=[kh], w=[('outh', i)])
                    act(junk, hbuf, AF.Square, [kh], [('ss', i)], accum=ss[:, i:i + 1])
                    cp('dve', rstd[:, i:i + 1], ss[:, i:i + 1], [('ss', i)], [('rstd', i)])
                    rsq(rstd[:, i:i + 1], 1.0 / D, ('rstd', i))
                    stt('dve', ub, hbuf, rstd[:, i:i + 1], normw2, ALU.mult, ALU.mult, [kh, ('rstd', i), 'normw2'], [('ub', i % 2)])

                def sd_():
                    for dc in range(8):
                        tp(psb(bk2)[:, dc * 128:(dc + 1) * 128], ub[:, dc * 128:(dc + 1) * 128], ident, [('ub', i % 2), 'ident'], [('ps', bk2)])

                def se_():
                    cp('act', u2T[:, :, 128 * i:128 * i + 128], psb(bk2).rearrange("p (a b) -> p a b", a=8), [('ps', bk2)], ['u2T'])
                return [sa, sb_, sc_, sd_, se_]
            items.append(mk())
        run_pipeline(items, [0, 1, 2, 3, 4])
        S.barrier()
        A.release(m_seq)
        actT_lo = A.alloc([16, 1024], BF16)
        assert A.off == m_ab
        A.off = m_c1
        actT_hi = A.alloc([6, 1024], BF16)
        wdn = A.alloc([22, 1024], BF16)
        accg = [A.alloc([512], F32) for _ in range(2)]
        accv_ = [A.alloc([512], F32) for _ in range(2)]
        sil = [A.alloc([512], F32) for _ in range(2)]
        obuf = A.alloc([1024], F32)
        fx = A.alloc([8], F32)

        def actT(j):
            return actT_lo[:, j, :] if j < 16 else actT_hi[:, j - 16, :]

        memset('dve', halo, 0.0, ['halo'])
        for blk in range(2):
            S.dma('pool', wdn, wdn_d.rearrange("j p n -> p j n"), slot='wdn', w=['wdn'])
            for j in range(22):
                wg, wgk = w_get()
                wv, wvk = w_get()
                for tb in range(2):
                    n_ = (j * 2 + tb) % 2
                    col0 = 1024 * blk + 512 * tb
                    for which, wch, wk, pb, accs, fc in ((0, wg, wgk, 0 + n_, accg, j), (1, wv, wvk, 2 + n_, accv_, 22 + j)):
                        for dc in range(8):
                            mm(psf(pb), wch[:, dc, :], u2T[:, dc, col0:col0 + 512], dc == 0, dc == 7, [wk, 'u2T'], [('ps', pb)])
                        ac = accs[n_]
                        ak = ('acc', which, n_)
                        G = psf(pb)
                        S.op('act', lambda e, o=ac, i=G, fc=fc: e.activation(out=o, in_=i, func=AF.Identity, bias=convp[:, fc, 3:4], scale=convp[:, fc, 2:3]),
                             r=[('ps', pb), 'convp'], w=[ak])
                        stt('dve', ac[:, 1:512], G[:, 0:511], convp[:, fc, 1:2], ac[:, 1:512], ALU.mult, ALU.add, [('ps', pb), 'convp', ak], [ak])
                        stt('dve', ac[:, 2:512], G[:, 0:510], convp[:, fc, 0:1], ac[:, 2:512], ALU.mult, ALU.add, [('ps', pb), 'convp', ak], [ak])
                        stt('dve', ac[:, 0:1], halo[:, fc, 1:2], convp[:, fc, 1:2], ac[:, 0:1], ALU.mult, ALU.add, ['halo', 'convp', ak], [ak])
                        stt('dve', ac[:, 0:2], halo[:, fc, 0:2], convp[:, fc, 0:1], ac[:, 0:2], ALU.mult, ALU.add, ['halo', 'convp', ak], [ak])
                        cp('dve', halo[:, fc, :], G[:, 510:512], [('ps', pb)], ['halo'])
                    act(sil[n_], accg[n_], AF.Silu, [('acc', 0, n_)], [('sil', n_)])
                    tt('pool', actT(j)[:, 512 * tb:512 * tb + 512], sil[n_], accv_[n_], ALU.mult, [('sil', n_), ('acc', 1, n_)], ['actT'])
                w_issue()
                w_issue()
            memset('dve', ss, 0.0, ['ss'])
            for tl in range(8):
                i = 8 * blk + tl
                xb = xbuf[i % 2]
                S.dma('sp', xb, out_d[b, 128 * i:128 * i + 128, :], slot='x%d' % (i % 2), r=[('outh', i)], w=[('xbuf', i % 2)])
                for half in range(2):
                    pb = 4 + (2 * tl + half) % 4
                    for j in range(22):
                        mm(psf(pb), actT(j)[:, 128 * tl:128 * tl + 128], wdn[:, j, 512 * half:512 * half + 512], j == 0, j == 21, ['actT', 'wdn'], [('ps', pb)])
                    tt('dve', xb[:, 512 * half:512 * half + 512], psf(pb), xb[:, 512 * half:512 * half + 512], ALU.add, [('ps', pb), ('xbuf', i % 2)], [('xbuf', i % 2)])
                act(junk, xb, AF.Square, [('xbuf', i % 2)], ['junk', 'ss'], accum=ss[:, tl:tl + 1])
                cp('dve', fx[:, tl:tl + 1], ss[:, tl:tl + 1], ['ss'], ['fx'])
                rsq(fx[:, tl:tl + 1], 1.0 / D, 'fx')
                stt('dve', obuf, xb, fx[:, tl:tl + 1], normwf, ALU.mult, ALU.mult, [('xbuf', i % 2), 'fx', 'normwf'], ['obuf'])
                S.dma('sp', out_d[b, 128 * i:128 * i + 128, :], obuf, slot='ost', r=['obuf'], w=[('outf', i)])
        S.barrier()
        A.release(m_seq)

    S.barrier()
    build_program.info = {'peak': A.peak, 'ops': dict(S.cnt)}
    S.emit(nc, st)
    st.close()
    return nc


_CACHE = {}


def kernel(**inputs):
    x = np.ascontiguousarray(np.asarray(inputs['x'], dtype=np.float32))
    B = x.shape[0]
    nseq = B // NCORE
    w = _prep_weights(inputs)
    if nseq not in _CACHE:
        _CACHE[nseq] = build_program(nseq)
    nc = _CACHE[nseq]
    in_maps = []
    for c in range(NCORE):
        m = dict(w)
        m['x'] = np.ascontiguousarray(x[c * nseq:(c + 1) * nseq])
        in_maps.append(m)
    res = run_bass_kernel_spmd(nc, in_maps, core_ids=list(range(NCORE)))
    out = np.concatenate([np.asarray(r['out'], dtype=np.float32) for r in res.results], axis=0)
    return out
```
